# Optimizing a Trainium2 kernel written in Bass

```python
import math
import jax
import jax.numpy as jnp
from jax import lax
import numpy as np

D_MODEL = 2048
BATCH = 4
SEQ = 8192
DEPTH = 4

CTX_LEN = 256
GRID_W = 64
HEAD_DIM = 128
NORM_EPS = 1e-6
NEG_INF = -1e30
ROPE_THETA = 10000.0
F32 = jnp.float32

NA_HEADS = D_MODEL // (2 * HEAD_DIM)
NA_WIDTH = NA_HEADS * HEAD_DIM
NA_WIN_ROWS = 8
NA_WIN_COLS = 16

GLA_HEADS = 4
GLA_DK = 128
GLA_DV = D_MODEL // (2 * GLA_HEADS)
GLA_QK = GLA_HEADS * GLA_DK
GLA_V = GLA_HEADS * GLA_DV
GLA_LOWRANK = 16
GLA_GATE_NORMALIZER = 16.0
GLA_CHUNK = 64
GLA_DECAY_UNIQUE = GLA_HEADS * GLA_DK // 2

EVEN_IN = 3 * NA_WIDTH + 2 * GLA_QK + 2 * GLA_V + 2 * GLA_LOWRANK
EVEN_MIX = NA_WIDTH + GLA_V

HY_WIDTH = D_MODEL // 2
HY_ORDER = 2
HY_EMB = 33
HY_FFN = 64
HY_SIN_FREQ = 1.0
HY_DECAY_TARGET = 1e-2
HY_FAST_DECAY = 0.3
HY_SLOW_DECAY = 1.5
SC_WIDTH = D_MODEL // 2
CONV_W = 3
ODD_IN = (HY_ORDER + 1) * HY_WIDTH + 3 * SC_WIDTH
ODD_MIX = HY_WIDTH + SC_WIDTH

N_GROUPS = 4
EXPERTS_PER_GROUP = 8
N_EXPERTS = N_GROUPS * EXPERTS_PER_GROUP
TOP_K_IN_GROUP = 2
D_EXPERT = 512
MOE_BLOCK = 128

kernel_name = 'hybrid_na_gla_hyena_shortconv_hmoe_dit'


def rms_norm(x, gain):
    xf = x.astype(F32)
    y = xf * lax.rsqrt(jnp.mean(xf * xf, axis=-1, keepdims=True) + NORM_EPS)
    return (y * gain.astype(F32)).astype(x.dtype)


def modulate(h, shift, scale):
    return h * (1.0 + scale) + shift


def dwconv_centred(u, w):
    return lax.conv_general_dilated(
        u, w.astype(u.dtype)[:, None, :], window_strides=(1,),
        padding=[(CONV_W // 2, CONV_W // 2)],
        dimension_numbers=('NWC', 'WIO', 'NWC'),
        feature_group_count=u.shape[-1])


def axial_rope(t):
    L, dh = t.shape[1], t.shape[-1]
    half = dh // 2
    nf = half // 2
    pos = jnp.arange(L)
    inv_freq = ROPE_THETA ** (-jnp.arange(nf, dtype=F32) / nf)

    def rotate(xp, p):
        ang = p.astype(F32)[:, None] * inv_freq[None, :]
        cos = jnp.cos(ang)[None, :, None, :]
        sin = jnp.sin(ang)[None, :, None, :]
        x1, x2 = xp[..., :nf], xp[..., nf:]
        return jnp.concatenate([x1 * cos - x2 * sin, x2 * cos + x1 * sin], axis=-1)

    tf = t.astype(F32)
    return jnp.concatenate([rotate(tf[..., :half], pos // GRID_W),
                            rotate(tf[..., half:], pos % GRID_W)], axis=-1)


def neighbourhood_attention(q, k, v, k_ctx, v_ctx, rpb):
    B, S, H, dh = q.shape
    rows = S // GRID_W
    kr = min(NA_WIN_ROWS, rows)
    kc = NA_WIN_COLS
    r = jnp.arange(rows)
    row_start = jnp.clip(r - kr // 2, 0, rows - kr)
    row_idx = row_start[:, None] + jnp.arange(kr)[None, :]
    cols = jnp.arange(GRID_W)
    col_start = jnp.clip(cols - kc // 2, 0, GRID_W - kc)
    col_ok = (cols[None, :] >= col_start[:, None]) & (cols[None, :] < col_start[:, None] + kc)
    roff = row_idx - r[:, None] + NA_WIN_ROWS - 1
    coff = jnp.clip(cols[None, :] - cols[:, None], -(kc - 1), kc - 1) + kc - 1
    bias = jnp.take(rpb.astype(F32)[:, roff], coff, axis=-1)
    bias = jnp.where(col_ok[None, None, None], bias, NEG_INF).transpose(0, 1, 3, 2, 4)
    scale = dh ** -0.5
    qg = q.reshape(B, rows, GRID_W, H, dh)
    kg = k.reshape(B, rows, GRID_W, H, dh)[:, row_idx]
    vg = v.reshape(B, rows, GRID_W, H, dh)[:, row_idx]
    s_loc = jnp.einsum('brqhd,brkwhd->bhrqkw', qg, kg, preferred_element_type=F32) * scale + bias[None]
    s_ctx = jnp.einsum('brqhd,bchd->bhrqc', qg, k_ctx, preferred_element_type=F32) * scale
    m = jnp.maximum(s_loc.max(axis=(-2, -1)), s_ctx.max(axis=-1))
    p_loc = jnp.exp(s_loc - m[..., None, None])
    p_ctx = jnp.exp(s_ctx - m[..., None])
    den = p_loc.sum(axis=(-2, -1)) + p_ctx.sum(axis=-1)
    o = (jnp.einsum('bhrqkw,brkwhd->brqhd', p_loc, vg.astype(F32))
         + jnp.einsum('bhrqc,bchd->brqhd', p_ctx, v_ctx.astype(F32)))
    o = o / den.transpose(0, 2, 3, 1)[..., None]
    return o.reshape(B, S, H * dh).astype(q.dtype)


def context_attention(q, k, v):
    B, L, H, dh = q.shape
    s = jnp.einsum('bqhd,bkhd->bhqk', q, k, preferred_element_type=F32) * dh ** -0.5
    p = jax.nn.softmax(s, axis=-1)
    o = jnp.einsum('bhqk,bkhd->bqhd', p, v.astype(F32))
    return o.reshape(B, L, H * dh).astype(q.dtype)


def tie_rotary_pairs(u):
    lead = u.shape[:-1]
    u = u.reshape(lead + (GLA_HEADS, 2, 1, GLA_DK // 4))
    u = jnp.broadcast_to(u, lead + (GLA_HEADS, 2, 2, GLA_DK // 4))
    return u.reshape(lead + (GLA_HEADS, GLA_DK))


def gla_chunked(q, k, v, log_a, s0):
    B, H, L, dk = q.shape
    dv = v.shape[-1]
    C = GLA_CHUNK
    n = L // C
    q, k, log_a = (t.reshape(B, H, n, C, dk) for t in (q, k, log_a))
    v = v.reshape(B, H, n, C, dv)
    b = jnp.cumsum(log_a, axis=3)
    b_last = b[:, :, :, -1:, :]
    b_mid = b[:, :, :, C // 2 - 1:C // 2, :]
    att = jnp.einsum('bhncd,bhnsd->bhncs', q * jnp.exp(b - b_mid), k * jnp.exp(b_mid - b))
    att = jnp.where(jnp.tril(jnp.ones((C, C), dtype=bool)), att, 0.0)
    o = jnp.einsum('bhncs,bhnsv->bhncv', att, v)
    d_state = jnp.einsum('bhncd,bhncv->bhndv', k * jnp.exp(b_last - b), v)
    decay = jnp.exp(b_last[:, :, :, 0, :])

    def step(s, inp):
        ds, dec = inp
        return dec[..., None] * s + ds, s

    s_fin, s_start = lax.scan(step, s0, (jnp.moveaxis(d_state, 2, 0), jnp.moveaxis(decay, 2, 0)))
    o = o + jnp.einsum('bhncd,bhndv->bhncv', q * jnp.exp(b), jnp.moveaxis(s_start, 0, 2))
    return o.reshape(B, H, L, dv), s_fin


def gla_mixer(p_lat, p_ctx, ctx_out, gk_up, gk_bias, gnorm):
    def prep(p, latent):
        q, k, v, g, lr = p
        B, L = q.shape[:2]
        q = q.reshape(B, L, GLA_HEADS, GLA_DK)
        k = k.reshape(B, L, GLA_HEADS, GLA_DK)
        if latent:
            q, k = axial_rope(q), axial_rope(k)
        z = jnp.einsum('blnr,nru->blnu', lr.reshape(B, L, 2, GLA_LOWRANK).astype(F32),
                       gk_up.astype(F32)) + gk_bias.astype(F32)
        log_a = tie_rotary_pairs(jax.nn.log_sigmoid(z) / GLA_GATE_NORMALIZER)
        bh = lambda a: jnp.swapaxes(a.astype(F32), 1, 2)
        return (bh(q) * GLA_DK ** -0.5, bh(k), bh(v.reshape(B, L, GLA_HEADS, GLA_DV)),
                bh(log_a[:, :, 0]), bh(log_a[:, :, 1]), g)

    qc, kc, vc, fc, bc, gc = prep(p_ctx, False)
    ql, kl, vl, fl, bl, gl = prep(p_lat, True)
    zero = jnp.zeros(qc.shape[:2] + (GLA_DK, GLA_DV), F32)
    flip = lambda a: jnp.flip(a, axis=2)
    oc_f, s_f = gla_chunked(qc, kc, vc, fc, zero)
    ol_f, _ = gla_chunked(ql, kl, vl, fl, s_f)
    oc_b, s_b = gla_chunked(flip(qc), flip(kc), flip(vc), flip(bc), zero)
    ol_b, _ = gla_chunked(flip(ql), flip(kl), flip(vl), flip(bl), s_b)

    def finish(o, g):
        o = jnp.swapaxes(o, 1, 2)
        o = o * lax.rsqrt(jnp.mean(o * o, axis=-1, keepdims=True) + NORM_EPS) * gnorm.astype(F32)
        return (o.reshape(o.shape[0], o.shape[1], GLA_V) * jax.nn.silu(g.astype(F32))).astype(g.dtype)

    y_lat = finish(ol_f + flip(ol_b), gl)
    y_ctx = finish(oc_f + flip(oc_b), gc) if ctx_out else None
    return y_lat, y_ctx


def even_mixer(h, hc, ctx_out, w_in, w_out, rpb, gk_up, gk_bias, gnorm):
    bounds = [NA_WIDTH, 2 * NA_WIDTH, 3 * NA_WIDTH, 3 * NA_WIDTH + GLA_QK, 3 * NA_WIDTH + 2 * GLA_QK,
              3 * NA_WIDTH + 2 * GLA_QK + GLA_V, 3 * NA_WIDTH + 2 * GLA_QK + 2 * GLA_V]
    heads = lambda t: t.reshape(t.shape[0], t.shape[1], NA_HEADS, HEAD_DIM)
    pl = jnp.split(h @ w_in, bounds, axis=-1)
    pc = jnp.split(hc @ w_in, bounds, axis=-1)
    qa_l, ka_l, va_l = (heads(t) for t in pl[:3])
    qa_c, ka_c, va_c = (heads(t) for t in pc[:3])
    ya = neighbourhood_attention(qa_l, ka_l, va_l, ka_c, va_c, rpb)
    yb, yb_c = gla_mixer(pl[3:], pc[3:], ctx_out, gk_up, gk_bias, gnorm)
    y = jnp.concatenate([ya, yb], axis=-1) @ w_out
    if not ctx_out:
        return y, None
    ya_c = context_attention(qa_c, ka_c, va_c)
    return y, jnp.concatenate([ya_c, yb_c], axis=-1) @ w_out


def hyena_filters(L, w1, b1, w2, b2, w3, b3, w4):
    t = jnp.linspace(0.0, 1.0, L, dtype=F32)[:, None]
    bands = (HY_EMB - 1) // 2
    freqs = jnp.linspace(1e-4, bands - 1, bands, dtype=F32)[None, :]
    w = (2.0 * math.pi / L) * jnp.arange(L, dtype=F32)[:, None]
    emb = jnp.concatenate([t, jnp.cos(freqs * w), -jnp.sin(freqs * w)], axis=-1)
    a = jnp.sin(HY_SIN_FREQ * (emb @ w1.astype(F32) + b1.astype(F32)))
    a = jnp.sin(HY_SIN_FREQ * (a @ w2.astype(F32) + b2.astype(F32)))
    a = jnp.sin(HY_SIN_FREQ * (a @ w3.astype(F32) + b3.astype(F32)))
    h = (a @ w4.astype(F32)).reshape(L, HY_ORDER, 2, HY_WIDTH)
    deltas = jnp.abs(jnp.linspace(math.log(HY_DECAY_TARGET) / HY_SLOW_DECAY,
                                  math.log(HY_DECAY_TARGET) / HY_FAST_DECAY, HY_WIDTH, dtype=F32))
    h = h * jnp.exp(-t * deltas)[:, None, None, :]
    return h / (jnp.sum(jnp.abs(h), axis=(0, 2), keepdims=True) + NORM_EPS)


def bidir_long_conv(z, h_fwd, h_bwd, bias):
    L = z.shape[1]
    n = 2 * L
    Z = jnp.fft.rfft(z, n=n, axis=1)
    Hs = jnp.fft.rfft(h_fwd, n=n, axis=0) + jnp.conj(jnp.fft.rfft(h_bwd, n=n, axis=0))
    y = jnp.fft.irfft(Z * Hs[None], n=n, axis=1)[:, :L]
    return y + z * bias


def odd_mixer(h, hc, w_in, w_out, hy_short, hy_w1, hy_b1, hy_w2, hy_b2, hy_w3, hy_b3, hy_w4, hy_bias, sc_conv):
    n_hy = (HY_ORDER + 1) * HY_WIDTH

    def mix(u):
        L = u.shape[1]
        p = u @ w_in
        v, *gates = jnp.split(dwconv_centred(p[..., :n_hy], hy_short), HY_ORDER + 1, axis=-1)
        filt = hyena_filters(L, hy_w1, hy_b1, hy_w2, hy_b2, hy_w3, hy_b3, hy_w4)
        z = v.astype(F32)
        for n, gate in enumerate(gates):
            z = gate.astype(F32) * bidir_long_conv(z, filt[:, n, 0], filt[:, n, 1], hy_bias[n].astype(F32))
        b_gate, c_gate, x_in = jnp.split(p[..., n_hy:], 3, axis=-1)
        y_sc = b_gate * dwconv_centred(c_gate * x_in, sc_conv)
        return jnp.concatenate([z.astype(u.dtype), y_sc], axis=-1) @ w_out

    return mix(h), (None if hc is None else mix(hc))


def hier_moe(h, w_group, b_group, w_expert, b_expert, w1, w3, w2):
    N, D = h.shape
    hf = h.astype(F32)
    lg = hf @ w_group.astype(F32) + b_group.astype(F32)
    grp = jnp.argmax(lg, axis=-1)
    p_grp = jnp.take_along_axis(jax.nn.softmax(lg, axis=-1), grp[:, None], axis=-1)
    le = (hf @ w_expert.astype(F32) + b_expert.astype(F32)).reshape(N, N_GROUPS, EXPERTS_PER_GROUP)
    le = jnp.take_along_axis(le, grp[:, None, None], axis=1)[:, 0]
    top_v, top_i = lax.top_k(le, TOP_K_IN_GROUP)
    gate = p_grp * jax.nn.softmax(top_v, axis=-1)
    eid = grp[:, None] * EXPERTS_PER_GROUP + top_i
    A = N * TOP_K_IN_GROUP
    e_flat = eid.reshape(A)
    tok_flat = jnp.arange(A) // TOP_K_IN_GROUP
    order = jnp.argsort(e_flat)
    e_sorted = e_flat[order]
    counts = jax.ops.segment_sum(jnp.ones_like(e_flat), e_flat, num_segments=N_EXPERTS)
    padded = (counts + MOE_BLOCK - 1) // MOE_BLOCK * MOE_BLOCK
    start = jnp.cumsum(counts) - counts
    pstart = jnp.cumsum(padded) - padded
    slot = pstart[e_sorted] + jnp.arange(A) - start[e_sorted]
    n_blocks = -(-A // MOE_BLOCK) + N_EXPERTS
    cap = n_blocks * MOE_BLOCK
    slot_tok = jnp.full((cap,), N, dtype=jnp.int32).at[slot].set(tok_flat[order])
    h_pad = jnp.concatenate([h, jnp.zeros((1, D), h.dtype)], axis=0)
    xs = h_pad[slot_tok].reshape(n_blocks, MOE_BLOCK, D)
    block_expert = jnp.minimum(
        jnp.searchsorted(jnp.cumsum(padded), jnp.arange(n_blocks) * MOE_BLOCK, side='right'), N_EXPERTS - 1)

    def expert_block(args):
        xb, e = args
        return (jax.nn.silu(xb @ w1[e]) * (xb @ w3[e])) @ w2[e]

    ys = lax.map(expert_block, (xs, block_expert)).reshape(cap, D)
    contrib = ys[slot] * gate.reshape(A)[order][:, None].astype(ys.dtype)
    return jnp.zeros_like(h).at[tok_flat[order]].add(contrib)


def setup_inputs(seed: int = 0) -> dict:
    key = jax.random.key(seed)
    keys = iter(jax.random.split(key, 40))

    def rnd(shape, std):
        return jax.random.normal(next(keys), shape, F32) * std

    D = D_MODEL
    NL, NE, NO = DEPTH, (DEPTH + 1) // 2, DEPTH // 2
    return {
        'x': rnd((BATCH, SEQ, D), 1.0),
        'c': rnd((BATCH, D), 1.0),
        'ctx': rnd((BATCH, CTX_LEN, D), 1.0),
        'c_ctx': rnd((D,), 1.0),
        'w_mod': rnd((NL, D, 6 * D), 0.5 * D ** -0.5),
        'b_mod': rnd((NL, 6 * D), 0.02),
        'norm_mix': 1.0 + rnd((NL, D), 0.02),
        'norm_ffn': 1.0 + rnd((NL, D), 0.02),
        'norm_final': 1.0 + rnd((D,), 0.02),
        'w_in_even': rnd((NE, D, EVEN_IN), D ** -0.5),
        'w_out_even': rnd((NE, EVEN_MIX, D), EVEN_MIX ** -0.5),
        'na_rpb': rnd((NE, NA_HEADS, 2 * NA_WIN_ROWS - 1, 2 * NA_WIN_COLS - 1), 0.02),
        'gla_gk_up': rnd((NE, 2, GLA_LOWRANK, GLA_DECAY_UNIQUE), GLA_LOWRANK ** -0.5),
        'gla_gk_bias': rnd((NE, 2, GLA_DECAY_UNIQUE), 0.1),
        'gla_norm': 1.0 + rnd((NE, GLA_DV), 0.02),
        'w_in_odd': rnd((NO, D, ODD_IN), D ** -0.5),
        'w_out_odd': rnd((NO, ODD_MIX, D), ODD_MIX ** -0.5),
        'hy_short': rnd((NO, CONV_W, (HY_ORDER + 1) * HY_WIDTH), CONV_W ** -0.5),
        'hy_w1': rnd((NO, HY_EMB, HY_FFN), HY_EMB ** -0.5),
        'hy_b1': rnd((NO, HY_FFN), 0.02),
        'hy_w2': rnd((NO, HY_FFN, HY_FFN), HY_FFN ** -0.5),
        'hy_b2': rnd((NO, HY_FFN), 0.02),
        'hy_w3': rnd((NO, HY_FFN, HY_FFN), HY_FFN ** -0.5),
        'hy_b3': rnd((NO, HY_FFN), 0.02),
        'hy_w4': rnd((NO, HY_FFN, HY_ORDER * 2 * HY_WIDTH), HY_FFN ** -0.5),
        'hy_bias': rnd((NO, HY_ORDER, HY_WIDTH), 1.0),
        'sc_conv': rnd((NO, CONV_W, SC_WIDTH), CONV_W ** -0.5),
        'moe_w_group': rnd((NL, D, N_GROUPS), D ** -0.5),
        'moe_b_group': rnd((NL, N_GROUPS), 0.01),
        'moe_w_expert': rnd((NL, D, N_EXPERTS), D ** -0.5),
        'moe_b_expert': rnd((NL, N_EXPERTS), 0.01),
        'moe_w1': rnd((NL, N_EXPERTS, D, D_EXPERT), D ** -0.5),
        'moe_w3': rnd((NL, N_EXPERTS, D, D_EXPERT), D ** -0.5),
        'moe_w2': rnd((NL, N_EXPERTS, D_EXPERT, D), D_EXPERT ** -0.5),
    }


def reference(x, c, ctx, c_ctx, w_mod, b_mod, norm_mix, norm_ffn, norm_final,
              w_in_even, w_out_even, na_rpb, gla_gk_up, gla_gk_bias, gla_norm,
              w_in_odd, w_out_odd, hy_short, hy_w1, hy_b1, hy_w2, hy_b2, hy_w3, hy_b3, hy_w4, hy_bias, sc_conv,
              moe_w_group, moe_b_group, moe_w_expert, moe_b_expert, moe_w1, moe_w3, moe_w2):
    B, S, D = x.shape
    Lc = ctx.shape[1]
    last_even = 2 * ((DEPTH - 1) // 2)
    for l in range(DEPTH):
        even = (l % 2 == 0)
        ctx_out = l < last_even
        need_ctx = even or ctx_out
        mod = jnp.split((jax.nn.silu(c) @ w_mod[l] + b_mod[l])[:, None, :], 6, axis=-1)
        hx = modulate(rms_norm(x, norm_mix[l]), mod[0], mod[1])
        hc = None
        if need_ctx:
            cmod = jnp.split(jax.nn.silu(c_ctx) @ w_mod[l] + b_mod[l], 6)
            hc = modulate(rms_norm(ctx, norm_mix[l]), cmod[0], cmod[1])
        if even:
            e = l // 2
            y, yc = even_mixer(hx, hc, ctx_out, w_in_even[e], w_out_even[e], na_rpb[e],
                               gla_gk_up[e], gla_gk_bias[e], gla_norm[e])
        else:
            o = l // 2
            y, yc = odd_mixer(hx, hc if ctx_out else None, w_in_odd[o], w_out_odd[o], hy_short[o],
                              hy_w1[o], hy_b1[o], hy_w2[o], hy_b2[o], hy_w3[o], hy_b3[o], hy_w4[o],
                              hy_bias[o], sc_conv[o])
        x = x + mod[2] * y
        if ctx_out:
            ctx = ctx + cmod[2] * yc
        tokens = modulate(rms_norm(x, norm_ffn[l]), mod[3], mod[4]).reshape(B * S, D)
        if ctx_out:
            hc2 = modulate(rms_norm(ctx, norm_ffn[l]), cmod[3], cmod[4])
            tokens = jnp.concatenate([tokens, hc2.reshape(B * Lc, D)], axis=0)
        f = hier_moe(tokens, moe_w_group[l], moe_b_group[l], moe_w_expert[l], moe_b_expert[l],
                     moe_w1[l], moe_w3[l], moe_w2[l])
        x = x + mod[5] * f[:B * S].reshape(B, S, D)
        if ctx_out:
            ctx = ctx + cmod[5] * f[B * S:].reshape(B, Lc, D)
    return rms_norm(x, norm_final)
```

```python
import contextlib
import math
import numpy as np
import concourse.bass as bass
import concourse.mybir as mybir
from concourse.bass_utils import run_bass_kernel_spmd

F32 = mybir.dt.float32
BF16 = mybir.dt.bfloat16
I32 = mybir.dt.int32
AF = mybir.ActivationFunctionType
ALU = mybir.AluOpType
AX = mybir.AxisListType


class Buf:
    def __init__(self, name, ap=None, dram=False):
        self.name = name
        self.ap = ap
        self.dram = dram
        self.w = {}
        self.r = {}
        self.dsem = None
        self.dval = 0

    def __getitem__(self, k):
        return self.ap[k]


class Prog:
    ENG = ("pe", "act", "dve", "pool", "sp")

    def __init__(self, nc, stack):
        self.nc = nc
        self.gstack = stack
        self.stacks = [stack]
        self.eng = {"pe": nc.tensor, "act": nc.scalar, "dve": nc.vector, "pool": nc.gpsimd, "sp": nc.sync}
        self.semobj = {}
        self.semval = {}
        for e in ("pe", "act", "dve", "pool"):
            self.semobj[e] = stack.enter_context(nc.semaphore("s_" + e))
            self.semval[e] = 0
        self.waited = {e: {} for e in self.ENG}
        self.free_dsems = []
        self.ndsem = 0
        self.live = [[]]
        self.ninst = {e: 0 for e in self.ENG}
        self.semA = stack.enter_context(nc.semaphore("s_rsA"))
        self.semB = stack.enter_context(nc.semaphore("s_rsB"))
        self.epoch = 0
        self.LIMIT = 1 << 30

    @contextlib.contextmanager
    def scope(self):
        self.barrier()
        st = contextlib.ExitStack()
        self.stacks.append(st)
        self.live.append([])
        try:
            with st:
                yield
                self.barrier()
        finally:
            self.stacks.pop()
            for b in self.live.pop():
                if b.dsem is not None:
                    self.free_dsems.append(b.dsem)
                    b.dsem = None

    def sbuf(self, name, shape, dtype):
        self.uid = getattr(self, "uid", 0) + 1
        name = "%s_%d" % (name, self.uid)
        t = self.stacks[-1].enter_context(self.nc.sbuf_tensor(name, list(shape), dtype))
        b = Buf(name, t)
        self.live[-1].append(b)
        return b

    def psum(self, name, shape, dtype):
        self.uid = getattr(self, "uid", 0) + 1
        name = "%s_%d" % (name, self.uid)
        t = self.stacks[-1].enter_context(self.nc.psum_tensor(name, list(shape), dtype))
        b = Buf(name, t)
        self.live[-1].append(b)
        return b

    def dram(self, name, shape, dtype, kind="Internal"):
        t = self.nc.dram_tensor(name, list(shape), dtype, kind=kind)
        b = Buf(name, t.ap(), dram=True)
        b.handle = t
        self.live[0].append(b)
        return b

    def _dsem(self, b):
        if b.dsem is None:
            if self.free_dsems:
                b.dsem = self.free_dsems.pop()
            else:
                b.dsem = "d%d" % self.ndsem
                self.ndsem += 1
                self.semobj[b.dsem] = self.gstack.enter_context(self.nc.semaphore(b.dsem))
                self.semval[b.dsem] = 0
        return b.dsem

    def _wait(self, e, key, val):
        if val <= self.waited[e].get(key, 0):
            return
        self.waited[e][key] = val
        self.eng[e].wait_ge(self.semobj[key], val)
        self.ninst[e] += 1

    def _deps(self, e, reads, writes, pe_acc=False):
        for b in reads:
            for k, v in b.w.items():
                self._wait(e, k, v)
        for b in writes:
            for k, v in b.w.items():
                if b.dram:
                    continue
                if pe_acc and k == "pe":
                    continue
                self._wait(e, k, v)
            for k, v in b.r.items():
                self._wait(e, k, v)

    def _commit(self, reads, writes, key, val):
        for b in reads:
            b.r[key] = max(b.r.get(key, 0), val)
        for b in writes:
            if b.dram:
                b.w[key] = max(b.w.get(key, 0), val)
            else:
                b.w = {key: val}
            b.r = {}

    def reset(self):
        self.barrier()
        self.epoch += 1
        for e in self.ENG:
            self.eng[e].sem_inc(self.semA, 1)
        self.eng["sp"].wait_ge(self.semA, 5 * self.epoch)
        for key, v in self.semval.items():
            if v > 0:
                self.eng["sp"].sem_clear(self.semobj[key])
        self.eng["sp"].sem_inc(self.semB, 1)
        for e in self.ENG:
            self.eng[e].wait_ge(self.semB, self.epoch)
        for key in self.semval:
            self.semval[key] = 0
        self.waited = {e: {} for e in self.ENG}

    def _check(self):
        if max(self.semval.values()) >= self.LIMIT:
            self.reset()

    def op(self, e, fn, reads=(), writes=(), pe_acc=False):
        self._check()
        self._deps(e, reads, writes, pe_acc)
        ins = fn(self.eng[e])
        self.semval[e] += 1
        ins.then_inc(self.semobj[e], 1)
        self.ninst[e] += 1
        self._commit(reads, writes, e, self.semval[e])

    def dma(self, q, out_ap, in_ap, dst, src, extra_reads=(), **kw):
        self.dma_raw(q, lambda e: e.dma_start(out=out_ap, in_=in_ap, **kw), dst, [src] + list(extra_reads))

    def dma_raw(self, q, fn, dst, reads):
        self._check()
        self._deps(q, reads, [dst])
        key = self._dsem(dst)
        self.semval[key] += 16
        fn(self.eng[q]).then_inc(self.semobj[key], 16)
        self.ninst[q] += 1
        self._commit(reads, [dst], key, self.semval[key])

    def barrier(self):
        for e in self.ENG:
            for k, v in self.semval.items():
                if v > 0:
                    self._wait(e, k, v)
        for lst in self.live:
            for b in lst:
                b.w = {}
                b.r = {}

D = 2048
KC = 16
EVEN_IN = 6176
ODD_IN = 6144
EPS = 1e-6
BIGIDX = float(1 << 22)


class Cfg:
    def __init__(self, NB, S, layers, NG=4, EPG=8, LC=256):
        self.NB, self.S, self.LC = NB, S, LC
        self.layers = layers
        self.NG, self.EPG, self.E = NG, EPG, NG * EPG
        self.DE = 512
        self.TB = S + LC
        self.NT = NB * self.TB
        self.ntile = self.NT // 128
        self.rows = S // 64
        self.A = 2 * self.NT
        self.nblk = self.A // 128 + self.E
        self.cap = self.nblk * 128

    def groups(self, with_ctx=True):
        gs = []
        for b in range(self.NB):
            base = b * self.TB
            for g in range(self.S // 512):
                gs.append((base + 512 * g, 512, b, False))
            if with_ctx:
                gs.append((base + self.S, self.LC, b, True))
        return gs


class K:
    debug = False

    def dump(self, name, buf):
        if not self.debug:
            return
        P = self.P
        shape = list(buf.ap.shape)
        o = P.dram("dbg_" + name, shape, buf.ap.dtype, kind="ExternalOutput")
        P.barrier()
        if len(shape) == 3:
            for i in range(shape[0]):
                P.dma("sp", o.ap[i], buf.ap[i], o, buf)
        else:
            P.dma("sp", o.ap[:], buf.ap[:], o, buf)
        P.barrier()


def bc_rows(ap_row, n=128):
    return ap_row.partition_broadcast(n)


def build_program(cfg):
    nc = bass.Bass("TRN2", target_bir_lowering=False)
    with contextlib.ExitStack() as st:
        P = Prog(nc, st)
        k = K()
        k.P, k.cfg, k.nc = P, cfg, nc
        k.debug = getattr(cfg, 'debug', False)
        NB, S, LC, NT, E, NG = cfg.NB, cfg.S, cfg.LC, cfg.NT, cfg.E, cfg.NG
        NL = len(cfg.layers)
        NE = sum(1 for kd, _ in cfg.layers if kd == "even")
        NO = sum(1 for kd, _ in cfg.layers if kd == "odd")
        I = {}

        def inp(name, shape, dt=F32):
            I[name] = P.dram(name, shape, dt, kind="ExternalInput")
            return I[name]
        inp("x", [NB * S, D]); inp("ctx", [NB * LC, D]); inp("cvec", [NB + 1, D])
        inp("w_mod", [NL, D, 6 * D]); inp("b_mod", [NL, 6 * D])
        inp("norm_mix", [NL, D]); inp("norm_ffn", [NL, D]); inp("norm_final", [1, D])
        inp("w_router", [NL, D, NG + E]); inp("b_router", [NL, NG + E])
        inp("w1h", [NL * E * 128, KC * 512]); inp("w3h", [NL * E * 128, KC * 512]); inp("w2h", [NL * E * 128, 4 * D])
        inp("ident", [128, 128]); inp("lstrict", [128, 128]); inp("iota_p", [128, 1])
        inp("blkstart", [1, cfg.nblk]); inp("iota_e", [1, E])
        if NE:
            inp("w_in_even", [NE, D, EVEN_IN]); inp("w_out_even", [NE, D, D])
            inp("na_bias", [NE, 8, 8, 64, 512]); inp("gk_up", [NE, 2, 17, 256]); inp("gla_norm", [NE, 256])
            inp("rope_q", [S, 2, 64]); inp("rope_k", [S, 2, 64])
            inp("gla_cm", [2, 4, 128, 128]); inp("gla_mask", [2, 128, 128])
        if NO:
            inp("w_in_odd", [NO, D, ODD_IN]); inp("w_out_odd", [NO, D, D])
            inp("hy_short", [NO, 3072, 3]); inp("sc_conv", [NO, 1024, 3])
            inp("hy_w1", [NO, 33, 64]); inp("hy_b1", [NO, 64, 1]); inp("hy_w2", [NO, 64, 64]); inp("hy_b2", [NO, 64, 1])
            inp("hy_w3", [NO, 64, 64]); inp("hy_b3", [NO, 64, 1]); inp("hy_w4", [NO, 64, 4096]); inp("hy_bias", [NO, 2, 1024, 1])
            inp("hy_embT", [2, 2, 33, S]); inp("hy_t", [2, 2, S]); inp("hy_delta", [1024, 1])
            inp("antiid", [128, 128])
        k.I = I
        out = P.dram("out", [NB * S, D], F32, kind="ExternalOutput")
        k.X = P.dram("X", [NT, D], F32)
        k.MOD = P.dram("MOD", [NB + 1, 6 * D], F32)
        k.MG = P.dram("MG", [NB + 1, 2 * D], F32)
        k.PTM = P.dram("PTM", [NT, 4096], BF16)
        k.PFM = P.dram("PFM", [6144, NT], BF16)
        k.PFC = P.dram("PFC", [3072, NT], BF16)
        k.YTM = P.dram("YTM", [NT, D], BF16)
        k.YFM = P.dram("YFM", [D, NT], BF16)
        k.H2 = P.dram("H2", [NT, D], BF16)
        k.XS = P.dram("XS", [cfg.cap, D], BF16)
        k.YS = P.dram("YS", [cfg.cap, D], BF16)
        k.OF = P.dram("OF", [NT, 1024], F32)
        k.HG = P.dram("HG", [2, 1024, 2 * S], BF16)
        k.HGC = P.dram("HGC", [2, 1024, 2 * LC], BF16)
        k.identf = P.sbuf("identf", [128, 128], F32)
        k.identb = P.sbuf("identb", [128, 128], BF16)
        P.dma("sp", k.identf[:], I["ident"][:], k.identf, I["ident"])
        P.op("dve", lambda e: e.tensor_copy(k.identb[:], k.identf[:]), [k.identf], [k.identb])
        for b in range(NB):
            P.dma("sp", k.X.ap[b * cfg.TB:b * cfg.TB + S, :], I["x"].ap[b * S:(b + 1) * S, :], k.X, I["x"])
            P.dma("sp", k.X.ap[b * cfg.TB + S:(b + 1) * cfg.TB, :], I["ctx"].ap[b * LC:(b + 1) * LC, :], k.X, I["ctx"])
        ie = io = 0
        for l, (kind, ctx_out) in enumerate(cfg.layers):
            with_ctx = ctx_out or kind == "zero"
            phase_mod(k, l)
            if kind == "even":
                phase_win_even(k, l, ie)
                k.dump("PFM%d" % l, k.PFM); k.dump("PTM%d" % l, k.PTM)
                mixer_even(k, l, ie, ctx_out)
                k.dump("YTM%d" % l, k.YTM)
                phase_wout(k, l, I["w_out_even"].ap[ie], "tm", with_ctx)
                k.dump("X%d" % l, k.X)
                ie += 1
            elif kind == "odd":
                phase_win_odd(k, l, io, with_ctx)
                k.dump("PFM%d" % l, k.PFM)
                mixer_odd(k, l, io, with_ctx)
                k.dump("PFC%d" % l, k.PFC); k.dump("YFM%d" % l, k.YFM); k.dump("HG%d" % l, k.HG); k.dump("HGC%d" % l, k.HGC)
                phase_wout(k, l, I["w_out_odd"].ap[io], "fm", with_ctx)
                k.dump("X%d" % l, k.X)
                io += 1
            phase_moe(k, l, with_ctx)
        phase_final(k, out)
        P.barrier()
        k.ninst = dict(P.ninst)
    return nc, k


def phase_mod(k, l):
    P, cfg, I = k.P, k.cfg, k.I
    R = cfg.NB + 1
    with P.scope():
        cv = P.sbuf("cv", [128, R, KC], F32)
        sv = P.sbuf("sv", [128, KC, R], F32)
        P.dma("sp", cv[:], I["cvec"].ap.rearrange("r (k p) -> p r k", p=128), cv, I["cvec"], allow_slow_non_contiguous=True)
        for r in range(R):
            P.op("act", lambda e: e.activation(sv[:, :, r], cv[:, r, :], AF.Silu), [cv], [sv])
        wt = [P.sbuf("wmod%d" % i, [128, KC, 512], F32) for i in range(2)]
        bmt = [P.sbuf("bm%d" % i, [R, 512], F32) for i in range(2)]
        ps = [P.psum("psmod%d" % i, [R, 512], F32) for i in range(2)]
        mo = P.sbuf("mo", [R, 6 * D], F32)
        for j in range(24):
            w = wt[j % 2]
            P.dma("sp", w[:], I["w_mod"].ap[l, :, j * 512:(j + 1) * 512].rearrange("(k p) n -> p k n", p=128), w, I["w_mod"])
            pp = ps[j % 2]
            bm = bmt[j % 2]
            P.dma("sp", bm[:], bc_rows(I["b_mod"].ap[l:l + 1, j * 512:(j + 1) * 512], R), bm, I["b_mod"])
            for kk in range(KC):
                P.op("pe", lambda e: e.matmul(pp[:], sv[:, kk, :], w[:, kk, :], start=(kk == 0), stop=(kk == KC - 1)), [sv, w], [pp], pe_acc=True)
            P.op("dve", lambda e: e.tensor_tensor(mo[:, j * 512:(j + 1) * 512], pp[:], bm[:], ALU.add), [pp, bm], [mo])
        P.dma("sp", k.MOD.ap[:], mo[:], k.MOD, mo)
        nm = P.sbuf("nm", [R, 2 * D], F32)
        P.dma("sp", nm[:, 0:D], bc_rows(I["norm_mix"].ap[l:l + 1, :], R), nm, I["norm_mix"])
        P.dma("sp", nm[:, D:2 * D], bc_rows(I["norm_ffn"].ap[l:l + 1, :], R), nm, I["norm_ffn"])
        mg = P.sbuf("mg", [R, 2 * D], F32)
        P.op("dve", lambda e: e.scalar_tensor_tensor(mg[:, 0:D], mo[:, D:2 * D], 1.0, nm[:, 0:D], ALU.add, ALU.mult), [mo, nm], [mg])
        P.op("dve", lambda e: e.scalar_tensor_tensor(mg[:, D:2 * D], mo[:, 4 * D:5 * D], 1.0, nm[:, D:2 * D], ALU.add, ALU.mult), [mo, nm], [mg])
        P.dma("sp", k.MG.ap[:], mg[:], k.MG, mg)


class NormProvider:
    def __init__(self, k, which, on_tile=None, transpose=True):
        P = k.P
        self.k, self.which, self.on_tile = k, which, on_tile
        self.transpose = transpose
        self.xt = [P.sbuf("np_x%d" % i, [128, D], F32) for i in range(2)]
        self.sq = P.sbuf("np_sq", [128, D], BF16)
        self.st = [P.sbuf("np_st%d" % i, [128, 4], F32) for i in range(2)]
        self.hf = [P.sbuf("np_hf%d" % i, [128, D], F32) for i in range(2)]
        self.hb = [P.sbuf("np_hb%d" % i, [128, D], BF16) for i in range(2)]
        self.G = P.sbuf("np_G", [128, D], F32)
        self.SH = P.sbuf("np_SH", [128, D], F32)
        if transpose:
            self.aT = [P.sbuf("np_aT%d" % i, [128, KC, 512], BF16) for i in range(2)]
            self.pT = [P.psum("np_pT%d" % i, [128, 8, 128], BF16) for i in range(2)]
        self.cur = None
        self.n = 0
        self.ng = 0

    def get(self, grp):
        k, P = self.k, self.k.P
        tok0, ntok, b, is_ctx = grp
        r = k.cfg.NB if is_ctx else b
        if self.cur != r:
            self.cur = r
            so = 0 if self.which == 1 else 3 * D
            go = 0 if self.which == 1 else D
            P.dma("sp", self.G[:], bc_rows(k.MG.ap[r:r + 1, go:go + D]), self.G, k.MG)
            P.dma("sp", self.SH[:], bc_rows(k.MOD.ap[r:r + 1, so:so + D]), self.SH, k.MOD)
        aT = self.aT[self.ng % 2] if self.transpose else None
        self.ng += 1
        for sub in range(ntok // 128):
            i = self.n % 2
            self.n += 1
            xt, st, hf, hb = self.xt[i], self.st[i], self.hf[i], self.hb[i]
            t0 = tok0 + sub * 128
            P.dma("sp", xt[:], k.X.ap[t0:t0 + 128, :], xt, k.X)
            P.op("act", lambda e: e.activation(self.sq[:], xt[:], AF.Square, accum_out=st[:, 0:1]), [xt], [self.sq, st])
            P.op("dve", lambda e: e.tensor_scalar(st[:, 1:2], st[:, 0:1], 1.0 / D, EPS, ALU.mult, ALU.add), [st], [st])
            P.op("act", lambda e: e.activation(st[:, 2:3], st[:, 1:2], AF.Sqrt), [st], [st])
            P.op("dve", lambda e: e.reciprocal(st[:, 3:4], st[:, 2:3]), [st], [st])
            P.op("dve", lambda e: e.scalar_tensor_tensor(hf[:], xt[:], st[:, 3:4], self.G[:], ALU.mult, ALU.mult), [xt, st, self.G], [hf])
            P.op("pool", lambda e: e.tensor_tensor(hf[:], hf[:], self.SH[:], ALU.add), [hf, self.SH], [hf])
            P.op("act", lambda e: e.copy(hb[:], hf[:]), [hf], [hb])
            if self.on_tile is not None:
                self.on_tile(t0, hf, hb)
            if not self.transpose:
                continue
            for half in range(2):
                pT = self.pT[half]
                for kk in range(8):
                    kq = half * 8 + kk
                    P.op("pe", lambda e: e.transpose(pT[:, kk, :], hb[:, kq * 128:(kq + 1) * 128], k.identb[:]), [hb, k.identb], [pT], pe_acc=True)
                eng = "dve" if half == 0 else "act"
                if eng == "dve":
                    P.op("dve", lambda e: e.tensor_copy(aT[:, half * 8:(half + 1) * 8, sub * 128:(sub + 1) * 128], pT[:]), [pT], [aT])
                else:
                    P.op("act", lambda e: e.copy(aT[:, half * 8:(half + 1) * 8, sub * 128:(sub + 1) * 128], pT[:]), [pT], [aT])
        return aT


class TmProvider:
    def __init__(self, k, src):
        P = k.P
        self.k, self.src = k, src
        self.yb = [P.sbuf("tp_y%d" % i, [128, D], BF16) for i in range(2)]
        self.aT = [P.sbuf("tp_aT%d" % i, [128, KC, 512], BF16) for i in range(2)]
        self.pT = [P.psum("tp_pT%d" % i, [128, 8, 128], BF16) for i in range(2)]
        self.n = 0
        self.ng = 0

    def get(self, grp):
        k, P = self.k, self.k.P
        tok0, ntok, b, is_ctx = grp
        aT = self.aT[self.ng % 2]
        self.ng += 1
        for sub in range(ntok // 128):
            yb = self.yb[self.n % 2]
            self.n += 1
            t0 = tok0 + sub * 128
            P.dma("sp", yb[:], self.src.ap[t0:t0 + 128, :], yb, self.src)
            for half in range(2):
                pT = self.pT[half]
                for kk in range(8):
                    kq = half * 8 + kk
                    P.op("pe", lambda e: e.transpose(pT[:, kk, :], yb[:, kq * 128:(kq + 1) * 128], k.identb[:]), [yb, k.identb], [pT], pe_acc=True)
                if half == 0:
                    P.op("dve", lambda e: e.tensor_copy(aT[:, 0:8, sub * 128:(sub + 1) * 128], pT[:]), [pT], [aT])
                else:
                    P.op("act", lambda e: e.copy(aT[:, 8:16, sub * 128:(sub + 1) * 128], pT[:]), [pT], [aT])
        return aT


class FmProvider:
    def __init__(self, k, src):
        P = k.P
        self.k, self.src = k, src
        self.aT = [P.sbuf("fp_aT%d" % i, [128, KC, 512], BF16) for i in range(2)]
        self.ng = 0

    def get(self, grp):
        k, P = self.k, self.k.P
        tok0, ntok, b, is_ctx = grp
        aT = self.aT[self.ng % 2]
        self.ng += 1
        P.dma("sp", aT[:, :, 0:ntok], self.src.ap[:, tok0:tok0 + ntok].rearrange("(k p) t -> p k t", p=128), aT, self.src)
        return aT


def gemm(k, W, passes, groups, provider, epilogue, wcols=2048):
    P = k.P
    wb = P.sbuf("g_wb", [128, KC, wcols], BF16)
    ps = [P.psum("g_ps%d" % i, [128, 512], F32) for i in range(4)]
    npz = 0
    for chunks in passes:
        off = 0
        offs = []
        for (kind, c0, ncols, tag) in chunks:
            P.dma("pool", wb[:, :, off:off + ncols], W[:, c0:c0 + ncols].rearrange("(k p) n -> p k n", p=128), wb, k.I["ident"])
            offs.append(off)
            off += ncols
        for grp in groups:
            tok0, ntok, b, is_ctx = grp
            aT = provider.get(grp)
            for ci, (kind, c0, ncols, tag) in enumerate(chunks):
                o = offs[ci]
                if kind != "fm":
                    continue
                pp = ps[npz % 4]
                npz += 1
                for kk in range(KC):
                    P.op("pe", lambda e: e.matmul(pp[0:ncols, 0:ntok], wb[:, kk, o:o + ncols], aT[:, kk, 0:ntok], start=(kk == 0), stop=(kk == KC - 1)), [aT, wb], [pp], pe_acc=True)
                epilogue(grp, None, (kind, c0, ncols, tag), pp)
            for sub in range(ntok // 128):
                for ci, (kind, c0, ncols, tag) in enumerate(chunks):
                    o = offs[ci]
                    if kind != "tm":
                        continue
                    pp = ps[npz % 4]
                    npz += 1
                    for kk in range(KC):
                        P.op("pe", lambda e: e.matmul(pp[:, 0:ncols], aT[:, kk, sub * 128:(sub + 1) * 128], wb[:, kk, o:o + ncols], start=(kk == 0), stop=(kk == KC - 1)), [aT, wb], [pp], pe_acc=True)
                    epilogue(grp, sub, (kind, c0, ncols, tag), pp)


class Evac:
    def __init__(self, k, n=4):
        P = k.P
        self.k = k
        self.st = [P.sbuf("ev_st%d" % i, [128, 512], BF16) for i in range(n)]
        self.i = 0

    def put(self, pp_buf, pp_ap, dst_buf, dst_ap, scale=None):
        P = self.k.P
        st = self.st[self.i % len(self.st)]
        use_act = (self.i % 2 == 0)
        self.i += 1
        shp = pp_ap.shape
        sap = st[0:shp[0], 0:shp[1]]
        if scale is not None:
            P.op("act", lambda e: e.activation(sap, pp_ap, AF.Copy, scale=float(scale)), [pp_buf], [st])
        elif use_act:
            P.op("act", lambda e: e.copy(sap, pp_ap), [pp_buf], [st])
        else:
            P.op("dve", lambda e: e.tensor_copy(sap, pp_ap), [pp_buf], [st])
        P.dma("pool", dst_ap, sap, dst_buf, st)


def phase_wout(k, l, W, src_kind, with_ctx):
    P, cfg = k.P, k.cfg
    with P.scope():
        prov = TmProvider(k, k.YTM) if src_kind == "tm" else FmProvider(k, k.YFM)
        xt = [P.sbuf("wo_x%d" % i, [128, D], F32) for i in range(2)]
        tmp = [P.sbuf("wo_t%d" % i, [128, 512], F32) for i in range(2)]
        gate = P.sbuf("wo_gate", [128, D], F32)
        state = {"cur": None, "n": 0, "t": 0}

        def epi(grp, sub, chunk, pp):
            tok0, ntok, b, is_ctx = grp
            kind, c0, ncols, tag = chunk
            r = cfg.NB if is_ctx else b
            if state["cur"] != r:
                state["cur"] = r
                P.dma("sp", gate[:], bc_rows(k.MOD.ap[r:r + 1, 2 * D:3 * D]), gate, k.MOD)
            t0 = tok0 + sub * 128
            if c0 == 0:
                state["n"] += 1
                x = xt[state["n"] % 2]
                P.dma("sp", x[:], k.X.ap[t0:t0 + 128, :], x, k.X)
            x = xt[state["n"] % 2]
            tt = tmp[state["t"] % 2]
            state["t"] += 1
            P.op("dve", lambda e: e.tensor_tensor(tt[:], pp[:, 0:512], gate[:, c0:c0 + 512], ALU.mult), [pp, gate], [tt])
            P.op("pool", lambda e: e.tensor_tensor(x[:, c0:c0 + 512], x[:, c0:c0 + 512], tt[:], ALU.add), [x, tt], [x])
            if c0 == D - 512:
                P.dma("pool", k.X.ap[t0:t0 + 128, :], x[:], k.X, x)
        passes = [[("tm", c, 512, None) for c in range(0, D, 512)]]
        gemm(k, W, passes, cfg.groups(with_ctx), prov, epi)


def phase_final(k, out):
    P, cfg = k.P, k.cfg
    with P.scope():
        xt = [P.sbuf("fin_x%d" % i, [128, D], F32) for i in range(2)]
        sq = P.sbuf("fin_sq", [128, D], BF16)
        st = [P.sbuf("fin_st%d" % i, [128, 4], F32) for i in range(2)]
        ot = [P.sbuf("fin_o%d" % i, [128, D], F32) for i in range(2)]
        nf = P.sbuf("fin_nf", [128, D], F32)
        P.dma("sp", nf[:], bc_rows(k.I["norm_final"].ap[0:1, :]), nf, k.I["norm_final"])
        n = 0
        for b in range(cfg.NB):
            for t in range(cfg.S // 128):
                i = n % 2
                n += 1
                t0 = b * cfg.TB + t * 128
                x, s, o = xt[i], st[i], ot[i]
                P.dma("sp", x[:], k.X.ap[t0:t0 + 128, :], x, k.X)
                P.op("act", lambda e: e.activation(sq[:], x[:], AF.Square, accum_out=s[:, 0:1]), [x], [sq, s])
                P.op("dve", lambda e: e.tensor_scalar(s[:, 1:2], s[:, 0:1], 1.0 / D, EPS, ALU.mult, ALU.add), [s], [s])
                P.op("act", lambda e: e.activation(s[:, 2:3], s[:, 1:2], AF.Sqrt), [s], [s])
                P.op("dve", lambda e: e.reciprocal(s[:, 3:4], s[:, 2:3]), [s], [s])
                P.op("dve", lambda e: e.scalar_tensor_tensor(o[:], x[:], s[:, 3:4], nf[:], ALU.mult, ALU.mult), [x, s, nf], [o])
                P.dma("pool", out.ap[b * cfg.S + t * 128:b * cfg.S + (t + 1) * 128, :], o[:], out, o)

def _win(k, W, passes, groups, pfm_row, ptm_col, scale_fn):
    P = k.P
    with P.scope():
        prov = NormProvider(k, 1)
        ev = Evac(k)

        def epi(grp, sub, chunk, pp):
            tok0, ntok, b, is_ctx = grp
            kind, c0, ncols, tag = chunk
            if kind == "fm":
                r0 = pfm_row(c0)
                ev.put(pp, pp[0:ncols, 0:ntok], k.PFM, k.PFM.ap[r0:r0 + ncols, tok0:tok0 + ntok], scale_fn(c0))
            else:
                t0 = tok0 + sub * 128
                pc = ptm_col(c0)
                ev.put(pp, pp[:, 0:ncols], k.PTM, k.PTM.ap[t0:t0 + 128, pc:pc + ncols])
        gemm(k, W, passes, groups, prov, epi, wcols=2080)


def phase_win_even(k, l, ie):
    W = k.I["w_in_even"].ap[ie]
    pa = [("fm", c, 128, None) for c in range(0, 2048, 128)]
    pb = [("tm", c, 512, None) for c in range(2048, 4096, 512)]
    pc = [("tm", c, 512, None) for c in range(4096, 6144, 512)] + [("fm", 6144, 32, None)]
    _win(k, W, [pa, pb, pc], k.cfg.groups(True),
         lambda c: c if c < 2048 else 2048, lambda c: c - 2048,
         lambda c: (128.0 ** -0.5) if c < 1024 else None)


def phase_win_odd(k, l, io, with_ctx):
    W = k.I["w_in_odd"].ap[io]
    passes = [[("fm", c, 128, None) for c in range(p0, p0 + 2048, 128)] for p0 in (0, 2048, 4096)]
    _win(k, W, passes, k.cfg.groups(with_ctx), lambda c: c, lambda c: c, lambda c: None)

def phase_moe(k, l, with_ctx):
    P, cfg, I = k.P, k.cfg, k.I
    E, NG, EPG = cfg.E, cfg.NG, cfg.EPG
    NR = NG + E
    groups = cfg.groups(with_ctx)
    tiles = []
    for (tok0, ntok, b, is_ctx) in groups:
        for sub in range(ntok // 128):
            tiles.append((tok0 + sub * 128, b, is_ctx))
    ntl = len(tiles)
    nblk = cfg.nblk
    with P.scope():
        GATE = P.sbuf("m_GATE", [128, ntl, 2], F32)
        SLOT = P.sbuf("m_SLOT", [128, ntl, 2], I32)
        IDXW = P.sbuf("m_IDXW", [128, nblk], I32)
        Ccnt = P.sbuf("m_C", [128, E], F32)
        P.op("dve", lambda e: e.memset(Ccnt[:], 0.0), [], [Ccnt])
        routing = contextlib.ExitStack()
        routing.enter_context(P.scope())
        OH = P.sbuf("m_OH", [128, ntl, 2, E], BF16)
        RANK = P.sbuf("m_RANK", [128, ntl, E], F32)
        with P.scope():
            wr = P.sbuf("m_wr", [128, KC, NR], F32)
            P.dma("sp", wr[:], I["w_router"].ap[l].rearrange("(k p) n -> p k n", p=128), wr, I["w_router"])
            br = P.sbuf("m_br", [128, NR], F32)
            P.dma("sp", br[:], bc_rows(I["b_router"].ap[l:l + 1, :]), br, I["b_router"])
            lstr = P.sbuf("m_lstr", [128, 128], F32)
            lstrb = P.sbuf("m_lstrb", [128, 128], BF16)
            onesb = P.sbuf("m_onesb", [128, 128], BF16)
            P.dma("sp", lstr[:], I["lstrict"].ap[:], lstr, I["lstrict"])
            P.op("dve", lambda e: e.tensor_copy(lstrb[:], lstr[:]), [lstr], [lstrb])
            P.op("dve", lambda e: e.memset(onesb[:], 1.0), [], [onesb])
            hTf = P.sbuf("m_hTf", [128, KC, 128], F32)
            pTf = [P.psum("m_pTf%d" % i, [128, 4, 128], F32) for i in range(2)]
            plg = P.psum("m_plg", [128, 64], F32)
            prk = P.psum("m_prk", [128, 2, 64], F32)
            sm = [P.sbuf("m_sm%d" % i, [128, 256], F32) for i in range(2)]
            st = {"i": 0, "ti": 0}

            def on_tile(t0, hf, hb):
                ti = st["ti"]
                st["ti"] += 1
                s = sm[ti % 2]
                P.dma("pool", k.H2.ap[t0:t0 + 128, :], hb[:], k.H2, hb)
                for q in range(4):
                    pT = pTf[q % 2]
                    for kk in range(4):
                        kq = q * 4 + kk
                        P.op("pe", lambda e: e.transpose(pT[:, kk, :], hf[:, kq * 128:(kq + 1) * 128], k.identf[:]), [hf, k.identf], [pT], pe_acc=True)
                    if q % 2 == 0:
                        P.op("dve", lambda e: e.tensor_copy(hTf[:, q * 4:(q + 1) * 4, :], pT[:]), [pT], [hTf])
                    else:
                        P.op("act", lambda e: e.copy(hTf[:, q * 4:(q + 1) * 4, :], pT[:]), [pT], [hTf])
                for kk in range(KC):
                    P.op("pe", lambda e: e.matmul(plg[:, 0:NR], hTf[:, kk, :], wr[:, kk, :], start=(kk == 0), stop=(kk == KC - 1)), [hTf, wr], [plg], pe_acc=True)
                lg = s[:, 0:NR]
                P.op("dve", lambda e: e.tensor_tensor(lg, plg[:, 0:NR], br[:], ALU.add), [plg, br], [s])
                c = 64
                P.op("dve", lambda e: e.tensor_reduce(s[:, c:c + 1], s[:, 0:NG], AX.X, ALU.max), [s], [s])
                P.op("dve", lambda e: e.tensor_scalar(s[:, c + 1:c + 2], s[:, c:c + 1], -1.0, None, ALU.mult), [s], [s])
                P.op("dve", lambda e: e.tensor_scalar(s[:, 72:72 + NG], s[:, 0:NG], s[:, c:c + 1], None, ALU.is_equal), [s], [s])
                P.op("act", lambda e: e.activation(s[:, 80:80 + NG], s[:, 0:NG], AF.Exp, bias=s[:, c + 1:c + 2], scale=1.0, accum_out=s[:, c + 2:c + 3]), [s], [s])
                P.op("dve", lambda e: e.reciprocal(s[:, c + 3:c + 4], s[:, c + 2:c + 3]), [s], [s])
                P.op("dve", lambda e: e.tensor_scalar(s[:, 88:88 + NG], s[:, 72:72 + NG], -1.0, 1e30, ALU.add, ALU.mult), [s], [s])
                lem = s[:, 96:96 + E]
                P.op("dve", lambda e: e.tensor_tensor(lem.rearrange("p (g j) -> p g j", g=NG), s[:, NG:NG + E].rearrange("p (g j) -> p g j", g=NG),
                                                      s[:, 88:88 + NG].unsqueeze(2).to_broadcast([128, NG, EPG]), ALU.add), [s], [s])
                P.op("dve", lambda e: e.max(s[:, 136:144], lem), [s], [s])
                oh = OH[:, ti, :, :]
                P.op("dve", lambda e: e.tensor_scalar(OH[:, ti, 0, :], lem, s[:, 136:137], None, ALU.is_equal), [s], [OH])
                P.op("dve", lambda e: e.tensor_scalar(OH[:, ti, 1, :], lem, s[:, 137:138], None, ALU.is_equal), [s], [OH])
                P.op("dve", lambda e: e.tensor_tensor(s[:, c + 4:c + 5], s[:, 137:138], s[:, 136:137], ALU.subtract), [s], [s])
                P.op("act", lambda e: e.activation(s[:, c + 5:c + 6], s[:, c + 4:c + 5], AF.Exp), [s], [s])
                P.op("dve", lambda e: e.tensor_scalar(s[:, c + 5:c + 6], s[:, c + 5:c + 6], 1.0, None, ALU.add), [s], [s])
                P.op("dve", lambda e: e.reciprocal(s[:, c + 6:c + 7], s[:, c + 5:c + 6]), [s], [s])
                P.op("dve", lambda e: e.tensor_tensor(GATE[:, ti, 0:1], s[:, c + 6:c + 7], s[:, c + 3:c + 4], ALU.mult), [s], [GATE])
                P.op("dve", lambda e: e.tensor_tensor(GATE[:, ti, 1:2], s[:, c + 3:c + 4], GATE[:, ti, 0:1], ALU.subtract), [s, GATE], [GATE])
                A = sm_A[ti % 2]
                P.op("dve", lambda e: e.tensor_tensor(A[:], OH[:, ti, 0, :], OH[:, ti, 1, :], ALU.add), [OH], [A])
                P.op("pe", lambda e: e.matmul(prk[:, 0, 0:E], lstrb[:], A[:], start=True, stop=True), [lstrb, A], [prk], pe_acc=True)
                P.op("pe", lambda e: e.matmul(prk[:, 1, 0:E], onesb[:], A[:], start=True, stop=True), [onesb, A], [prk], pe_acc=True)
                P.op("dve", lambda e: e.tensor_tensor(RANK[:, ti, :], prk[:, 0, 0:E], Ccnt[:], ALU.add), [prk, Ccnt], [RANK])
                P.op("dve", lambda e: e.tensor_tensor(Ccnt[:], prk[:, 1, 0:E], Ccnt[:], ALU.add), [prk, Ccnt], [Ccnt])
            sm_A = [P.sbuf("m_A%d" % i, [128, E], BF16) for i in range(2)]
            prov = NormProvider(k, 2, on_tile=on_tile, transpose=False)
            for grp in groups:
                prov.get(grp)
        with P.scope():
            w = P.sbuf("m_w", [128, 8, E], F32)
            P.op("dve", lambda e: e.tensor_scalar(w[:, 0, :], Ccnt[:], 1.0 / 128.0, 63.5 / 128.0, ALU.mult, ALU.add), [Ccnt], [w])
            P.op("dve", lambda e: e.tensor_scalar(w[:, 5, :], w[:, 0, :], 8388608.0, None, ALU.add), [w], [w])
            P.op("dve", lambda e: e.tensor_scalar(w[:, 6, :], w[:, 5, :], -8388608.0, 128.0, ALU.add, ALU.mult), [w], [w])
            P.op("dve", lambda e: e.tensor_copy(w[:, 1, :], w[:, 6, :]), [w], [w])
            P.op("dve", lambda e: e.memset(w[:, 2, :], 1.0), [], [w])
            P.op("dve", lambda e: e.tensor_tensor_scan(w[:, 3, :], w[:, 2, :], w[:, 1, :], 0.0, ALU.mult, ALU.add), [w], [w])
            P.op("dve", lambda e: e.tensor_tensor(w[:, 4, :], w[:, 3, :], w[:, 1, :], ALU.subtract), [w], [w])
            tmp = P.sbuf("m_tmp", [128, 2, E], F32)
            sl = P.sbuf("m_sl", [128, ntl, 2], F32)
            for ti in range(ntl):
                P.op("dve", lambda e: e.tensor_tensor(tmp[:, 0, :], RANK[:, ti, :], w[:, 4, :], ALU.add), [RANK, w], [tmp])
                for j in range(2):
                    P.op("dve", lambda e: e.tensor_tensor(tmp[:, 1, :], tmp[:, 0, :], OH[:, ti, j, :], ALU.mult), [tmp, OH], [tmp])
                    P.op("dve", lambda e: e.tensor_reduce(sl[:, ti, j:j + 1], tmp[:, 1, :], AX.X, ALU.add), [tmp], [sl])
            P.op("dve", lambda e: e.tensor_copy(SLOT[:], sl[:]), [sl], [SLOT])
            bs = P.sbuf("m_bs", [128, nblk], F32)
            be = P.sbuf("m_be", [128, nblk], F32)
            be2 = P.sbuf("m_be2", [128, nblk], F32)
            ip = P.sbuf("m_ip", [128, 1], F32)
            P.dma("sp", bs[:], bc_rows(I["blkstart"].ap[0:1, :]), bs, I["blkstart"])
            P.dma("sp", ip[:], I["iota_p"].ap[:], ip, I["iota_p"])
            P.op("dve", lambda e: e.memset(be[:], 0.0), [], [be])
            for ex in range(E):
                P.op("dve", lambda e: e.scalar_tensor_tensor(be[:], bs[:], w[:, 3, ex:ex + 1], be[:], ALU.is_ge, ALU.add), [bs, w, be], [be])
            P.op("dve", lambda e: e.tensor_scalar(be[:], be[:], float(E - 1), None, ALU.min), [be], [be])
            P.op("dve", lambda e: e.memset(be2[:, 0:2], 1.0), [], [be2])
            P.op("dve", lambda e: e.tensor_tensor(be2[:, 2:nblk], be[:, 2:nblk], be[:, 0:nblk - 2], ALU.not_equal), [be], [be2])
            P.op("dve", lambda e: e.tensor_scalar(be[:], be[:], 128.0, ip[:, 0:1], ALU.mult, ALU.add), [be, ip], [be])
            P.op("dve", lambda e: e.tensor_scalar(be2[:], be2[:], -BIGIDX, BIGIDX + float(l * E * 128), ALU.mult, ALU.add), [be2], [be2])
            P.op("dve", lambda e: e.tensor_tensor(be[:], be[:], be2[:], ALU.add), [be, be2], [be])
            P.op("dve", lambda e: e.tensor_copy(IDXW[:], be[:]), [be], [IDXW])
            hb = [P.sbuf("m_hb%d" % i, [128, D], BF16) for i in range(3)]
            for ti, (t0, b, is_ctx) in enumerate(tiles):
                h = hb[ti % 3]
                P.dma("sp", h[:], k.H2.ap[t0:t0 + 128, :], h, k.H2)
                for j in range(2):
                    P.dma_raw("pool", lambda e: e.indirect_dma_start(out=k.XS.ap[:, :], out_offset=bass.IndirectOffsetOnAxis(ap=SLOT[:, ti, j:j + 1], axis=0), in_=h[:], in_offset=None),
                              k.XS, [h, SLOT])
        routing.close()
        with P.scope():
            w1bb = [P.sbuf("m_w1b%d" % i, [128, KC * 512], BF16) for i in range(2)]
            w3bb = [P.sbuf("m_w3b%d" % i, [128, KC * 512], BF16) for i in range(2)]
            w2bb = [P.sbuf("m_w2b%d" % i, [128, 4 * D], BF16) for i in range(2)]
            xs = [P.sbuf("m_xs%d" % i, [128, D], BF16) for i in range(2)]
            xT = [P.sbuf("m_xT%d" % i, [128, KC, 128], BF16) for i in range(2)]
            pT = [P.psum("m_pT%d" % i, [128, 8, 128], BF16) for i in range(2)]
            ph = [P.psum("m_ph%d" % i, [128, 512], F32) for i in range(2)]
            py = [P.psum("m_py%d" % i, [128, 512], F32) for i in range(2)]
            paT = P.psum("m_paT", [128, 4, 128], BF16)
            sil = [P.sbuf("m_sil%d" % i, [128, 512], F32) for i in range(2)]
            ab = [P.sbuf("m_ab%d" % i, [128, 512], BF16) for i in range(2)]
            aT = [P.sbuf("m_aT%d" % i, [128, 4, 128], BF16) for i in range(2)]
            yo = [P.sbuf("m_yo%d" % i, [128, D], BF16) for i in range(2)]
            nrow = E * 128
            breg = k.nc.gpsimd.alloc_register()
            k.nc.gpsimd.reg_mov(breg, (l + 1) * nrow - 1)
            for blk in range(nblk):
                i = blk % 2
                w1b, w3b, w2b = w1bb[i], w3bb[i], w2bb[i]
                for (dst, src) in ((w1b, I["w1h"]), (w3b, I["w3h"]), (w2b, I["w2h"])):
                    P.dma_raw("pool", lambda e: e.indirect_dma_start(out=dst[:], out_offset=None, in_=src.ap[:, :], in_offset=bass.IndirectOffsetOnAxis(ap=IDXW[:, blk:blk + 1], axis=0),
                                                                   bounds_check=breg, oob_is_err=False), dst, [IDXW, src])
                x = xs[i]
                P.dma("sp", x[:], k.XS.ap[blk * 128:(blk + 1) * 128, :], x, k.XS)
                for half in range(2):
                    for kk in range(8):
                        kq = half * 8 + kk
                        P.op("pe", lambda e: e.transpose(pT[half][:, kk, :], x[:, kq * 128:(kq + 1) * 128], k.identb[:]), [x, k.identb], [pT[half]], pe_acc=True)
                    if half == 0:
                        P.op("dve", lambda e: e.tensor_copy(xT[i][:, 0:8, :], pT[0][:]), [pT[0]], [xT[i]])
                    else:
                        P.op("act", lambda e: e.copy(xT[i][:, 8:16, :], pT[1][:]), [pT[1]], [xT[i]])
                for kk in range(KC):
                    P.op("pe", lambda e: e.matmul(ph[0][:], xT[i][:, kk, :], w1b[:, kk * 512:(kk + 1) * 512], start=(kk == 0), stop=(kk == KC - 1)), [xT[i], w1b], [ph[0]], pe_acc=True)
                for kk in range(KC):
                    P.op("pe", lambda e: e.matmul(ph[1][:], xT[i][:, kk, :], w3b[:, kk * 512:(kk + 1) * 512], start=(kk == 0), stop=(kk == KC - 1)), [xT[i], w3b], [ph[1]], pe_acc=True)
                P.op("act", lambda e: e.activation(sil[i][:], ph[0][:], AF.Silu), [ph[0]], [sil[i]])
                P.op("dve", lambda e: e.tensor_tensor(ab[i][:], ph[1][:], sil[i][:], ALU.mult), [ph[1], sil[i]], [ab[i]])
                for kk in range(4):
                    P.op("pe", lambda e: e.transpose(paT[:, kk, :], ab[i][:, kk * 128:(kk + 1) * 128], k.identb[:]), [ab[i], k.identb], [paT], pe_acc=True)
                P.op("dve", lambda e: e.tensor_copy(aT[i][:], paT[:]), [paT], [aT[i]])
                for cc in range(4):
                    pp = py[cc % 2]
                    for kk in range(4):
                        P.op("pe", lambda e: e.matmul(pp[:], aT[i][:, kk, :], w2b[:, kk * D + cc * 512:kk * D + (cc + 1) * 512], start=(kk == 0), stop=(kk == 3)), [aT[i], w2b], [pp], pe_acc=True)
                    if cc % 2 == 0:
                        P.op("act", lambda e: e.copy(yo[i][:, cc * 512:(cc + 1) * 512], pp[:]), [pp], [yo[i]])
                    else:
                        P.op("dve", lambda e: e.tensor_copy(yo[i][:, cc * 512:(cc + 1) * 512], pp[:]), [pp], [yo[i]])
                P.dma("sp", k.YS.ap[blk * 128:(blk + 1) * 128, :], yo[i][:], k.YS, yo[i])
        with P.scope():
            ya = [P.sbuf("m_ya%d" % i, [128, D], BF16) for i in range(2)]
            yb = [P.sbuf("m_yb%d" % i, [128, D], BF16) for i in range(2)]
            xt = [P.sbuf("m_x%d" % i, [128, D], F32) for i in range(2)]
            f = [P.sbuf("m_f%d" % i, [128, D], F32) for i in range(2)]
            gate = P.sbuf("m_gate", [128, D], F32)
            cur = None
            for ti, (t0, b, is_ctx) in enumerate(tiles):
                i = ti % 2
                r = cfg.NB if is_ctx else b
                if cur != r:
                    cur = r
                    P.dma("sp", gate[:], bc_rows(k.MOD.ap[r:r + 1, 5 * D:6 * D]), gate, k.MOD)
                for (dst, j) in ((ya[i], 0), (yb[i], 1)):
                    P.dma_raw("pool", lambda e: e.indirect_dma_start(out=dst[:], out_offset=None, in_=k.YS.ap[:, :], in_offset=bass.IndirectOffsetOnAxis(ap=SLOT[:, ti, j:j + 1], axis=0)),
                              dst, [SLOT, k.YS])
                P.dma("sp", xt[i][:], k.X.ap[t0:t0 + 128, :], xt[i], k.X)
                P.op("dve", lambda e: e.tensor_scalar(f[i][:], ya[i][:], GATE[:, ti, 0:1], None, ALU.mult), [ya[i], GATE], [f[i]])
                P.op("dve", lambda e: e.scalar_tensor_tensor(f[i][:], yb[i][:], GATE[:, ti, 1:2], f[i][:], ALU.mult, ALU.add), [yb[i], GATE, f[i]], [f[i]])
                P.op("pool", lambda e: e.tensor_tensor(f[i][:], f[i][:], gate[:], ALU.mult), [f[i], gate], [f[i]])
                P.op("pool", lambda e: e.tensor_tensor(xt[i][:], xt[i][:], f[i][:], ALU.add), [xt[i], f[i]], [xt[i]])
                P.dma("sp", k.X.ap[t0:t0 + 128, :], xt[i][:], k.X, xt[i])

TWO_PI = 2.0 * math.pi
MAGIC = 12582912.0


def hyena_filters(k, io, seg, L, HGbuf):
    P, I = k.P, k.I
    CH = min(512, L)
    nch = L // CH
    with P.scope():
        w1 = P.sbuf("hf_w1", [33, 64], F32); w2 = P.sbuf("hf_w2", [64, 64], F32); w3 = P.sbuf("hf_w3", [64, 64], F32)
        w4 = P.sbuf("hf_w4", [64, 4096], F32)
        bb = P.sbuf("hf_bb", [64, 3], F32)
        P.dma("sp", w1[:], I["hy_w1"].ap[io], w1, I["hy_w1"]); P.dma("sp", w2[:], I["hy_w2"].ap[io], w2, I["hy_w2"])
        P.dma("sp", w3[:], I["hy_w3"].ap[io], w3, I["hy_w3"]); P.dma("sp", w4[:], I["hy_w4"].ap[io], w4, I["hy_w4"])
        for j, nm in enumerate(("hy_b1", "hy_b2", "hy_b3")):
            P.dma("sp", bb[:, j:j + 1], I[nm].ap[io], bb, I[nm])
        a3 = P.sbuf("hf_a3", [64, 2, L], BF16)
        w4b = P.sbuf("hf_w4b", [64, 4096], BF16)
        P.op("act", lambda e: e.copy(w4b[:], w4[:]), [w4], [w4b])
        embc = [P.sbuf("hf_emb%d" % i, [33, 512], F32) for i in range(2)]
        act_ = [P.sbuf("hf_act%d" % i, [64, 512], F32) for i in range(2)]
        ps = [P.psum("hf_ps%d" % i, [128, 512], F32) for i in range(2)]
        tmp = [P.sbuf("hf_tmp%d" % i, [64, 2, 512], F32) for i in range(2)]
        n = 0
        ne = 0
        for d in range(2):
            for c in range(nch):
                em = embc[ne % 2]; ne += 1
                P.dma("sp", em[:, 0:CH], I["hy_embT"].ap[seg, d, :, c * CH:(c + 1) * CH], em, I["hy_embT"])
                for layer in range(3):
                    wl = (w1, w2, w3)[layer]
                    srcb = em if layer == 0 else act_[(layer - 1) % 2]
                    src = srcb[:, 0:CH]
                    pp = ps[n % 2]; t = tmp[n % 2]; n += 1
                    P.op("pe", lambda e: e.matmul(pp[0:64, 0:CH], wl[:], src, start=True, stop=True), [wl, srcb], [pp], pe_acc=True)
                    x = t[:, 0, 0:CH]; q = t[:, 1, 0:CH]
                    P.op("dve", lambda e: e.tensor_scalar(x, pp[0:64, 0:CH], bb[:, layer:layer + 1], None, ALU.add), [pp, bb], [t])
                    P.op("dve", lambda e: e.tensor_scalar(q, x, 1.0 / TWO_PI, MAGIC, ALU.mult, ALU.add), [t], [t])
                    P.op("dve", lambda e: e.tensor_scalar(q, q, -MAGIC, None, ALU.add), [t], [t])
                    P.op("dve", lambda e: e.scalar_tensor_tensor(x, q, -TWO_PI, x, ALU.mult, ALU.add), [t], [t])
                    P.op("dve", lambda e: e.tensor_scalar(x, x, math.pi, -math.pi, ALU.min, ALU.max), [t], [t])
                    if layer < 2:
                        dstb = act_[layer % 2]
                        P.op("act", lambda e: e.activation(dstb[:, 0:CH], x, AF.Sin), [t], [dstb])
                    else:
                        P.op("act", lambda e: e.activation(a3[:, d, c * CH:(c + 1) * CH], x, AF.Sin), [t], [a3])
        dl = P.sbuf("hf_dl", [128, 8], F32)
        P.dma("sp", dl[:], I["hy_delta"].ap.rearrange("(c p) o -> p (c o)", p=128), dl, I["hy_delta"], allow_slow_non_contiguous=True)
        ndl = P.sbuf("hf_ndl", [128, 8], F32)
        P.op("dve", lambda e: e.tensor_scalar(ndl[:], dl[:], -1.0, None, ALU.mult), [dl], [ndl])
        hb = P.sbuf("hf_bias", [128, 2, 8], F32)
        P.dma("sp", hb[:], I["hy_bias"].ap[io].rearrange("n (c p) o -> p n (c o)", p=128), hb, I["hy_bias"], allow_slow_non_contiguous=True)
        dec = [P.sbuf("hf_dec%d" % i, [128, 512], F32) for i in range(2)]
        tbc = [P.sbuf("hf_tbc%d" % i, [128, 512], F32) for i in range(2)]
        rw = P.sbuf("hf_row", [128, 2 * L], F32)
        rb = P.sbuf("hf_rowb", [128, 2 * L], BF16)
        part = P.sbuf("hf_part", [128, 8], F32)
        b0 = P.sbuf("hf_b0", [128, 4], F32)
        P.op("dve", lambda e: e.memset(b0[:], 0.0), [], [b0])
        for order in range(2):
            for cc in range(8):
                P.op("pool", lambda e: e.memset(rw[:, 2 * L - 1:2 * L], 0.0), [], [rw])
                for half in range(2):
                    fdir = half if order == 0 else 1 - half
                    tord = 1 if half == 0 else 0
                    col0 = order * 2048 + fdir * 1024 + cc * 128
                    for c in range(nch):
                        pp = ps[n % 2]; dc = dec[n % 2]; tb = tbc[n % 2]; n += 1
                        P.dma("sp", tb[:, 0:CH], bc_rows(I["hy_t"].ap[seg, tord:tord + 1, c * CH:(c + 1) * CH]), tb, I["hy_t"])
                        P.op("pe", lambda e: e.matmul(pp[:, 0:CH], w4b[:, col0:col0 + 128], a3[:, tord, c * CH:(c + 1) * CH], start=True, stop=True), [w4b, a3], [pp], pe_acc=True)
                        P.op("act", lambda e: e.activation(dc[:, 0:CH], tb[:, 0:CH], AF.Exp, scale=ndl[:, cc:cc + 1]), [tb, ndl], [dc])
                        if half == 0:
                            P.op("dve", lambda e: e.tensor_tensor(rw[:, c * CH:(c + 1) * CH], pp[:, 0:CH], dc[:, 0:CH], ALU.mult), [pp, dc], [rw])
                        elif c == 0:
                            P.op("dve", lambda e: e.tensor_tensor(b0[:, 0:1], pp[:, 0:1], dc[:, 0:1], ALU.mult), [pp, dc], [b0])
                            P.op("dve", lambda e: e.tensor_tensor(rw[:, L:L + CH - 1], pp[:, 1:CH], dc[:, 1:CH], ALU.mult), [pp, dc], [rw])
                        else:
                            P.op("dve", lambda e: e.tensor_tensor(rw[:, L + c * CH - 1:L + (c + 1) * CH - 1], pp[:, 0:CH], dc[:, 0:CH], ALU.mult), [pp, dc], [rw])
                P.op("dve", lambda e: e.tensor_reduce(part[:, 0:1], rw[:, 0:2 * L - 1], AX.X, ALU.add, apply_absolute_value=True), [rw], [part])
                P.op("dve", lambda e: e.tensor_reduce(part[:, 1:2], b0[:, 0:2], AX.X, ALU.add, apply_absolute_value=True), [b0], [part])
                P.op("dve", lambda e: e.tensor_tensor(part[:, 2:3], part[:, 0:1], part[:, 1:2], ALU.add), [part], [part])
                P.op("dve", lambda e: e.tensor_scalar(part[:, 2:3], part[:, 2:3], EPS, None, ALU.add), [part], [part])
                P.op("dve", lambda e: e.reciprocal(part[:, 3:4], part[:, 2:3]), [part], [part])
                P.op("dve", lambda e: e.tensor_tensor(rw[:, L - 1:L], rw[:, L - 1:L], b0[:, 0:1], ALU.add), [rw, b0], [rw])
                P.op("dve", lambda e: e.tensor_scalar(rb[:], rw[:], part[:, 3:4], None, ALU.mult), [rw, part], [rb])
                P.op("dve", lambda e: e.scalar_tensor_tensor(rb[:, L - 1:L], rw[:, L - 1:L], part[:, 3:4], hb[:, order, cc:cc + 1], ALU.mult, ALU.add), [rw, part, hb], [rb])
                P.dma("pool", HGbuf.ap[order, cc * 128:(cc + 1) * 128, :], rb[:], HGbuf, rb)


def short_conv_fm(k, io, with_ctx):
    P, cfg, I = k.P, k.cfg, k.I
    PC = 2048
    segs = []
    for b in range(cfg.NB):
        segs.append((b * cfg.TB, cfg.S))
        if with_ctx:
            segs.append((b * cfg.TB + cfg.S, cfg.LC))
    pieces = []
    for (t0, L) in segs:
        for p0 in range(0, L, PC):
            pl = min(PC, L - p0)
            pieces.append((t0, L, p0, pl))
    with P.scope():
        hs = P.sbuf("sc_hs", [128, 24, 3], F32)
        P.dma("sp", hs[:], I["hy_short"].ap[io].rearrange("(c p) j -> p c j", p=128), hs, I["hy_short"])
        scw = P.sbuf("sc_w", [128, 8, 3], F32)
        P.dma("sp", scw[:], I["sc_conv"].ap[io].rearrange("(c p) j -> p c j", p=128), scw, I["sc_conv"])
        xin = [P.sbuf("sc_x%d" % i, [128, PC + 2], BF16) for i in range(3)]
        acc = [P.sbuf("sc_a%d" % i, [128, PC], F32) for i in range(2)]
        ob = [P.sbuf("sc_o%d" % i, [128, PC], BF16) for i in range(2)]
        bg = [P.sbuf("sc_b%d" % i, [128, PC], BF16) for i in range(2)]
        cx = [P.sbuf("sc_cx%d" % i, [128, PC + 2], F32) for i in range(2)]
        n = 0

        def conv3(x, xb, w, ci, pl, a, ab):
            P.op("dve", lambda e: e.tensor_scalar(a[:, 0:pl], x[:, 1:pl + 1], w[:, ci, 1:2], None, ALU.mult), [xb, w], [ab])
            P.op("dve", lambda e: e.scalar_tensor_tensor(a[:, 0:pl], x[:, 0:pl], w[:, ci, 0:1], a[:, 0:pl], ALU.mult, ALU.add), [xb, w, ab], [ab])
            P.op("dve", lambda e: e.scalar_tensor_tensor(a[:, 0:pl], x[:, 2:pl + 2], w[:, ci, 2:3], a[:, 0:pl], ALU.mult, ALU.add), [xb, w, ab], [ab])

        def load_halo(xt, row0, t0, L, p0, pl):
            lo = max(p0 - 1, 0); hi = min(p0 + pl + 1, L)
            if p0 == 0:
                P.op("pool", lambda e: e.memset(xt[:, 0:1], 0.0), [], [xt])
            if p0 + pl == L:
                P.op("pool", lambda e: e.memset(xt[:, pl + 1:pl + 2], 0.0), [], [xt])
            P.dma("sp", xt[:, lo - p0 + 1:hi - p0 + 1], k.PFM.ap[row0:row0 + 128, t0 + lo:t0 + hi], xt, k.PFM)
        for ci in range(24):
            for (t0, L, p0, pl) in pieces:
                xt = xin[n % 3]; a = acc[n % 2]; o = ob[n % 2]; n += 1
                load_halo(xt, ci * 128, t0, L, p0, pl)
                conv3(xt, xt, hs, ci, pl, a, a)
                P.op("act", lambda e: e.copy(o[:, 0:pl], a[:, 0:pl]), [a], [o])
                P.dma("pool", k.PFC.ap[ci * 128:(ci + 1) * 128, t0 + p0:t0 + p0 + pl], o[:, 0:pl], k.PFC, o)
        for ci in range(8):
            for (t0, L, p0, pl) in pieces:
                xc = xin[n % 3]; xx = xin[(n + 1) % 3]; a = acc[n % 2]; o = ob[n % 2]; bgt = bg[n % 2]; cxt = cx[n % 2]; n += 2
                load_halo(xc, 3072 + 1024 + ci * 128, t0, L, p0, pl)
                load_halo(xx, 3072 + 2048 + ci * 128, t0, L, p0, pl)
                P.dma("sp", bgt[:, 0:pl], k.PFM.ap[3072 + ci * 128:3072 + (ci + 1) * 128, t0 + p0:t0 + p0 + pl], bgt, k.PFM)
                P.op("pool", lambda e: e.tensor_tensor(cxt[:, 0:pl + 2], xc[:, 0:pl + 2], xx[:, 0:pl + 2], ALU.mult), [xc, xx], [cxt])
                conv3(cxt, cxt, scw, ci, pl, a, a)
                P.op("dve", lambda e: e.tensor_tensor(o[:, 0:pl], a[:, 0:pl], bgt[:, 0:pl], ALU.mult), [a, bgt], [o])
                P.dma("pool", k.YFM.ap[1024 + ci * 128:1024 + (ci + 1) * 128, t0 + p0:t0 + p0 + pl], o[:, 0:pl], k.YFM, o)


def hyena_conv(k, io, seg_ctx, L, HGbuf):
    P, cfg, I = k.P, k.cfg, k.I
    NB = cfg.NB
    n2 = L // 128
    NC = n2 * NB
    dmax = n2 - 1
    TW = 128 * (2 * n2 - 1)
    QC = 32
    tokbase = [b * cfg.TB + (cfg.S if seg_ctx else 0) for b in range(NB)]
    with P.scope():
        anti = P.sbuf("hc_antif", [128, 128], F32); antib = P.sbuf("hc_antib", [128, 128], BF16)
        P.dma("sp", anti[:], I["antiid"].ap[:], anti, I["antiid"])
        P.op("dve", lambda e: e.tensor_copy(antib[:], anti[:]), [anti], [antib])
        Vt = P.sbuf("hc_Vt", [128, QC, NC], BF16)
        G1t = P.sbuf("hc_G1t", [128, QC, NC], BF16)
        G2t = P.sbuf("hc_G2t", [128, QC, NC], BF16)
        Y2t = P.sbuf("hc_Y2t", [128, NC, QC], BF16)
        TT = [P.sbuf("hc_T%d" % i, [128, TW], BF16) for i in range(2)]
        z1 = [P.sbuf("hc_z1%d" % i, [128, NC], BF16) for i in range(2)]
        fm = [P.sbuf("hc_fm%d" % i, [QC, 1024], BF16) for i in range(3)]
        ptr = [P.psum("hc_ptr%d" % i, [128, 4, QC], BF16) for i in range(2)]
        py = [P.psum("hc_py%d" % i, [128, 512], F32) for i in range(2)]
        pob = [P.psum("hc_pob%d" % i, [QC, 512], BF16) for i in range(2)]
        ost = [P.sbuf("hc_ost%d" % i, [QC, 512], BF16) for i in range(2)]
        nf = nt = ntt = nz = no = 0
        PL = min(1024, L)
        for q in range(1024 // QC):
            c0 = q * QC
            for (dst, rbase, idm) in ((Vt, 0, k.identb), (G1t, 1024, antib), (G2t, 2048, k.identb)):
                for b in range(NB):
                    for p0 in range(0, L, PL):
                        f = fm[nf % 3]; nf += 1
                        P.dma("sp", f[:, 0:PL], k.PFC.ap[rbase + c0:rbase + c0 + QC, tokbase[b] + p0:tokbase[b] + p0 + PL], f, k.PFC)
                        for j0 in range(0, PL // 128, 4):
                            nj = min(4, PL // 128 - j0)
                            pt = ptr[nt % 2]; nt += 1
                            for j in range(nj):
                                P.op("pe", lambda e: e.transpose(pt[:, j, :], f[:, (j0 + j) * 128:(j0 + j + 1) * 128], k.identb[0:QC, 0:QC]), [f, k.identb], [pt], pe_acc=True)
                            s2 = p0 // 128 + j0
                            oap = dst[:].rearrange("p c (s b) -> p c s b", b=NB)[:, :, s2:s2 + nj, b]
                            iap = pt[:, 0:nj, :].rearrange("p j c -> p c j")
                            if nt % 2 == 0:
                                P.op("dve", lambda e: e.tensor_copy(oap, iap), [pt], [dst])
                            else:
                                P.op("act", lambda e: e.copy(oap, iap), [pt], [dst])
            g1flat = G1t[:].rearrange("p c n -> p (c n)")
            tot = QC * NC
            for o0 in range(0, tot, 512):
                w_ = min(512, tot - o0)
                pp = py[nz % 2]; nz += 1
                P.op("pe", lambda e: e.matmul(pp[:, 0:w_], antib[:], g1flat[:, o0:o0 + w_], start=True, stop=True), [antib, G1t], [pp], pe_acc=True)
                P.op("dve", lambda e: e.tensor_copy(g1flat[:, o0:o0 + w_], pp[:, 0:w_]), [pp], [G1t])
            for ci in range(QC):
                ch = c0 + ci
                for order in range(2):
                    T = TT[ntt % 2]; ntt += 1
                    srcap = bass.AP(HGbuf.handle, (order * 1024 + ch) * 2 * L, [[1, 128], [1, TW]])
                    P.dma("sp", T[:], srcap, T, HGbuf)
                    pp = py[nz % 2]; nz += 1
                    mov = Vt[:, ci, :] if order == 0 else z1[ci % 2][:]
                    movb = Vt if order == 0 else z1[ci % 2]
                    ds = [0] + [d for d in range(-dmax, dmax + 1) if d != 0]
                    for di, d in enumerate(ds):
                        blk = (dmax - d) if order == 0 else (dmax + d)
                        lo, hi = max(0, d), min(n2, n2 + d)
                        P.op("pe", lambda e: e.matmul(pp[:, lo * NB:hi * NB], T[:, blk * 128:(blk + 1) * 128], mov[:, (lo - d) * NB:(hi - d) * NB],
                                                      start=(di == 0), stop=(di == len(ds) - 1)), [T, movb], [pp], pe_acc=True)
                    if order == 0:
                        zz = z1[ci % 2]
                        P.op("dve", lambda e: e.tensor_tensor(zz[:], pp[:, 0:NC], G1t[:, ci, :], ALU.mult), [pp, G1t], [zz])
                    else:
                        P.op("dve", lambda e: e.tensor_tensor(Y2t[:, :, ci], pp[:, 0:NC], G2t[:, ci, :], ALU.mult), [pp, G2t], [Y2t])
            for b in range(NB):
                for s0 in range(0, n2, 4):
                    ns = min(4, n2 - s0)
                    po = pob[no % 2]; os_ = ost[no % 2]; no += 1
                    for j in range(ns):
                        P.op("pe", lambda e: e.transpose(po[:, j * 128:(j + 1) * 128], Y2t[:, (s0 + j) * NB + b, :], k.identb[:]), [Y2t, k.identb], [po], pe_acc=True)
                    P.op("act", lambda e: e.copy(os_[:, 0:ns * 128], po[:, 0:ns * 128]), [po], [os_])
                    P.dma("pool", k.YFM.ap[c0:c0 + QC, tokbase[b] + s0 * 128:tokbase[b] + (s0 + ns) * 128], os_[:, 0:ns * 128], k.YFM, os_)


def mixer_odd(k, l, io, with_ctx):
    cfg = k.cfg
    short_conv_fm(k, io, with_ctx)
    hyena_filters(k, io, 0, cfg.S, k.HG)
    hyena_conv(k, io, False, cfg.S, k.HG)
    if with_ctx:
        hyena_filters(k, io, 1, cfg.LC, k.HGC)
        hyena_conv(k, io, True, cfg.LC, k.HGC)


def host_odd(cfg, inp, m):
    S, LC = cfg.S, cfg.LC
    f = lambda a: np.ascontiguousarray(a, dtype=np.float32)
    NO = inp["w_in_odd"].shape[0]
    m["w_in_odd"] = f(inp["w_in_odd"]); m["w_out_odd"] = f(inp["w_out_odd"])
    m["hy_short"] = f(np.transpose(inp["hy_short"], (0, 2, 1)))
    m["sc_conv"] = f(np.transpose(inp["sc_conv"], (0, 2, 1)))
    for nm in ("hy_w1", "hy_w2", "hy_w3", "hy_w4"):
        m[nm] = f(inp[nm])
    for nm in ("hy_b1", "hy_b2", "hy_b3"):
        m[nm] = f(inp[nm][:, :, None])
    m["hy_bias"] = f(inp["hy_bias"][:, :, :, None])
    embT = np.zeros((2, 2, 33, S), np.float32)
    tt = np.zeros((2, 2, S), np.float32)
    for seg, L in enumerate((S, LC)):
        t = np.linspace(0.0, 1.0, L, dtype=np.float32)[:, None]
        bands = 16
        freqs = np.linspace(1e-4, bands - 1, bands, dtype=np.float32)[None, :]
        w = (np.float32(2.0 * math.pi / L) * np.arange(L, dtype=np.float32))[:, None]
        emb = np.concatenate([t, np.cos(freqs * w), -np.sin(freqs * w)], axis=-1).astype(np.float32)
        embT[seg, 0, :, :L] = emb.T
        embT[seg, 1, :, :L] = emb[::-1].T
        tt[seg, 0, :L] = t[:, 0]
        tt[seg, 1, :L] = t[::-1, 0]
    m["hy_embT"] = embT
    m["hy_t"] = tt
    deltas = np.abs(np.linspace(math.log(1e-2) / 1.5, math.log(1e-2) / 0.3, 1024, dtype=np.float32))
    m["hy_delta"] = f(deltas[:, None])
    m["antiid"] = f(np.eye(128)[::-1])

def mixer_na(k, ie, ctx_out):
    P, cfg, I = k.P, k.cfg, k.I
    S, LC, TB, rows = cfg.S, cfg.LC, cfg.TB, cfg.rows
    kr = min(8, rows)
    RB = 16
    with P.scope():
        qT = P.sbuf("na_qT", [128, TB], BF16)
        kT = P.sbuf("na_kT", [128, TB], BF16)
        V = P.sbuf("na_V", [64, rows, 128], BF16)
        Vc = P.sbuf("na_Vc", [128, 2, 128], BF16)
        bias = P.sbuf("na_bias", [64, 8, 512], F32)
        ps_s = [P.psum("na_ps%d" % i, [128, 2, 512], F32) for i in range(1)]
        pTl = P.psum("na_pTl", [64, 8, 64], BF16)
        pTc = P.psum("na_pTc", [128, 2, 128], BF16)
        po = [P.psum("na_po%d" % i, [128, 128], F32) for i in range(2)]
        sl = [P.sbuf("na_sl%d" % i, [128, 768], F32) for i in range(2)]
        pe_ = [P.sbuf("na_pe%d" % i, [128, 768], BF16) for i in range(2)]
        st = [P.sbuf("na_st%d" % i, [128, 4], F32) for i in range(2)]
        pl = [P.sbuf("na_pl%d" % i, [64, 8, 64], BF16) for i in range(2)]
        pc = [P.sbuf("na_pc%d" % i, [128, 2, 128], BF16) for i in range(2)]
        ost = [P.sbuf("na_ost%d" % i, [64, RB, 128], BF16) for i in range(2)]
        oc = [P.sbuf("na_oc%d" % i, [128, 128], BF16) for i in range(2)]
        n = 0
        nos = 0
        for h in range(8):
            P.dma("sp", bias[:], I["na_bias"].ap[ie, h].rearrange("v q n -> q v n"), bias, I["na_bias"])
            for b in range(cfg.NB):
                tb0 = b * TB
                P.dma("sp", qT[:], k.PFM.ap[h * 128:(h + 1) * 128, tb0:tb0 + TB], qT, k.PFM)
                P.dma("sp", kT[:], k.PFM.ap[1024 + h * 128:1024 + (h + 1) * 128, tb0:tb0 + TB], kT, k.PFM)
                P.dma("sp", V[:], k.PTM.ap[tb0:tb0 + S, h * 128:(h + 1) * 128].rearrange("(r w) d -> w r d", w=64), V, k.PTM)
                P.dma("sp", Vc[:], k.PTM.ap[tb0 + S:tb0 + TB, h * 128:(h + 1) * 128].rearrange("(i p) d -> p i d", p=128), Vc, k.PTM)
                for r in range(rows):
                    i = n % 2; n += 1
                    rs = min(max(r - kr // 2, 0), rows - kr)
                    delta = r - rs
                    pp = ps_s[0]
                    q_ = qT[:, r * 64:(r + 1) * 64]
                    P.op("pe", lambda e: e.matmul(pp[0:64, 0, 0:kr * 64], q_, kT[:, rs * 64:(rs + kr) * 64], start=True, stop=True), [qT, kT], [pp], pe_acc=True)
                    P.op("pe", lambda e: e.matmul(pp[0:64, 1, 0:LC], q_, kT[:, S:S + LC], start=True, stop=True), [qT, kT], [pp], pe_acc=True)
                    s_ = sl[i]; e_ = pe_[i]; t_ = st[i]
                    nl = kr * 64
                    P.op("dve", lambda e: e.tensor_tensor(s_[0:64, 0:nl], pp[0:64, 0, 0:nl], bias[:, delta, 0:nl], ALU.add), [pp, bias], [s_])
                    P.op("act", lambda e: e.copy(s_[0:64, nl:nl + LC], pp[0:64, 1, 0:LC]), [pp], [s_])
                    P.op("dve", lambda e: e.tensor_reduce(t_[0:64, 0:1], s_[0:64, 0:nl + LC], AX.X, ALU.max, negate=True), [s_], [t_])
                    P.op("act", lambda e: e.activation(e_[0:64, 0:nl + LC], s_[0:64, 0:nl + LC], AF.Exp, bias=t_[0:64, 0:1], scale=1.0, accum_out=t_[0:64, 1:2]), [s_, t_], [e_, t_])
                    for j in range(kr):
                        P.op("pe", lambda e: e.transpose(pTl[:, j, :], e_[0:64, j * 64:(j + 1) * 64], k.identb[0:64, 0:64]), [e_, k.identb], [pTl], pe_acc=True)
                    for j in range(LC // 128):
                        P.op("pe", lambda e: e.transpose(pTc[:, j, 0:64], e_[0:64, nl + j * 128:nl + (j + 1) * 128], k.identb[0:64, 0:64]), [e_, k.identb], [pTc], pe_acc=True)
                    P.op("dve", lambda e: e.tensor_copy(pl[i][:, 0:kr, :], pTl[:, 0:kr, :]), [pTl], [pl[i]])
                    P.op("act", lambda e: e.copy(pc[i][:, :, 0:64], pTc[:, :, 0:64]), [pTc], [pc[i]])
                    o_ = po[i]
                    for j in range(kr):
                        P.op("pe", lambda e: e.matmul(o_[0:64, :], pl[i][:, j, :], V[:, rs + j, :], start=(j == 0), stop=False), [pl[i], V], [o_], pe_acc=True)
                    for j in range(LC // 128):
                        P.op("pe", lambda e: e.matmul(o_[0:64, :], pc[i][:, j, 0:64], Vc[:, j, :], start=False, stop=(j == LC // 128 - 1)), [pc[i], Vc], [o_], pe_acc=True)
                    P.op("dve", lambda e: e.reciprocal(t_[0:64, 2:3], t_[0:64, 1:2]), [t_], [t_])
                    os_ = ost[nos % 2]
                    P.op("dve", lambda e: e.tensor_scalar(os_[:, r % RB, :], o_[0:64, :], t_[0:64, 2:3], None, ALU.mult), [o_, t_], [os_])
                    if r % RB == RB - 1 or r == rows - 1:
                        r0 = (r // RB) * RB
                        nr = r - r0 + 1
                        P.dma("pool", k.YTM.ap[tb0 + r0 * 64:tb0 + (r0 + nr) * 64, h * 128:(h + 1) * 128].rearrange("(r w) d -> w r d", w=64), os_[:, 0:nr, :], k.YTM, os_)
                        nos += 1
                if ctx_out:
                    for qi in range(LC // 128):
                        i = n % 2; n += 1
                        pp = ps_s[0]
                        s_ = sl[i]; e_ = pe_[i]; t_ = st[i]
                        P.op("pe", lambda e: e.matmul(pp[:, 0, 0:LC], qT[:, S + qi * 128:S + (qi + 1) * 128], kT[:, S:S + LC], start=True, stop=True), [qT, kT], [pp], pe_acc=True)
                        P.op("dve", lambda e: e.tensor_reduce(t_[:, 0:1], pp[:, 0, 0:LC], AX.X, ALU.max, negate=True), [pp], [t_])
                        P.op("act", lambda e: e.activation(e_[:, 0:LC], pp[:, 0, 0:LC], AF.Exp, bias=t_[:, 0:1], scale=1.0, accum_out=t_[:, 1:2]), [pp, t_], [e_, t_])
                        for j in range(LC // 128):
                            P.op("pe", lambda e: e.transpose(pTc[:, j, :], e_[:, j * 128:(j + 1) * 128], k.identb[:]), [e_, k.identb], [pTc], pe_acc=True)
                        P.op("act", lambda e: e.copy(pc[i][:], pTc[:]), [pTc], [pc[i]])
                        o_ = po[i]
                        for j in range(LC // 128):
                            P.op("pe", lambda e: e.matmul(o_[:], pc[i][:, j, :], Vc[:, j, :], start=(j == 0), stop=(j == LC // 128 - 1)), [pc[i], Vc], [o_], pe_acc=True)
                        P.op("dve", lambda e: e.reciprocal(t_[:, 2:3], t_[:, 1:2]), [t_], [t_])
                        P.op("dve", lambda e: e.tensor_scalar(oc[i][:], o_[:], t_[:, 2:3], None, ALU.mult), [o_, t_], [oc[i]])
                        P.dma("pool", k.YTM.ap[tb0 + S + qi * 128:tb0 + S + (qi + 1) * 128, h * 128:(h + 1) * 128], oc[i][:], k.YTM, oc[i])


def mixer_gla(k, ie):
    P, cfg, I = k.P, k.cfg, k.I
    S, LC, TB = cfg.S, cfg.LC, cfg.TB
    nlt = S // 128
    nct = LC // 128
    with P.scope():
        cm = P.sbuf("gl_cm", [128, 2, 4, 128], F32)
        P.dma("sp", cm[:], I["gla_cm"].ap.rearrange("d m s t -> s d m t"), cm, I["gla_cm"])
        mk = P.sbuf("gl_mk", [128, 2, 128], F32)
        P.dma("sp", mk[:], I["gla_mask"].ap.rearrange("d s t -> s d t"), mk, I["gla_mask"])
        gup = P.sbuf("gl_gup", [16, 2, 256], F32)
        P.dma("sp", gup[:], I["gk_up"].ap[ie, :, 0:16, :].rearrange("d r u -> r d u"), gup, I["gk_up"])
        gkb = P.sbuf("gl_gkb", [1, 2, 256], F32)
        P.dma("sp", gkb[:], I["gk_up"].ap[ie, :, 16:17, :].rearrange("d r u -> r d u"), gkb, I["gk_up"])
        ones1 = P.sbuf("gl_ones", [1, 128], F32)
        P.op("dve", lambda e: e.memset(ones1[:], 1.0), [], [ones1])
        gn = P.sbuf("gl_gn", [128, 256], F32)
        P.dma("sp", gn[:], bc_rows(I["gla_norm"].ap[ie:ie + 1, :]), gn, I["gla_norm"])
        qk = [P.sbuf("gl_qk%d" % i, [128, 1024], BF16) for i in range(2)]
        vv = [P.sbuf("gl_v%d" % i, [128, 1024], BF16) for i in range(2)]
        gg = [P.sbuf("gl_g%d" % i, [128, 1024], BF16) for i in range(2)]
        lrb = [P.sbuf("gl_lrb%d" % i, [16, 128], BF16) for i in range(2)]
        lrf = [P.sbuf("gl_lrf%d" % i, [16, 128], F32) for i in range(2)]
        rq = [P.sbuf("gl_rq%d" % i, [128, 2, 64], F32) for i in range(2)]
        rk = [P.sbuf("gl_rk%d" % i, [128, 2, 64], F32) for i in range(2)]
        qr = P.sbuf("gl_qr", [128, 512], BF16)
        kr_ = P.sbuf("gl_kr", [128, 512], BF16)
        tmp = [P.sbuf("gl_tmp%d" % i, [128, 256], F32) for i in range(4)]
        la = P.sbuf("gl_la", [128, 256], F32)
        lax = P.sbuf("gl_lax", [128, 512], F32)
        e3 = P.sbuf("gl_e3", [128, 256], F32)
        khat = P.sbuf("gl_khat", [128, 512], BF16)
        E1 = P.sbuf("gl_E1", [128, 512], F32); E2 = P.sbuf("gl_E2", [128, 512], F32); E4 = P.sbuf("gl_E4", [128, 512], F32)
        dec = P.sbuf("gl_dec", [128, 8], F32)
        qtT = P.sbuf("gl_qtT", [128, 512], BF16); ktT = P.sbuf("gl_ktT", [128, 512], BF16); qeT = P.sbuf("gl_qeT", [128, 512], BF16)
        att = [P.sbuf("gl_att%d" % i, [128, 128], BF16) for i in range(2)]
        St = [P.sbuf("gl_S%d" % i, [128, 256], F32) for i in range(4)]
        Sb = [P.sbuf("gl_Sb%d" % i, [128, 256], BF16) for i in range(4)]
        of = [P.sbuf("gl_of%d" % i, [128, 1024], F32) for i in range(2)]
        ot = P.sbuf("gl_ot", [128, 1024], F32)
        sq = P.sbuf("gl_sq", [128, 256], BF16)
        fs = P.sbuf("gl_fs", [128, 16], F32)
        sg = P.sbuf("gl_sg", [128, 1024], F32)
        yo = [P.sbuf("gl_yo%d" % i, [128, 1024], BF16) for i in range(2)]
        psZ = P.psum("gl_psZ", [128, 256], F32)
        ps1 = P.psum("gl_ps1", [128, 512], F32)
        ps2 = P.psum("gl_ps2", [128, 512], F32)
        ps4 = P.psum("gl_ps4", [128, 4, 128], F32)
        psT = P.psum("gl_psT", [128, 8, 128], BF16)
        psA = P.psum("gl_psA", [128, 128], F32)
        psO = P.psum("gl_psO", [128, 256], F32)
        psD = P.psum("gl_psD", [128, 256], F32)
        n = 0
        SCALE = 128.0 ** -0.5

        def v4(ap):
            return ap.rearrange("p (h a b i) -> p h a b i", h=4, a=2, b=2)

        def u3(ap):
            return ap.rearrange("p (h a i) -> p h a i", h=4, a=2)

        for b in range(cfg.NB):
            tb0 = b * TB
            for d in range(2):
                for h in range(4):
                    P.op("dve", lambda e: e.memset(St[h][:], 0.0), [], [St[h]])
                    P.op("pool", lambda e: e.memset(Sb[h][:], 0.0), [], [Sb[h]])
                if d == 0:
                    order = [(True, i) for i in range(nct)] + [(False, t) for t in range(nlt)]
                else:
                    order = [(True, i) for i in reversed(range(nct))] + [(False, t) for t in reversed(range(nlt))]
                for (is_ctx, ti) in order:
                    i = n % 2; n += 1
                    t0 = tb0 + (S + ti * 128 if is_ctx else ti * 128)
                    q_, v_, g_ = qk[i], vv[i], gg[i]
                    P.dma("sp", q_[:], k.PTM.ap[t0:t0 + 128, 1024:2048], q_, k.PTM)
                    P.dma("sp", v_[:], k.PTM.ap[t0:t0 + 128, 2048:3072], v_, k.PTM)
                    P.dma("sp", lrb[i][:], k.PFM.ap[2048 + d * 16:2048 + (d + 1) * 16, t0:t0 + 128], lrb[i], k.PFM)
                    P.op("act", lambda e: e.copy(lrf[i][:], lrb[i][:]), [lrb[i]], [lrf[i]])
                    if not is_ctx:
                        P.dma("sp", rq[i][:], I["rope_q"].ap[ti * 128:(ti + 1) * 128], rq[i], I["rope_q"])
                        P.dma("sp", rk[i][:], I["rope_k"].ap[ti * 128:(ti + 1) * 128], rk[i], I["rope_k"])
                        for (src, dst, tab, eng) in ((q_[:, 0:512], qr, rq[i], "dve"), (q_[:, 512:1024], kr_, rk[i], "pool")):
                            x1 = v4(src)[:, :, :, 0, :]; x2 = v4(src)[:, :, :, 1, :]
                            o1 = v4(dst[:])[:, :, :, 0, :]; o2 = v4(dst[:])[:, :, :, 1, :]
                            cs = tab[:, 0, :].rearrange("p (a i) -> p a i", a=2).unsqueeze(1).to_broadcast([128, 4, 2, 32])
                            sn = tab[:, 1, :].rearrange("p (a i) -> p a i", a=2).unsqueeze(1).to_broadcast([128, 4, 2, 32])
                            ta, tb_ = u3(tmp[0][:]), u3(tmp[1][:])
                            if eng == "pool":
                                ta, tb_ = u3(tmp[2][:]), u3(tmp[3][:])
                            tA = tmp[0] if eng == "dve" else tmp[2]
                            tB = tmp[1] if eng == "dve" else tmp[3]
                            P.op(eng, lambda e: e.tensor_tensor(ta, x1, cs, ALU.mult), [q_, tab], [tA])
                            P.op(eng, lambda e: e.tensor_tensor(tb_, x2, sn, ALU.mult), [q_, tab], [tB])
                            P.op(eng, lambda e: e.tensor_tensor(o1, ta, tb_, ALU.subtract), [tA, tB], [dst])
                            P.op(eng, lambda e: e.tensor_tensor(ta, x2, cs, ALU.mult), [q_, tab, dst], [tA])
                            P.op(eng, lambda e: e.tensor_tensor(tb_, x1, sn, ALU.mult), [q_, tab, dst], [tB])
                            P.op(eng, lambda e: e.tensor_tensor(o2, ta, tb_, ALU.add), [tA, tB], [dst])
                    else:
                        P.op("dve", lambda e: e.tensor_scalar(qr[:], q_[:, 0:512], SCALE, None, ALU.mult), [q_], [qr])
                        P.op("pool", lambda e: e.tensor_copy(kr_[:], q_[:, 512:1024]), [q_], [kr_])
                    P.op("pe", lambda e: e.matmul(psZ[:], lrf[i][:], gup[:, d, :], start=True, stop=False), [lrf[i], gup], [psZ], pe_acc=True)
                    P.op("pe", lambda e: e.matmul(psZ[:], ones1[:], gkb[:, d, :], start=False, stop=True), [ones1, gkb], [psZ], pe_acc=True)
                    P.op("act", lambda e: e.activation(la[:], psZ[:], AF.Exp, scale=-1.0), [psZ], [la])
                    P.op("act", lambda e: e.activation(la[:], la[:], AF.Ln, bias=1.0, scale=1.0), [la], [la])
                    for bb in range(2):
                        P.op("pool", lambda e: e.tensor_copy(v4(lax[:])[:, :, :, bb, :], u3(la[:])), [la], [lax])
                    P.op("pe", lambda e: e.matmul(psZ[:], cm[:, d, 2, :], la[:], start=True, stop=True), [cm, la], [psZ], pe_acc=True)
                    P.op("act", lambda e: e.activation(e3[:], psZ[:], AF.Exp), [psZ], [e3])
                    for bb in range(2):
                        P.op("dve", lambda e: e.tensor_tensor(v4(khat[:])[:, :, :, bb, :], v4(kr_[:])[:, :, :, bb, :], u3(e3[:]), ALU.mult), [kr_, e3], [khat])
                    for h in range(4):
                        P.op("pe", lambda e: e.matmul(ps1[:, h * 128:(h + 1) * 128], lax[:, h * 128:(h + 1) * 128], cm[:, d, 0, :], start=True, stop=True), [lax, cm], [ps1], pe_acc=True)
                        P.op("pe", lambda e: e.matmul(ps2[:, h * 128:(h + 1) * 128], lax[:, h * 128:(h + 1) * 128], cm[:, d, 1, :], start=True, stop=True), [lax, cm], [ps2], pe_acc=True)
                        P.op("pe", lambda e: e.matmul(ps4[:, h, :], lax[:, h * 128:(h + 1) * 128], cm[:, d, 3, :], start=True, stop=True), [lax, cm], [ps4], pe_acc=True)
                    P.op("act", lambda e: e.activation(E1[:], ps1[:], AF.Exp), [ps1], [E1])
                    P.op("act", lambda e: e.activation(E2[:], ps1[:], AF.Exp, scale=-1.0), [ps1], [E2])
                    P.op("act", lambda e: e.activation(E4[:], ps2[:], AF.Exp), [ps2], [E4])
                    P.op("act", lambda e: e.activation(dec[:].rearrange("p (h c) -> p h c", h=4), ps4[:].rearrange("p h (c x) -> p h c x", x=64)[:, :, :, 0], AF.Exp), [ps4], [dec])
                    for h in range(4):
                        P.op("pe", lambda e: e.transpose(psT[:, h, :], qr[:, h * 128:(h + 1) * 128], k.identb[:]), [qr, k.identb], [psT], pe_acc=True)
                        P.op("pe", lambda e: e.transpose(psT[:, 4 + h, :], kr_[:, h * 128:(h + 1) * 128], k.identb[:]), [kr_, k.identb], [psT], pe_acc=True)
                    pq = psT[:, 0:4, :].rearrange("p h t -> p (h t)")
                    pk = psT[:, 4:8, :].rearrange("p h t -> p (h t)")
                    P.op("dve", lambda e: e.tensor_tensor(qtT[:], pq, E1[:], ALU.mult), [psT, E1], [qtT])
                    P.op("dve", lambda e: e.tensor_tensor(ktT[:], pk, E2[:], ALU.mult), [psT, E2], [ktT])
                    P.op("dve", lambda e: e.tensor_tensor(qeT[:], pq, E4[:], ALU.mult), [psT, E4], [qeT])
                    if d == 1:
                        P.dma("sp", of[i][:], k.OF.ap[t0:t0 + 128, :], of[i], k.OF)
                        P.dma("sp", g_[:], k.PTM.ap[t0:t0 + 128, 3072:4096], g_, k.PTM)
                    chunks = (0, 1) if d == 0 else (1, 0)
                    for h in range(4):
                        a_ = att[h % 2]
                        P.op("pe", lambda e: e.matmul(psA[:], ktT[:, h * 128:(h + 1) * 128], qtT[:, h * 128:(h + 1) * 128], start=True, stop=True), [ktT, qtT], [psA], pe_acc=True)
                        P.op("dve", lambda e: e.tensor_tensor(a_[:], psA[:], mk[:, d, :], ALU.mult), [psA, mk], [a_])
                        P.op("pe", lambda e: e.matmul(psO[:], a_[:], v_[:, h * 256:(h + 1) * 256], start=True, stop=False), [a_, v_], [psO], pe_acc=True)
                        for ci, c in enumerate(chunks):
                            rsl = slice(c * 64, (c + 1) * 64)
                            P.op("pe", lambda e: e.matmul(psO[rsl, :], qeT[:, h * 128 + c * 64:h * 128 + (c + 1) * 64], Sb[h][:], start=False, stop=(ci == 1)), [qeT, Sb[h]], [psO], pe_acc=True)
                            P.op("pe", lambda e: e.matmul(psD[:], khat[rsl, h * 128:(h + 1) * 128], v_[rsl, h * 256:(h + 1) * 256], start=True, stop=True), [khat, v_], [psD], pe_acc=True)
                            P.op("dve", lambda e: e.scalar_tensor_tensor(St[h][:], St[h][:], dec[:, h * 2 + c:h * 2 + c + 1], psD[:], ALU.mult, ALU.add), [St[h], dec, psD], [St[h]])
                            P.op("act", lambda e: e.copy(Sb[h][:], St[h][:]), [St[h]], [Sb[h]])
                        if d == 0:
                            P.op("act", lambda e: e.copy(of[i][:, h * 256:(h + 1) * 256], psO[:]), [psO], [of[i]])
                        else:
                            P.op("dve", lambda e: e.tensor_tensor(ot[:, h * 256:(h + 1) * 256], psO[:], of[i][:, h * 256:(h + 1) * 256], ALU.add), [psO, of[i]], [ot])
                    if d == 0:
                        P.dma("pool", k.OF.ap[t0:t0 + 128, :], of[i][:], k.OF, of[i])
                    else:
                        for h in range(4):
                            P.op("act", lambda e: e.activation(sq[:], ot[:, h * 256:(h + 1) * 256], AF.Square, accum_out=fs[:, h:h + 1]), [ot], [sq, fs])
                        P.op("dve", lambda e: e.tensor_scalar(fs[:, 4:8], fs[:, 0:4], 1.0 / 256.0, EPS, ALU.mult, ALU.add), [fs], [fs])
                        P.op("act", lambda e: e.activation(fs[:, 8:12], fs[:, 4:8], AF.Sqrt), [fs], [fs])
                        P.op("dve", lambda e: e.reciprocal(fs[:, 12:16], fs[:, 8:12]), [fs], [fs])
                        P.op("act", lambda e: e.activation(sg[:], g_[:], AF.Silu), [g_], [sg])
                        for h in range(4):
                            P.op("dve", lambda e: e.scalar_tensor_tensor(ot[:, h * 256:(h + 1) * 256], ot[:, h * 256:(h + 1) * 256], fs[:, 12 + h:13 + h], gn[:], ALU.mult, ALU.mult), [ot, fs, gn], [ot])
                        y_ = yo[i]
                        P.op("pool", lambda e: e.tensor_tensor(y_[:], ot[:], sg[:], ALU.mult), [ot, sg], [y_])
                        P.dma("pool", k.YTM.ap[t0:t0 + 128, 1024:2048], y_[:], k.YTM, y_)


def mixer_even(k, l, ie, ctx_out):
    mixer_na(k, ie, ctx_out)
    mixer_gla(k, ie)


def host_even(cfg, inp, m):
    S = cfg.S
    rows = cfg.rows
    f = lambda a: np.ascontiguousarray(a, dtype=np.float32)
    NE = inp["w_in_even"].shape[0]
    m["w_in_even"] = f(inp["w_in_even"]); m["w_out_even"] = f(inp["w_out_even"])
    rpb = np.asarray(inp["na_rpb"], np.float32)
    kr = min(8, rows)
    q = np.arange(64)[:, None]; w = np.arange(64)[None, :]
    cstart = np.clip(q - 8, 0, 64 - 16)
    col_ok = (w >= cstart) & (w < cstart + 16)
    coff = np.clip(w - q, -15, 15) + 15
    nab = np.full((NE, 8, 8, 64, 512), -1e30, np.float32)
    for delta in range(8):
        for j in range(kr):
            roff = j - delta + 7
            if roff < 0 or roff > 14:
                continue
            g = rpb[:, :, roff, :][:, :, coff]
            nab[:, :, delta, :, j * 64:(j + 1) * 64] = np.where(col_ok[None, None], g, np.float32(-1e30))
    m["na_bias"] = nab
    gk = np.zeros((NE, 2, 17, 256), np.float32)
    gk[:, :, 0:16, :] = inp["gla_gk_up"]
    gk[:, :, 16, :] = inp["gla_gk_bias"]
    m["gk_up"] = gk
    m["gla_norm"] = f(inp["gla_norm"])
    nf = 32
    inv = (10000.0 ** (-np.arange(nf, dtype=np.float32) / nf)).astype(np.float32)
    pos = np.arange(S)
    ang = np.stack([(pos // 64).astype(np.float32)[:, None] * inv[None, :], (pos % 64).astype(np.float32)[:, None] * inv[None, :]], axis=1)
    cs = np.cos(ang).astype(np.float32).reshape(S, 64); sn = np.sin(ang).astype(np.float32).reshape(S, 64)
    m["rope_k"] = f(np.stack([cs, sn], axis=1))
    m["rope_q"] = f(np.stack([cs, sn], axis=1) * np.float32(128.0 ** -0.5))
    cmat = np.zeros((2, 5, 128, 128), np.float32)
    mask = np.zeros((2, 128, 128), np.float32)
    s = np.arange(128)[:, None]; t = np.arange(128)[None, :]
    same = (s // 64) == (t // 64)
    g = -1.0 / 16.0
    for d in range(2):
        if d == 0:
            Mb = same & (s <= t)
            Mmid = same & ((s % 64) <= 31)
        else:
            Mb = same & (s >= t)
            Mmid = same & ((s % 64) >= 32)
        Mlast = same
        cmat[d, 0] = g * (Mb.astype(np.float32) - Mmid.astype(np.float32))
        cmat[d, 1] = g * Mb
        cmat[d, 2] = g * (Mlast.astype(np.float32) - Mb.astype(np.float32))
        cmat[d, 3] = g * Mlast
        mask[d] = Mb
    m["gla_cm"] = f(cmat[:, 0:4])
    m["gla_mask"] = mask

def host_inputs(cfg, inp, batches):
    NB, S, LC, E, NG = cfg.NB, cfg.S, cfg.LC, cfg.E, cfg.NG
    NL = len(cfg.layers)
    f = lambda a: np.ascontiguousarray(a, dtype=np.float32)
    m = {}
    m["x"] = f(inp["x"][batches].reshape(NB * S, D))
    m["ctx"] = f(inp["ctx"][batches].reshape(NB * LC, D))
    m["cvec"] = f(np.concatenate([inp["c"][batches], inp["c_ctx"][None, :]], axis=0))
    m["w_mod"] = f(inp["w_mod"][:NL]); m["b_mod"] = f(inp["b_mod"][:NL])
    m["norm_mix"] = f(inp["norm_mix"][:NL]); m["norm_ffn"] = f(inp["norm_ffn"][:NL])
    m["norm_final"] = f(inp["norm_final"][None, :])
    m["w_router"] = f(np.concatenate([inp["moe_w_group"][:NL], inp["moe_w_expert"][:NL]], axis=2))
    m["b_router"] = f(np.concatenate([inp["moe_b_group"][:NL], inp["moe_b_expert"][:NL]], axis=1))
    w1 = inp["moe_w1"][:NL]; w3 = inp["moe_w3"][:NL]; w2 = inp["moe_w2"][:NL]
    m["w1h"] = f(w1.reshape(NL, E, KC, 128, 512).transpose(0, 1, 3, 2, 4).reshape(NL * E * 128, KC * 512))
    m["w3h"] = f(w3.reshape(NL, E, KC, 128, 512).transpose(0, 1, 3, 2, 4).reshape(NL * E * 128, KC * 512))
    m["w2h"] = f(w2.reshape(NL, E, 4, 128, D).transpose(0, 1, 3, 2, 4).reshape(NL * E * 128, 4 * D))
    m["ident"] = np.eye(128, dtype=np.float32)
    m["lstrict"] = np.triu(np.ones((128, 128), np.float32), 1)
    m["iota_p"] = np.arange(128, dtype=np.float32).reshape(128, 1)
    m["blkstart"] = (128.0 * np.arange(cfg.nblk, dtype=np.float32)).reshape(1, -1)
    m["iota_e"] = np.arange(E, dtype=np.float32).reshape(1, -1)
    kinds = [kd for kd, _ in cfg.layers]
    if "even" in kinds:
        host_even(cfg, inp, m)
    if "odd" in kinds:
        host_odd(cfg, inp, m)
    return m


FULL_LAYERS = [("even", True), ("odd", True), ("even", False), ("odd", False)]


def kernel(**inputs):
    inp = {k_: np.asarray(v) for k_, v in inputs.items()}
    NCORE, NB = 2, 2
    cfg = Cfg(NB=NB, S=8192, layers=FULL_LAYERS)
    nc, k = build_program(cfg)
    maps = [host_inputs(cfg, inp, list(range(c * NB, (c + 1) * NB))) for c in range(NCORE)]
    for c in range(1, NCORE):
        for name, arr in maps[0].items():
            if name not in ("x", "ctx", "cvec"):
                maps[c][name] = arr
    res = run_bass_kernel_spmd(nc, maps, core_ids=list(range(NCORE)))
    out = np.concatenate([res.results[c]["out"].reshape(NB, 8192, D) for c in range(NCORE)], axis=0)
    return np.ascontiguousarray(out.astype(np.float32))
```

```python
import contextlib
import math
import numpy as np
import concourse.bass as bass
import concourse.mybir as mybir
from concourse.bass_utils import run_bass_kernel_spmd

F32 = mybir.dt.float32
BF16 = mybir.dt.bfloat16
I32 = mybir.dt.int32
AF = mybir.ActivationFunctionType
ALU = mybir.AluOpType
AX = mybir.AxisListType


class Buf:
    def __init__(self, name, ap=None, dram=False):
        self.name = name
        self.ap = ap
        self.dram = dram
        self.w = {}
        self.r = {}
        self.dsem = None
        self.dval = 0

    def __getitem__(self, k):
        return self.ap[k]


class Prog:
    ENG = ("pe", "act", "dve", "pool", "sp")

    def __init__(self, nc, stack):
        self.nc = nc
        self.gstack = stack
        self.stacks = [stack]
        self.eng = {"pe": nc.tensor, "act": nc.scalar, "dve": nc.vector, "pool": nc.gpsimd, "sp": nc.sync}
        self.semobj = {}
        self.semval = {}
        for e in ("pe", "act", "dve", "pool"):
            self.semobj[e] = stack.enter_context(nc.semaphore("s_" + e))
            self.semval[e] = 0
        self.waited = {e: {} for e in self.ENG}
        self.free_dsems = []
        self.ndsem = 0
        self.live = [[]]
        self.ninst = {e: 0 for e in self.ENG}
        self.semA = stack.enter_context(nc.semaphore("s_rsA"))
        self.semB = stack.enter_context(nc.semaphore("s_rsB"))
        self.epoch = 0
        self.LIMIT = 1 << 30

    @contextlib.contextmanager
    def scope(self):
        self.barrier()
        st = contextlib.ExitStack()
        self.stacks.append(st)
        self.live.append([])
        try:
            with st:
                yield
                self.barrier()
        finally:
            self.stacks.pop()
            for b in self.live.pop():
                if b.dsem is not None:
                    self.free_dsems.append(b.dsem)
                    b.dsem = None

    def sbuf(self, name, shape, dtype):
        self.uid = getattr(self, "uid", 0) + 1
        name = "%s_%d" % (name, self.uid)
        t = self.stacks[-1].enter_context(self.nc.sbuf_tensor(name, list(shape), dtype))
        b = Buf(name, t)
        self.live[-1].append(b)
        return b

    def psum(self, name, shape, dtype):
        self.uid = getattr(self, "uid", 0) + 1
        name = "%s_%d" % (name, self.uid)
        t = self.stacks[-1].enter_context(self.nc.psum_tensor(name, list(shape), dtype))
        b = Buf(name, t)
        self.live[-1].append(b)
        return b

    def dram(self, name, shape, dtype, kind="Internal"):
        t = self.nc.dram_tensor(name, list(shape), dtype, kind=kind)
        b = Buf(name, t.ap(), dram=True)
        b.handle = t
        self.live[0].append(b)
        return b

    def _dsem(self, b):
        if b.dsem is None:
            if self.free_dsems:
                b.dsem = self.free_dsems.pop()
            else:
                b.dsem = "d%d" % self.ndsem
                self.ndsem += 1
                self.semobj[b.dsem] = self.gstack.enter_context(self.nc.semaphore(b.dsem))
                self.semval[b.dsem] = 0
        return b.dsem

    def _wait(self, e, key, val):
        if val <= self.waited[e].get(key, 0):
            return
        self.waited[e][key] = val
        self.eng[e].wait_ge(self.semobj[key], val)
        self.ninst[e] += 1

    def _deps(self, e, reads, writes, pe_acc=False):
        for b in reads:
            for k, v in b.w.items():
                self._wait(e, k, v)
        for b in writes:
            for k, v in b.w.items():
                if b.dram:
                    continue
                if pe_acc and k == "pe":
                    continue
                self._wait(e, k, v)
            for k, v in b.r.items():
                self._wait(e, k, v)

    def _commit(self, reads, writes, key, val):
        for b in reads:
            b.r[key] = max(b.r.get(key, 0), val)
        for b in writes:
            if b.dram:
                b.w[key] = max(b.w.get(key, 0), val)
            else:
                b.w = {key: val}
            b.r = {}

    def reset(self):
        self.barrier()
        self.epoch += 1
        for e in self.ENG:
            self.eng[e].sem_inc(self.semA, 1)
        self.eng["sp"].wait_ge(self.semA, 5 * self.epoch)
        for key, v in self.semval.items():
            if v > 0:
                self.eng["sp"].sem_clear(self.semobj[key])
        self.eng["sp"].sem_inc(self.semB, 1)
        for e in self.ENG:
            self.eng[e].wait_ge(self.semB, self.epoch)
        for key in self.semval:
            self.semval[key] = 0
        self.waited = {e: {} for e in self.ENG}

    def _check(self):
        if max(self.semval.values()) >= self.LIMIT:
            self.reset()

    def op(self, e, fn, reads=(), writes=(), pe_acc=False):
        self._check()
        self._deps(e, reads, writes, pe_acc)
        ins = fn(self.eng[e])
        self.semval[e] += 1
        ins.then_inc(self.semobj[e], 1)
        self.ninst[e] += 1
        self._commit(reads, writes, e, self.semval[e])

    def dma(self, q, out_ap, in_ap, dst, src, extra_reads=(), **kw):
        self.dma_raw(q, lambda e: e.dma_start(out=out_ap, in_=in_ap, **kw), dst, [src] + list(extra_reads))

    def dma_raw(self, q, fn, dst, reads):
        self._check()
        self._deps(q, reads, [dst])
        key = self._dsem(dst)
        self.semval[key] += 16
        fn(self.eng[q]).then_inc(self.semobj[key], 16)
        self.ninst[q] += 1
        self._commit(reads, [dst], key, self.semval[key])

    def barrier(self):
        for e in self.ENG:
            for k, v in self.semval.items():
                if v > 0:
                    self._wait(e, k, v)
        for lst in self.live:
            for b in lst:
                b.w = {}
                b.r = {}

D = 2048
KC = 16
EVEN_IN = 6176
ODD_IN = 6144
EPS = 1e-6
BIGIDX = float(1 << 22)


class Cfg:
    def __init__(self, NB, S, layers, NG=4, EPG=8, LC=256):
        self.NB, self.S, self.LC = NB, S, LC
        self.layers = layers
        self.NG, self.EPG, self.E = NG, EPG, NG * EPG
        self.DE = 512
        self.TB = S + LC
        self.NT = NB * self.TB
        self.ntile = self.NT // 128
        self.rows = S // 64
        self.A = 2 * self.NT
        self.nblk = self.A // 128 + self.E
        self.cap = self.nblk * 128

    def groups(self, with_ctx=True):
        gs = []
        for b in range(self.NB):
            base = b * self.TB
            for g in range(self.S // 512):
                gs.append((base + 512 * g, 512, b, False))
            if with_ctx:
                gs.append((base + self.S, self.LC, b, True))
        return gs


class K:
    debug = False

    def dump(self, name, buf):
        if not self.debug:
            return
        P = self.P
        shape = list(buf.ap.shape)
        o = P.dram("dbg_" + name, shape, buf.ap.dtype, kind="ExternalOutput")
        P.barrier()
        if len(shape) == 3:
            for i in range(shape[0]):
                P.dma("sp", o.ap[i], buf.ap[i], o, buf)
        else:
            P.dma("sp", o.ap[:], buf.ap[:], o, buf)
        P.barrier()


def bc_rows(ap_row, n=128):
    return ap_row.partition_broadcast(n)


def build_program(cfg):
    nc = bass.Bass("TRN2", target_bir_lowering=False)
    with contextlib.ExitStack() as st:
        P = Prog(nc, st)
        k = K()
        k.P, k.cfg, k.nc = P, cfg, nc
        k.debug = getattr(cfg, 'debug', False)
        NB, S, LC, NT, E, NG = cfg.NB, cfg.S, cfg.LC, cfg.NT, cfg.E, cfg.NG
        NL = len(cfg.layers)
        NE = sum(1 for kd, _ in cfg.layers if kd == "even")
        NO = sum(1 for kd, _ in cfg.layers if kd == "odd")
        I = {}

        def inp(name, shape, dt=F32):
            I[name] = P.dram(name, shape, dt, kind="ExternalInput")
            return I[name]
        inp("x", [NB * S, D]); inp("ctx", [NB * LC, D]); inp("cvec", [NB + 1, D])
        inp("w_mod", [NL, D, 6 * D]); inp("b_mod", [NL, 6 * D])
        inp("norm_mix", [NL, D]); inp("norm_ffn", [NL, D]); inp("norm_final", [1, D])
        inp("w_router", [NL, D, NG + E]); inp("b_router", [NL, NG + E])
        inp("w1h", [NL * E * 128, KC * 512]); inp("w3h", [NL * E * 128, KC * 512]); inp("w2h", [NL * E * 128, 4 * D])
        inp("ident", [128, 128]); inp("lstrict", [128, 128]); inp("iota_p", [128, 1])
        inp("blkstart", [1, cfg.nblk]); inp("iota_e", [1, E])
        if NE:
            inp("w_in_even", [NE, D, EVEN_IN]); inp("w_out_even", [NE, D, D])
            inp("na_bias", [NE, 8, 8, 64, 512]); inp("gk_up", [NE, 2, 17, 256]); inp("gla_norm", [NE, 256])
            inp("rope_q", [S, 2, 64]); inp("rope_k", [S, 2, 64])
            inp("gla_cm", [2, 4, 128, 128]); inp("gla_mask", [2, 128, 128])
        if NO:
            inp("w_in_odd", [NO, D, ODD_IN]); inp("w_out_odd", [NO, D, D])
            inp("hy_short", [NO, 3072, 3]); inp("sc_conv", [NO, 1024, 3])
            inp("hy_w1", [NO, 33, 64]); inp("hy_b1", [NO, 64, 1]); inp("hy_w2", [NO, 64, 64]); inp("hy_b2", [NO, 64, 1])
            inp("hy_w3", [NO, 64, 64]); inp("hy_b3", [NO, 64, 1]); inp("hy_w4", [NO, 64, 4096]); inp("hy_bias", [NO, 2, 1024, 1])
            inp("hy_embT", [2, 2, 33, S]); inp("hy_t", [2, 2, S]); inp("hy_delta", [1024, 1])
            inp("antiid", [128, 128])
        k.I = I
        out = P.dram("out", [NB * S, D], F32, kind="ExternalOutput")
        k.X = P.dram("X", [NT, D], F32)
        k.MOD = P.dram("MOD", [NB + 1, 6 * D], F32)
        k.MG = P.dram("MG", [NB + 1, 2 * D], F32)
        k.PTM = P.dram("PTM", [NT, 4096], BF16)
        k.PFM = P.dram("PFM", [6144, NT], BF16)
        k.PFC = P.dram("PFC", [3072, NT], BF16)
        k.YTM = P.dram("YTM", [NT, D], BF16)
        k.YFM = P.dram("YFM", [D, NT], BF16)
        k.H2 = P.dram("H2", [NT, D], BF16)
        k.XS = P.dram("XS", [cfg.cap, D], BF16)
        k.YS = P.dram("YS", [cfg.cap, D], BF16)
        k.OF = P.dram("OF", [NT, 1024], F32)
        k.HG = P.dram("HG", [2, 1024, 2 * S], BF16)
        k.HGC = P.dram("HGC", [2, 1024, 2 * LC], BF16)
        k.identf = P.sbuf("identf", [128, 128], F32)
        k.identb = P.sbuf("identb", [128, 128], BF16)
        P.dma("sp", k.identf[:], I["ident"][:], k.identf, I["ident"])
        P.op("dve", lambda e: e.tensor_copy(k.identb[:], k.identf[:]), [k.identf], [k.identb])
        for b in range(NB):
            P.dma("sp", k.X.ap[b * cfg.TB:b * cfg.TB + S, :], I["x"].ap[b * S:(b + 1) * S, :], k.X, I["x"])
            P.dma("sp", k.X.ap[b * cfg.TB + S:(b + 1) * cfg.TB, :], I["ctx"].ap[b * LC:(b + 1) * LC, :], k.X, I["ctx"])
        ie = io = 0
        for l, (kind, ctx_out) in enumerate(cfg.layers):
            with_ctx = ctx_out or kind == "zero"
            phase_mod(k, l)
            if kind == "even":
                phase_win_even(k, l, ie)
                k.dump("PFM%d" % l, k.PFM); k.dump("PTM%d" % l, k.PTM)
                mixer_even(k, l, ie, ctx_out)
                k.dump("YTM%d" % l, k.YTM)
                phase_wout(k, l, I["w_out_even"].ap[ie], "tm", with_ctx)
                k.dump("X%d" % l, k.X)
                ie += 1
            elif kind == "odd":
                phase_win_odd(k, l, io, with_ctx)
                k.dump("PFM%d" % l, k.PFM)
                mixer_odd(k, l, io, with_ctx)
                k.dump("PFC%d" % l, k.PFC); k.dump("YFM%d" % l, k.YFM); k.dump("HG%d" % l, k.HG); k.dump("HGC%d" % l, k.HGC)
                phase_wout(k, l, I["w_out_odd"].ap[io], "fm", with_ctx)
                k.dump("X%d" % l, k.X)
                io += 1
            phase_moe(k, l, with_ctx)
        phase_final(k, out)
        P.barrier()
        k.ninst = dict(P.ninst)
    return nc, k


def phase_mod(k, l):
    P, cfg, I = k.P, k.cfg, k.I
    R = cfg.NB + 1
    with P.scope():
        cv = P.sbuf("cv", [128, R, KC], F32)
        sv = P.sbuf("sv", [128, KC, R], F32)
        P.dma("sp", cv[:], I["cvec"].ap.rearrange("r (k p) -> p r k", p=128), cv, I["cvec"], allow_slow_non_contiguous=True)
        for r in range(R):
            P.op("act", lambda e: e.activation(sv[:, :, r], cv[:, r, :], AF.Silu), [cv], [sv])
        wt = [P.sbuf("wmod%d" % i, [128, KC, 512], F32) for i in range(2)]
        bmt = [P.sbuf("bm%d" % i, [R, 512], F32) for i in range(2)]
        ps = [P.psum("psmod%d" % i, [R, 512], F32) for i in range(2)]
        mo = P.sbuf("mo", [R, 6 * D], F32)
        for j in range(24):
            w = wt[j % 2]
            P.dma("sp", w[:], I["w_mod"].ap[l, :, j * 512:(j + 1) * 512].rearrange("(k p) n -> p k n", p=128), w, I["w_mod"])
            pp = ps[j % 2]
            bm = bmt[j % 2]
            P.dma("sp", bm[:], bc_rows(I["b_mod"].ap[l:l + 1, j * 512:(j + 1) * 512], R), bm, I["b_mod"])
            for kk in range(KC):
                P.op("pe", lambda e: e.matmul(pp[:], sv[:, kk, :], w[:, kk, :], start=(kk == 0), stop=(kk == KC - 1)), [sv, w], [pp], pe_acc=True)
            P.op("dve", lambda e: e.tensor_tensor(mo[:, j * 512:(j + 1) * 512], pp[:], bm[:], ALU.add), [pp, bm], [mo])
        P.dma("sp", k.MOD.ap[:], mo[:], k.MOD, mo)
        nm = P.sbuf("nm", [R, 2 * D], F32)
        P.dma("sp", nm[:, 0:D], bc_rows(I["norm_mix"].ap[l:l + 1, :], R), nm, I["norm_mix"])
        P.dma("sp", nm[:, D:2 * D], bc_rows(I["norm_ffn"].ap[l:l + 1, :], R), nm, I["norm_ffn"])
        mg = P.sbuf("mg", [R, 2 * D], F32)
        P.op("dve", lambda e: e.scalar_tensor_tensor(mg[:, 0:D], mo[:, D:2 * D], 1.0, nm[:, 0:D], ALU.add, ALU.mult), [mo, nm], [mg])
        P.op("dve", lambda e: e.scalar_tensor_tensor(mg[:, D:2 * D], mo[:, 4 * D:5 * D], 1.0, nm[:, D:2 * D], ALU.add, ALU.mult), [mo, nm], [mg])
        P.dma("sp", k.MG.ap[:], mg[:], k.MG, mg)


class NormProvider:
    def __init__(self, k, which, on_tile=None, transpose=True):
        P = k.P
        self.k, self.which, self.on_tile = k, which, on_tile
        self.transpose = transpose
        self.xt = [P.sbuf("np_x%d" % i, [128, D], F32) for i in range(2)]
        self.sq = P.sbuf("np_sq", [128, D], BF16)
        self.st = [P.sbuf("np_st%d" % i, [128, 4], F32) for i in range(2)]
        self.hf = [P.sbuf("np_hf%d" % i, [128, D], F32) for i in range(2)]
        self.hb = [P.sbuf("np_hb%d" % i, [128, D], BF16) for i in range(2)]
        self.G = P.sbuf("np_G", [128, D], F32)
        self.SH = P.sbuf("np_SH", [128, D], F32)
        if transpose:
            self.aT = [P.sbuf("np_aT%d" % i, [128, KC, 512], BF16) for i in range(2)]
            self.pT = [P.psum("np_pT%d" % i, [128, 8, 128], BF16) for i in range(2)]
        self.cur = None
        self.n = 0
        self.ng = 0

    def get(self, grp):
        k, P = self.k, self.k.P
        tok0, ntok, b, is_ctx = grp
        r = k.cfg.NB if is_ctx else b
        if self.cur != r:
            self.cur = r
            so = 0 if self.which == 1 else 3 * D
            go = 0 if self.which == 1 else D
            P.dma("sp", self.G[:], bc_rows(k.MG.ap[r:r + 1, go:go + D]), self.G, k.MG)
            P.dma("sp", self.SH[:], bc_rows(k.MOD.ap[r:r + 1, so:so + D]), self.SH, k.MOD)
        aT = self.aT[self.ng % 2] if self.transpose else None
        self.ng += 1
        for sub in range(ntok // 128):
            i = self.n % 2
            self.n += 1
            xt, st, hf, hb = self.xt[i], self.st[i], self.hf[i], self.hb[i]
            t0 = tok0 + sub * 128
            P.dma("sp", xt[:], k.X.ap[t0:t0 + 128, :], xt, k.X)
            P.op("act", lambda e: e.activation(self.sq[:], xt[:], AF.Square, accum_out=st[:, 0:1]), [xt], [self.sq, st])
            P.op("dve", lambda e: e.tensor_scalar(st[:, 1:2], st[:, 0:1], 1.0 / D, EPS, ALU.mult, ALU.add), [st], [st])
            P.op("act", lambda e: e.activation(st[:, 2:3], st[:, 1:2], AF.Sqrt), [st], [st])
            P.op("dve", lambda e: e.reciprocal(st[:, 3:4], st[:, 2:3]), [st], [st])
            P.op("dve", lambda e: e.scalar_tensor_tensor(hf[:], xt[:], st[:, 3:4], self.G[:], ALU.mult, ALU.mult), [xt, st, self.G], [hf])
            P.op("pool", lambda e: e.tensor_tensor(hf[:], hf[:], self.SH[:], ALU.add), [hf, self.SH], [hf])
            P.op("act", lambda e: e.copy(hb[:], hf[:]), [hf], [hb])
            if self.on_tile is not None:
                self.on_tile(t0, hf, hb)
            if not self.transpose:
                continue
            for half in range(2):
                pT = self.pT[half]
                for kk in range(8):
                    kq = half * 8 + kk
                    P.op("pe", lambda e: e.transpose(pT[:, kk, :], hb[:, kq * 128:(kq + 1) * 128], k.identb[:]), [hb, k.identb], [pT], pe_acc=True)
                eng = "dve" if half == 0 else "act"
                if eng == "dve":
                    P.op("dve", lambda e: e.tensor_copy(aT[:, half * 8:(half + 1) * 8, sub * 128:(sub + 1) * 128], pT[:]), [pT], [aT])
                else:
                    P.op("act", lambda e: e.copy(aT[:, half * 8:(half + 1) * 8, sub * 128:(sub + 1) * 128], pT[:]), [pT], [aT])
        return aT


class TmProvider:
    def __init__(self, k, src):
        P = k.P
        self.k, self.src = k, src
        self.yb = [P.sbuf("tp_y%d" % i, [128, D], BF16) for i in range(2)]
        self.aT = [P.sbuf("tp_aT%d" % i, [128, KC, 512], BF16) for i in range(2)]
        self.pT = [P.psum("tp_pT%d" % i, [128, 8, 128], BF16) for i in range(2)]
        self.n = 0
        self.ng = 0

    def get(self, grp):
        k, P = self.k, self.k.P
        tok0, ntok, b, is_ctx = grp
        aT = self.aT[self.ng % 2]
        self.ng += 1
        for sub in range(ntok // 128):
            yb = self.yb[self.n % 2]
            self.n += 1
            t0 = tok0 + sub * 128
            P.dma("sp", yb[:], self.src.ap[t0:t0 + 128, :], yb, self.src)
            for half in range(2):
                pT = self.pT[half]
                for kk in range(8):
                    kq = half * 8 + kk
                    P.op("pe", lambda e: e.transpose(pT[:, kk, :], yb[:, kq * 128:(kq + 1) * 128], k.identb[:]), [yb, k.identb], [pT], pe_acc=True)
                if half == 0:
                    P.op("dve", lambda e: e.tensor_copy(aT[:, 0:8, sub * 128:(sub + 1) * 128], pT[:]), [pT], [aT])
                else:
                    P.op("act", lambda e: e.copy(aT[:, 8:16, sub * 128:(sub + 1) * 128], pT[:]), [pT], [aT])
        return aT


class FmProvider:
    def __init__(self, k, src):
        P = k.P
        self.k, self.src = k, src
        self.aT = [P.sbuf("fp_aT%d" % i, [128, KC, 512], BF16) for i in range(2)]
        self.ng = 0

    def get(self, grp):
        k, P = self.k, self.k.P
        tok0, ntok, b, is_ctx = grp
        aT = self.aT[self.ng % 2]
        self.ng += 1
        P.dma("sp", aT[:, :, 0:ntok], self.src.ap[:, tok0:tok0 + ntok].rearrange("(k p) t -> p k t", p=128), aT, self.src)
        return aT


def gemm(k, W, passes, groups, provider, epilogue, wcols=2048):
    P = k.P
    wb = P.sbuf("g_wb", [128, KC, wcols], BF16)
    ps = [P.psum("g_ps%d" % i, [128, 512], F32) for i in range(4)]
    npz = 0
    for chunks in passes:
        off = 0
        offs = []
        for (kind, c0, ncols, tag) in chunks:
            P.dma("pool", wb[:, :, off:off + ncols], W[:, c0:c0 + ncols].rearrange("(k p) n -> p k n", p=128), wb, k.I["ident"])
            offs.append(off)
            off += ncols
        for grp in groups:
            tok0, ntok, b, is_ctx = grp
            aT = provider.get(grp)
            for ci, (kind, c0, ncols, tag) in enumerate(chunks):
                o = offs[ci]
                if kind != "fm":
                    continue
                pp = ps[npz % 4]
                npz += 1
                for kk in range(KC):
                    P.op("pe", lambda e: e.matmul(pp[0:ncols, 0:ntok], wb[:, kk, o:o + ncols], aT[:, kk, 0:ntok], start=(kk == 0), stop=(kk == KC - 1)), [aT, wb], [pp], pe_acc=True)
                epilogue(grp, None, (kind, c0, ncols, tag), pp)
            for sub in range(ntok // 128):
                for ci, (kind, c0, ncols, tag) in enumerate(chunks):
                    o = offs[ci]
                    if kind != "tm":
                        continue
                    pp = ps[npz % 4]
                    npz += 1
                    for kk in range(KC):
                        P.op("pe", lambda e: e.matmul(pp[:, 0:ncols], aT[:, kk, sub * 128:(sub + 1) * 128], wb[:, kk, o:o + ncols], start=(kk == 0), stop=(kk == KC - 1)), [aT, wb], [pp], pe_acc=True)
                    epilogue(grp, sub, (kind, c0, ncols, tag), pp)


class Evac:
    def __init__(self, k, n=4):
        P = k.P
        self.k = k
        self.st = [P.sbuf("ev_st%d" % i, [128, 512], BF16) for i in range(n)]
        self.i = 0

    def put(self, pp_buf, pp_ap, dst_buf, dst_ap, scale=None):
        P = self.k.P
        st = self.st[self.i % len(self.st)]
        use_act = (self.i % 2 == 0)
        self.i += 1
        shp = pp_ap.shape
        sap = st[0:shp[0], 0:shp[1]]
        if scale is not None:
            P.op("act", lambda e: e.activation(sap, pp_ap, AF.Copy, scale=float(scale)), [pp_buf], [st])
        elif use_act:
            P.op("act", lambda e: e.copy(sap, pp_ap), [pp_buf], [st])
        else:
            P.op("dve", lambda e: e.tensor_copy(sap, pp_ap), [pp_buf], [st])
        P.dma("pool", dst_ap, sap, dst_buf, st)


def phase_wout(k, l, W, src_kind, with_ctx):
    P, cfg = k.P, k.cfg
    with P.scope():
        prov = TmProvider(k, k.YTM) if src_kind == "tm" else FmProvider(k, k.YFM)
        xt = [P.sbuf("wo_x%d" % i, [128, D], F32) for i in range(2)]
        tmp = [P.sbuf("wo_t%d" % i, [128, 512], F32) for i in range(2)]
        gate = P.sbuf("wo_gate", [128, D], F32)
        state = {"cur": None, "n": 0, "t": 0}

        def epi(grp, sub, chunk, pp):
            tok0, ntok, b, is_ctx = grp
            kind, c0, ncols, tag = chunk
            r = cfg.NB if is_ctx else b
            if state["cur"] != r:
                state["cur"] = r
                P.dma("sp", gate[:], bc_rows(k.MOD.ap[r:r + 1, 2 * D:3 * D]), gate, k.MOD)
            t0 = tok0 + sub * 128
            if c0 == 0:
                state["n"] += 1
                x = xt[state["n"] % 2]
                P.dma("sp", x[:], k.X.ap[t0:t0 + 128, :], x, k.X)
            x = xt[state["n"] % 2]
            tt = tmp[state["t"] % 2]
            state["t"] += 1
            P.op("dve", lambda e: e.tensor_tensor(tt[:], pp[:, 0:512], gate[:, c0:c0 + 512], ALU.mult), [pp, gate], [tt])
            P.op("pool", lambda e: e.tensor_tensor(x[:, c0:c0 + 512], x[:, c0:c0 + 512], tt[:], ALU.add), [x, tt], [x])
            if c0 == D - 512:
                P.dma("pool", k.X.ap[t0:t0 + 128, :], x[:], k.X, x)
        passes = [[("tm", c, 512, None) for c in range(0, D, 512)]]
        gemm(k, W, passes, cfg.groups(with_ctx), prov, epi)


def phase_final(k, out):
    P, cfg = k.P, k.cfg
    with P.scope():
        xt = [P.sbuf("fin_x%d" % i, [128, D], F32) for i in range(2)]
        sq = P.sbuf("fin_sq", [128, D], BF16)
        st = [P.sbuf("fin_st%d" % i, [128, 4], F32) for i in range(2)]
        ot = [P.sbuf("fin_o%d" % i, [128, D], F32) for i in range(2)]
        nf = P.sbuf("fin_nf", [128, D], F32)
        P.dma("sp", nf[:], bc_rows(k.I["norm_final"].ap[0:1, :]), nf, k.I["norm_final"])
        n = 0
        for b in range(cfg.NB):
            for t in range(cfg.S // 128):
                i = n % 2
                n += 1
                t0 = b * cfg.TB + t * 128
                x, s, o = xt[i], st[i], ot[i]
                P.dma("sp", x[:], k.X.ap[t0:t0 + 128, :], x, k.X)
                P.op("act", lambda e: e.activation(sq[:], x[:], AF.Square, accum_out=s[:, 0:1]), [x], [sq, s])
                P.op("dve", lambda e: e.tensor_scalar(s[:, 1:2], s[:, 0:1], 1.0 / D, EPS, ALU.mult, ALU.add), [s], [s])
                P.op("act", lambda e: e.activation(s[:, 2:3], s[:, 1:2], AF.Sqrt), [s], [s])
                P.op("dve", lambda e: e.reciprocal(s[:, 3:4], s[:, 2:3]), [s], [s])
                P.op("dve", lambda e: e.scalar_tensor_tensor(o[:], x[:], s[:, 3:4], nf[:], ALU.mult, ALU.mult), [x, s, nf], [o])
                P.dma("pool", out.ap[b * cfg.S + t * 128:b * cfg.S + (t + 1) * 128, :], o[:], out, o)

def _win(k, W, passes, groups, pfm_row, ptm_col, scale_fn):
    P = k.P
    with P.scope():
        prov = NormProvider(k, 1)
        ev = Evac(k)

        def epi(grp, sub, chunk, pp):
            tok0, ntok, b, is_ctx = grp
            kind, c0, ncols, tag = chunk
            if kind == "fm":
                r0 = pfm_row(c0)
                ev.put(pp, pp[0:ncols, 0:ntok], k.PFM, k.PFM.ap[r0:r0 + ncols, tok0:tok0 + ntok], scale_fn(c0))
            else:
                t0 = tok0 + sub * 128
                pc = ptm_col(c0)
                ev.put(pp, pp[:, 0:ncols], k.PTM, k.PTM.ap[t0:t0 + 128, pc:pc + ncols])
        gemm(k, W, passes, groups, prov, epi, wcols=2080)


def phase_win_even(k, l, ie):
    W = k.I["w_in_even"].ap[ie]
    pa = [("fm", c, 128, None) for c in range(0, 2048, 128)]
    pb = [("tm", c, 512, None) for c in range(2048, 4096, 512)]
    pc = [("tm", c, 512, None) for c in range(4096, 6144, 512)] + [("fm", 6144, 32, None)]
    _win(k, W, [pa, pb, pc], k.cfg.groups(True),
         lambda c: c if c < 2048 else 2048, lambda c: c - 2048,
         lambda c: (128.0 ** -0.5) if c < 1024 else None)


def phase_win_odd(k, l, io, with_ctx):
    W = k.I["w_in_odd"].ap[io]
    passes = [[("fm", c, 128, None) for c in range(p0, p0 + 2048, 128)] for p0 in (0, 2048, 4096)]
    _win(k, W, passes, k.cfg.groups(with_ctx), lambda c: c, lambda c: c, lambda c: None)

def phase_moe(k, l, with_ctx):
    P, cfg, I = k.P, k.cfg, k.I
    E, NG, EPG = cfg.E, cfg.NG, cfg.EPG
    NR = NG + E
    groups = cfg.groups(with_ctx)
    tiles = []
    for (tok0, ntok, b, is_ctx) in groups:
        for sub in range(ntok // 128):
            tiles.append((tok0 + sub * 128, b, is_ctx))
    ntl = len(tiles)
    nblk = cfg.nblk
    with P.scope():
        GATE = P.sbuf("m_GATE", [128, ntl, 2], F32)
        SLOT = P.sbuf("m_SLOT", [128, ntl, 2], I32)
        IDXW = P.sbuf("m_IDXW", [128, nblk], I32)
        Ccnt = P.sbuf("m_C", [128, E], F32)
        P.op("dve", lambda e: e.memset(Ccnt[:], 0.0), [], [Ccnt])
        routing = contextlib.ExitStack()
        routing.enter_context(P.scope())
        OH = P.sbuf("m_OH", [128, ntl, 2, E], BF16)
        RANK = P.sbuf("m_RANK", [128, ntl, E], F32)
        with P.scope():
            wr = P.sbuf("m_wr", [128, KC, NR], F32)
            P.dma("sp", wr[:], I["w_router"].ap[l].rearrange("(k p) n -> p k n", p=128), wr, I["w_router"])
            br = P.sbuf("m_br", [128, NR], F32)
            P.dma("sp", br[:], bc_rows(I["b_router"].ap[l:l + 1, :]), br, I["b_router"])
            lstr = P.sbuf("m_lstr", [128, 128], F32)
            lstrb = P.sbuf("m_lstrb", [128, 128], BF16)
            onesb = P.sbuf("m_onesb", [128, 128], BF16)
            P.dma("sp", lstr[:], I["lstrict"].ap[:], lstr, I["lstrict"])
            P.op("dve", lambda e: e.tensor_copy(lstrb[:], lstr[:]), [lstr], [lstrb])
            P.op("dve", lambda e: e.memset(onesb[:], 1.0), [], [onesb])
            hTf = P.sbuf("m_hTf", [128, KC, 128], F32)
            pTf = [P.psum("m_pTf%d" % i, [128, 4, 128], F32) for i in range(2)]
            plg = P.psum("m_plg", [128, 64], F32)
            prk = P.psum("m_prk", [128, 2, 64], F32)
            sm = [P.sbuf("m_sm%d" % i, [128, 256], F32) for i in range(2)]
            st = {"i": 0, "ti": 0}

            def on_tile(t0, hf, hb):
                ti = st["ti"]
                st["ti"] += 1
                s = sm[ti % 2]
                P.dma("pool", k.H2.ap[t0:t0 + 128, :], hb[:], k.H2, hb)
                for q in range(4):
                    pT = pTf[q % 2]
                    for kk in range(4):
                        kq = q * 4 + kk
                        P.op("pe", lambda e: e.transpose(pT[:, kk, :], hf[:, kq * 128:(kq + 1) * 128], k.identf[:]), [hf, k.identf], [pT], pe_acc=True)
                    if q % 2 == 0:
                        P.op("dve", lambda e: e.tensor_copy(hTf[:, q * 4:(q + 1) * 4, :], pT[:]), [pT], [hTf])
                    else:
                        P.op("act", lambda e: e.copy(hTf[:, q * 4:(q + 1) * 4, :], pT[:]), [pT], [hTf])
                for kk in range(KC):
                    P.op("pe", lambda e: e.matmul(plg[:, 0:NR], hTf[:, kk, :], wr[:, kk, :], start=(kk == 0), stop=(kk == KC - 1)), [hTf, wr], [plg], pe_acc=True)
                lg = s[:, 0:NR]
                P.op("dve", lambda e: e.tensor_tensor(lg, plg[:, 0:NR], br[:], ALU.add), [plg, br], [s])
                c = 64
                P.op("dve", lambda e: e.tensor_reduce(s[:, c:c + 1], s[:, 0:NG], AX.X, ALU.max), [s], [s])
                P.op("dve", lambda e: e.tensor_scalar(s[:, c + 1:c + 2], s[:, c:c + 1], -1.0, None, ALU.mult), [s], [s])
                P.op("dve", lambda e: e.tensor_scalar(s[:, 72:72 + NG], s[:, 0:NG], s[:, c:c + 1], None, ALU.is_equal), [s], [s])
                P.op("act", lambda e: e.activation(s[:, 80:80 + NG], s[:, 0:NG], AF.Exp, bias=s[:, c + 1:c + 2], scale=1.0, accum_out=s[:, c + 2:c + 3]), [s], [s])
                P.op("dve", lambda e: e.reciprocal(s[:, c + 3:c + 4], s[:, c + 2:c + 3]), [s], [s])
                P.op("dve", lambda e: e.tensor_scalar(s[:, 88:88 + NG], s[:, 72:72 + NG], -1.0, 1e30, ALU.add, ALU.mult), [s], [s])
                lem = s[:, 96:96 + E]
                P.op("dve", lambda e: e.tensor_tensor(lem.rearrange("p (g j) -> p g j", g=NG), s[:, NG:NG + E].rearrange("p (g j) -> p g j", g=NG),
                                                      s[:, 88:88 + NG].unsqueeze(2).to_broadcast([128, NG, EPG]), ALU.add), [s], [s])
                P.op("dve", lambda e: e.max(s[:, 136:144], lem), [s], [s])
                oh = OH[:, ti, :, :]
                P.op("dve", lambda e: e.tensor_scalar(OH[:, ti, 0, :], lem, s[:, 136:137], None, ALU.is_equal), [s], [OH])
                P.op("dve", lambda e: e.tensor_scalar(OH[:, ti, 1, :], lem, s[:, 137:138], None, ALU.is_equal), [s], [OH])
                P.op("dve", lambda e: e.tensor_tensor(s[:, c + 4:c + 5], s[:, 137:138], s[:, 136:137], ALU.subtract), [s], [s])
                P.op("act", lambda e: e.activation(s[:, c + 5:c + 6], s[:, c + 4:c + 5], AF.Exp), [s], [s])
                P.op("dve", lambda e: e.tensor_scalar(s[:, c + 5:c + 6], s[:, c + 5:c + 6], 1.0, None, ALU.add), [s], [s])
                P.op("dve", lambda e: e.reciprocal(s[:, c + 6:c + 7], s[:, c + 5:c + 6]), [s], [s])
                P.op("dve", lambda e: e.tensor_tensor(GATE[:, ti, 0:1], s[:, c + 6:c + 7], s[:, c + 3:c + 4], ALU.mult), [s], [GATE])
                P.op("dve", lambda e: e.tensor_tensor(GATE[:, ti, 1:2], s[:, c + 3:c + 4], GATE[:, ti, 0:1], ALU.subtract), [s, GATE], [GATE])
                A = sm_A[ti % 2]
                P.op("dve", lambda e: e.tensor_tensor(A[:], OH[:, ti, 0, :], OH[:, ti, 1, :], ALU.add), [OH], [A])
                P.op("pe", lambda e: e.matmul(prk[:, 0, 0:E], lstrb[:], A[:], start=True, stop=True), [lstrb, A], [prk], pe_acc=True)
                P.op("pe", lambda e: e.matmul(prk[:, 1, 0:E], onesb[:], A[:], start=True, stop=True), [onesb, A], [prk], pe_acc=True)
                P.op("dve", lambda e: e.tensor_tensor(RANK[:, ti, :], prk[:, 0, 0:E], Ccnt[:], ALU.add), [prk, Ccnt], [RANK])
                P.op("dve", lambda e: e.tensor_tensor(Ccnt[:], prk[:, 1, 0:E], Ccnt[:], ALU.add), [prk, Ccnt], [Ccnt])
            sm_A = [P.sbuf("m_A%d" % i, [128, E], BF16) for i in range(2)]
            prov = NormProvider(k, 2, on_tile=on_tile, transpose=False)
            for grp in groups:
                prov.get(grp)
        with P.scope():
            w = P.sbuf("m_w", [128, 8, E], F32)
            P.op("dve", lambda e: e.tensor_scalar(w[:, 0, :], Ccnt[:], 1.0 / 128.0, 63.5 / 128.0, ALU.mult, ALU.add), [Ccnt], [w])
            P.op("dve", lambda e: e.tensor_scalar(w[:, 5, :], w[:, 0, :], 8388608.0, None, ALU.add), [w], [w])
            P.op("dve", lambda e: e.tensor_scalar(w[:, 6, :], w[:, 5, :], -8388608.0, 128.0, ALU.add, ALU.mult), [w], [w])
            P.op("dve", lambda e: e.tensor_copy(w[:, 1, :], w[:, 6, :]), [w], [w])
            P.op("dve", lambda e: e.memset(w[:, 2, :], 1.0), [], [w])
            P.op("dve", lambda e: e.tensor_tensor_scan(w[:, 3, :], w[:, 2, :], w[:, 1, :], 0.0, ALU.mult, ALU.add), [w], [w])
            P.op("dve", lambda e: e.tensor_tensor(w[:, 4, :], w[:, 3, :], w[:, 1, :], ALU.subtract), [w], [w])
            tmp = P.sbuf("m_tmp", [128, 2, E], F32)
            sl = P.sbuf("m_sl", [128, ntl, 2], F32)
            for ti in range(ntl):
                P.op("dve", lambda e: e.tensor_tensor(tmp[:, 0, :], RANK[:, ti, :], w[:, 4, :], ALU.add), [RANK, w], [tmp])
                for j in range(2):
                    P.op("dve", lambda e: e.tensor_tensor(tmp[:, 1, :], tmp[:, 0, :], OH[:, ti, j, :], ALU.mult), [tmp, OH], [tmp])
                    P.op("dve", lambda e: e.tensor_reduce(sl[:, ti, j:j + 1], tmp[:, 1, :], AX.X, ALU.add), [tmp], [sl])
            P.op("dve", lambda e: e.tensor_copy(SLOT[:], sl[:]), [sl], [SLOT])
            bs = P.sbuf("m_bs", [128, nblk], F32)
            be = P.sbuf("m_be", [128, nblk], F32)
            be2 = P.sbuf("m_be2", [128, nblk], F32)
            ip = P.sbuf("m_ip", [128, 1], F32)
            P.dma("sp", bs[:], bc_rows(I["blkstart"].ap[0:1, :]), bs, I["blkstart"])
            P.dma("sp", ip[:], I["iota_p"].ap[:], ip, I["iota_p"])
            P.op("dve", lambda e: e.memset(be[:], 0.0), [], [be])
            for ex in range(E):
                P.op("dve", lambda e: e.scalar_tensor_tensor(be[:], bs[:], w[:, 3, ex:ex + 1], be[:], ALU.is_ge, ALU.add), [bs, w, be], [be])
            P.op("dve", lambda e: e.tensor_scalar(be[:], be[:], float(E - 1), None, ALU.min), [be], [be])
            P.op("dve", lambda e: e.memset(be2[:, 0:2], 1.0), [], [be2])
            P.op("dve", lambda e: e.tensor_tensor(be2[:, 2:nblk], be[:, 2:nblk], be[:, 0:nblk - 2], ALU.not_equal), [be], [be2])
            P.op("dve", lambda e: e.tensor_scalar(be[:], be[:], 128.0, ip[:, 0:1], ALU.mult, ALU.add), [be, ip], [be])
            P.op("dve", lambda e: e.tensor_scalar(be2[:], be2[:], -BIGIDX, BIGIDX + float(l * E * 128), ALU.mult, ALU.add), [be2], [be2])
            P.op("dve", lambda e: e.tensor_tensor(be[:], be[:], be2[:], ALU.add), [be, be2], [be])
            P.op("dve", lambda e: e.tensor_copy(IDXW[:], be[:]), [be], [IDXW])
            hb = [P.sbuf("m_hb%d" % i, [128, D], BF16) for i in range(3)]
            for ti, (t0, b, is_ctx) in enumerate(tiles):
                h = hb[ti % 3]
                P.dma("sp", h[:], k.H2.ap[t0:t0 + 128, :], h, k.H2)
                for j in range(2):
                    P.dma_raw("pool", lambda e: e.indirect_dma_start(out=k.XS.ap[:, :], out_offset=bass.IndirectOffsetOnAxis(ap=SLOT[:, ti, j:j + 1], axis=0), in_=h[:], in_offset=None),
                              k.XS, [h, SLOT])
        routing.close()
        with P.scope():
            w1bb = [P.sbuf("m_w1b%d" % i, [128, KC * 512], BF16) for i in range(2)]
            w3bb = [P.sbuf("m_w3b%d" % i, [128, KC * 512], BF16) for i in range(2)]
            w2bb = [P.sbuf("m_w2b%d" % i, [128, 4 * D], BF16) for i in range(2)]
            xs = [P.sbuf("m_xs%d" % i, [128, D], BF16) for i in range(2)]
            xT = [P.sbuf("m_xT%d" % i, [128, KC, 128], BF16) for i in range(2)]
            pT = [P.psum("m_pT%d" % i, [128, 8, 128], BF16) for i in range(2)]
            ph = [P.psum("m_ph%d" % i, [128, 512], F32) for i in range(2)]
            py = [P.psum("m_py%d" % i, [128, 512], F32) for i in range(2)]
            paT = P.psum("m_paT", [128, 4, 128], BF16)
            sil = [P.sbuf("m_sil%d" % i, [128, 512], F32) for i in range(2)]
            ab = [P.sbuf("m_ab%d" % i, [128, 512], BF16) for i in range(2)]
            aT = [P.sbuf("m_aT%d" % i, [128, 4, 128], BF16) for i in range(2)]
            yo = [P.sbuf("m_yo%d" % i, [128, D], BF16) for i in range(2)]
            nrow = E * 128
            breg = k.nc.gpsimd.alloc_register()
            k.nc.gpsimd.reg_mov(breg, (l + 1) * nrow - 1)
            for blk in range(nblk):
                i = blk % 2
                w1b, w3b, w2b = w1bb[i], w3bb[i], w2bb[i]
                for (dst, src) in ((w1b, I["w1h"]), (w3b, I["w3h"]), (w2b, I["w2h"])):
                    P.dma_raw("pool", lambda e: e.indirect_dma_start(out=dst[:], out_offset=None, in_=src.ap[:, :], in_offset=bass.IndirectOffsetOnAxis(ap=IDXW[:, blk:blk + 1], axis=0),
                                                                   bounds_check=breg, oob_is_err=False), dst, [IDXW, src])
                x = xs[i]
                P.dma("sp", x[:], k.XS.ap[blk * 128:(blk + 1) * 128, :], x, k.XS)
                for half in range(2):
                    for kk in range(8):
                        kq = half * 8 + kk
                        P.op("pe", lambda e: e.transpose(pT[half][:, kk, :], x[:, kq * 128:(kq + 1) * 128], k.identb[:]), [x, k.identb], [pT[half]], pe_acc=True)
                    if half == 0:
                        P.op("dve", lambda e: e.tensor_copy(xT[i][:, 0:8, :], pT[0][:]), [pT[0]], [xT[i]])
                    else:
                        P.op("act", lambda e: e.copy(xT[i][:, 8:16, :], pT[1][:]), [pT[1]], [xT[i]])
                for kk in range(KC):
                    P.op("pe", lambda e: e.matmul(ph[0][:], xT[i][:, kk, :], w1b[:, kk * 512:(kk + 1) * 512], start=(kk == 0), stop=(kk == KC - 1)), [xT[i], w1b], [ph[0]], pe_acc=True)
                for kk in range(KC):
                    P.op("pe", lambda e: e.matmul(ph[1][:], xT[i][:, kk, :], w3b[:, kk * 512:(kk + 1) * 512], start=(kk == 0), stop=(kk == KC - 1)), [xT[i], w3b], [ph[1]], pe_acc=True)
                P.op("act", lambda e: e.activation(sil[i][:], ph[0][:], AF.Silu), [ph[0]], [sil[i]])
                P.op("dve", lambda e: e.tensor_tensor(ab[i][:], ph[1][:], sil[i][:], ALU.mult), [ph[1], sil[i]], [ab[i]])
                for kk in range(4):
                    P.op("pe", lambda e: e.transpose(paT[:, kk, :], ab[i][:, kk * 128:(kk + 1) * 128], k.identb[:]), [ab[i], k.identb], [paT], pe_acc=True)
                P.op("dve", lambda e: e.tensor_copy(aT[i][:], paT[:]), [paT], [aT[i]])
                for cc in range(4):
                    pp = py[cc % 2]
                    for kk in range(4):
                        P.op("pe", lambda e: e.matmul(pp[:], aT[i][:, kk, :], w2b[:, kk * D + cc * 512:kk * D + (cc + 1) * 512], start=(kk == 0), stop=(kk == 3)), [aT[i], w2b], [pp], pe_acc=True)
                    if cc % 2 == 0:
                        P.op("act", lambda e: e.copy(yo[i][:, cc * 512:(cc + 1) * 512], pp[:]), [pp], [yo[i]])
                    else:
                        P.op("dve", lambda e: e.tensor_copy(yo[i][:, cc * 512:(cc + 1) * 512], pp[:]), [pp], [yo[i]])
                P.dma("sp", k.YS.ap[blk * 128:(blk + 1) * 128, :], yo[i][:], k.YS, yo[i])
        with P.scope():
            ya = [P.sbuf("m_ya%d" % i, [128, D], BF16) for i in range(2)]
            yb = [P.sbuf("m_yb%d" % i, [128, D], BF16) for i in range(2)]
            xt = [P.sbuf("m_x%d" % i, [128, D], F32) for i in range(2)]
            f = [P.sbuf("m_f%d" % i, [128, D], F32) for i in range(2)]
            gate = P.sbuf("m_gate", [128, D], F32)
            cur = None
            for ti, (t0, b, is_ctx) in enumerate(tiles):
                i = ti % 2
                r = cfg.NB if is_ctx else b
                if cur != r:
                    cur = r
                    P.dma("sp", gate[:], bc_rows(k.MOD.ap[r:r + 1, 5 * D:6 * D]), gate, k.MOD)
                for (dst, j) in ((ya[i], 0), (yb[i], 1)):
                    P.dma_raw("pool", lambda e: e.indirect_dma_start(out=dst[:], out_offset=None, in_=k.YS.ap[:, :], in_offset=bass.IndirectOffsetOnAxis(ap=SLOT[:, ti, j:j + 1], axis=0)),
                              dst, [SLOT, k.YS])
                P.dma("sp", xt[i][:], k.X.ap[t0:t0 + 128, :], xt[i], k.X)
                P.op("dve", lambda e: e.tensor_scalar(f[i][:], ya[i][:], GATE[:, ti, 0:1], None, ALU.mult), [ya[i], GATE], [f[i]])
                P.op("dve", lambda e: e.scalar_tensor_tensor(f[i][:], yb[i][:], GATE[:, ti, 1:2], f[i][:], ALU.mult, ALU.add), [yb[i], GATE, f[i]], [f[i]])
                P.op("pool", lambda e: e.tensor_tensor(f[i][:], f[i][:], gate[:], ALU.mult), [f[i], gate], [f[i]])
                P.op("pool", lambda e: e.tensor_tensor(xt[i][:], xt[i][:], f[i][:], ALU.add), [xt[i], f[i]], [xt[i]])
                P.dma("sp", k.X.ap[t0:t0 + 128, :], xt[i][:], k.X, xt[i])

TWO_PI = 2.0 * math.pi
MAGIC = 12582912.0


def hyena_filters(k, io, seg, L, HGbuf):
    P, I = k.P, k.I
    CH = min(512, L)
    nch = L // CH
    with P.scope():
        w1 = P.sbuf("hf_w1", [33, 64], F32); w2 = P.sbuf("hf_w2", [64, 64], F32); w3 = P.sbuf("hf_w3", [64, 64], F32)
        w4 = P.sbuf("hf_w4", [64, 4096], F32)
        bb = P.sbuf("hf_bb", [64, 3], F32)
        P.dma("sp", w1[:], I["hy_w1"].ap[io], w1, I["hy_w1"]); P.dma("sp", w2[:], I["hy_w2"].ap[io], w2, I["hy_w2"])
        P.dma("sp", w3[:], I["hy_w3"].ap[io], w3, I["hy_w3"]); P.dma("sp", w4[:], I["hy_w4"].ap[io], w4, I["hy_w4"])
        for j, nm in enumerate(("hy_b1", "hy_b2", "hy_b3")):
            P.dma("sp", bb[:, j:j + 1], I[nm].ap[io], bb, I[nm])
        a3 = P.sbuf("hf_a3", [64, 2, L], BF16)
        w4b = P.sbuf("hf_w4b", [64, 4096], BF16)
        P.op("act", lambda e: e.copy(w4b[:], w4[:]), [w4], [w4b])
        embc = [P.sbuf("hf_emb%d" % i, [33, 512], F32) for i in range(2)]
        act_ = [P.sbuf("hf_act%d" % i, [64, 512], F32) for i in range(2)]
        ps = [P.psum("hf_ps%d" % i, [128, 512], F32) for i in range(2)]
        tmp = [P.sbuf("hf_tmp%d" % i, [64, 2, 512], F32) for i in range(2)]
        n = 0
        ne = 0
        for d in range(2):
            for c in range(nch):
                em = embc[ne % 2]; ne += 1
                P.dma("sp", em[:, 0:CH], I["hy_embT"].ap[seg, d, :, c * CH:(c + 1) * CH], em, I["hy_embT"])
                for layer in range(3):
                    wl = (w1, w2, w3)[layer]
                    srcb = em if layer == 0 else act_[(layer - 1) % 2]
                    src = srcb[:, 0:CH]
                    pp = ps[n % 2]; t = tmp[n % 2]; n += 1
                    P.op("pe", lambda e: e.matmul(pp[0:64, 0:CH], wl[:], src, start=True, stop=True), [wl, srcb], [pp], pe_acc=True)
                    x = t[:, 0, 0:CH]; q = t[:, 1, 0:CH]
                    P.op("dve", lambda e: e.tensor_scalar(x, pp[0:64, 0:CH], bb[:, layer:layer + 1], None, ALU.add), [pp, bb], [t])
                    P.op("dve", lambda e: e.tensor_scalar(q, x, 1.0 / TWO_PI, MAGIC, ALU.mult, ALU.add), [t], [t])
                    P.op("dve", lambda e: e.tensor_scalar(q, q, -MAGIC, None, ALU.add), [t], [t])
                    P.op("dve", lambda e: e.scalar_tensor_tensor(x, q, -TWO_PI, x, ALU.mult, ALU.add), [t], [t])
                    P.op("dve", lambda e: e.tensor_scalar(x, x, math.pi, -math.pi, ALU.min, ALU.max), [t], [t])
                    if layer < 2:
                        dstb = act_[layer % 2]
                        P.op("act", lambda e: e.activation(dstb[:, 0:CH], x, AF.Sin), [t], [dstb])
                    else:
                        P.op("act", lambda e: e.activation(a3[:, d, c * CH:(c + 1) * CH], x, AF.Sin), [t], [a3])
        dl = P.sbuf("hf_dl", [128, 8], F32)
        P.dma("sp", dl[:], I["hy_delta"].ap.rearrange("(c p) o -> p (c o)", p=128), dl, I["hy_delta"], allow_slow_non_contiguous=True)
        ndl = P.sbuf("hf_ndl", [128, 8], F32)
        P.op("dve", lambda e: e.tensor_scalar(ndl[:], dl[:], -1.0, None, ALU.mult), [dl], [ndl])
        hb = P.sbuf("hf_bias", [128, 2, 8], F32)
        P.dma("sp", hb[:], I["hy_bias"].ap[io].rearrange("n (c p) o -> p n (c o)", p=128), hb, I["hy_bias"], allow_slow_non_contiguous=True)
        dec = [P.sbuf("hf_dec%d" % i, [128, 512], F32) for i in range(2)]
        tbc = [P.sbuf("hf_tbc%d" % i, [128, 512], F32) for i in range(2)]
        rw = P.sbuf("hf_row", [128, 2 * L], F32)
        rb = P.sbuf("hf_rowb", [128, 2 * L], BF16)
        part = P.sbuf("hf_part", [128, 8], F32)
        b0 = P.sbuf("hf_b0", [128, 4], F32)
        P.op("dve", lambda e: e.memset(b0[:], 0.0), [], [b0])
        for order in range(2):
            for cc in range(8):
                P.op("pool", lambda e: e.memset(rw[:, 2 * L - 1:2 * L], 0.0), [], [rw])
                for half in range(2):
                    fdir = half if order == 0 else 1 - half
                    tord = 1 if half == 0 else 0
                    col0 = order * 2048 + fdir * 1024 + cc * 128
                    for c in range(nch):
                        pp = ps[n % 2]; dc = dec[n % 2]; tb = tbc[n % 2]; n += 1
                        P.dma("sp", tb[:, 0:CH], bc_rows(I["hy_t"].ap[seg, tord:tord + 1, c * CH:(c + 1) * CH]), tb, I["hy_t"])
                        P.op("pe", lambda e: e.matmul(pp[:, 0:CH], w4b[:, col0:col0 + 128], a3[:, tord, c * CH:(c + 1) * CH], start=True, stop=True), [w4b, a3], [pp], pe_acc=True)
                        P.op("act", lambda e: e.activation(dc[:, 0:CH], tb[:, 0:CH], AF.Exp, scale=ndl[:, cc:cc + 1]), [tb, ndl], [dc])
                        if half == 0:
                            P.op("dve", lambda e: e.tensor_tensor(rw[:, c * CH:(c + 1) * CH], pp[:, 0:CH], dc[:, 0:CH], ALU.mult), [pp, dc], [rw])
                        elif c == 0:
                            P.op("dve", lambda e: e.tensor_tensor(b0[:, 0:1], pp[:, 0:1], dc[:, 0:1], ALU.mult), [pp, dc], [b0])
                            P.op("dve", lambda e: e.tensor_tensor(rw[:, L:L + CH - 1], pp[:, 1:CH], dc[:, 1:CH], ALU.mult), [pp, dc], [rw])
                        else:
                            P.op("dve", lambda e: e.tensor_tensor(rw[:, L + c * CH - 1:L + (c + 1) * CH - 1], pp[:, 0:CH], dc[:, 0:CH], ALU.mult), [pp, dc], [rw])
                P.op("dve", lambda e: e.tensor_reduce(part[:, 0:1], rw[:, 0:2 * L - 1], AX.X, ALU.add, apply_absolute_value=True), [rw], [part])
                P.op("dve", lambda e: e.tensor_reduce(part[:, 1:2], b0[:, 0:2], AX.X, ALU.add, apply_absolute_value=True), [b0], [part])
                P.op("dve", lambda e: e.tensor_tensor(part[:, 2:3], part[:, 0:1], part[:, 1:2], ALU.add), [part], [part])
                P.op("dve", lambda e: e.tensor_scalar(part[:, 2:3], part[:, 2:3], EPS, None, ALU.add), [part], [part])
                P.op("dve", lambda e: e.reciprocal(part[:, 3:4], part[:, 2:3]), [part], [part])
                P.op("dve", lambda e: e.tensor_tensor(rw[:, L - 1:L], rw[:, L - 1:L], b0[:, 0:1], ALU.add), [rw, b0], [rw])
                P.op("dve", lambda e: e.tensor_scalar(rb[:], rw[:], part[:, 3:4], None, ALU.mult), [rw, part], [rb])
                P.op("dve", lambda e: e.scalar_tensor_tensor(rb[:, L - 1:L], rw[:, L - 1:L], part[:, 3:4], hb[:, order, cc:cc + 1], ALU.mult, ALU.add), [rw, part, hb], [rb])
                P.dma("pool", HGbuf.ap[order, cc * 128:(cc + 1) * 128, :], rb[:], HGbuf, rb)


def short_conv_fm(k, io, with_ctx):
    P, cfg, I = k.P, k.cfg, k.I
    PC = 2048
    segs = []
    for b in range(cfg.NB):
        segs.append((b * cfg.TB, cfg.S))
        if with_ctx:
            segs.append((b * cfg.TB + cfg.S, cfg.LC))
    pieces = []
    for (t0, L) in segs:
        for p0 in range(0, L, PC):
            pl = min(PC, L - p0)
            pieces.append((t0, L, p0, pl))
    with P.scope():
        hs = P.sbuf("sc_hs", [128, 24, 3], F32)
        P.dma("sp", hs[:], I["hy_short"].ap[io].rearrange("(c p) j -> p c j", p=128), hs, I["hy_short"])
        scw = P.sbuf("sc_w", [128, 8, 3], F32)
        P.dma("sp", scw[:], I["sc_conv"].ap[io].rearrange("(c p) j -> p c j", p=128), scw, I["sc_conv"])
        xin = [P.sbuf("sc_x%d" % i, [128, PC + 2], BF16) for i in range(3)]
        acc = [P.sbuf("sc_a%d" % i, [128, PC], F32) for i in range(2)]
        ob = [P.sbuf("sc_o%d" % i, [128, PC], BF16) for i in range(2)]
        bg = [P.sbuf("sc_b%d" % i, [128, PC], BF16) for i in range(2)]
        cx = [P.sbuf("sc_cx%d" % i, [128, PC + 2], F32) for i in range(2)]
        n = 0

        def conv3(x, xb, w, ci, pl, a, ab):
            P.op("dve", lambda e: e.tensor_scalar(a[:, 0:pl], x[:, 1:pl + 1], w[:, ci, 1:2], None, ALU.mult), [xb, w], [ab])
            P.op("dve", lambda e: e.scalar_tensor_tensor(a[:, 0:pl], x[:, 0:pl], w[:, ci, 0:1], a[:, 0:pl], ALU.mult, ALU.add), [xb, w, ab], [ab])
            P.op("dve", lambda e: e.scalar_tensor_tensor(a[:, 0:pl], x[:, 2:pl + 2], w[:, ci, 2:3], a[:, 0:pl], ALU.mult, ALU.add), [xb, w, ab], [ab])

        def load_halo(xt, row0, t0, L, p0, pl):
            lo = max(p0 - 1, 0); hi = min(p0 + pl + 1, L)
            if p0 == 0:
                P.op("pool", lambda e: e.memset(xt[:, 0:1], 0.0), [], [xt])
            if p0 + pl == L:
                P.op("pool", lambda e: e.memset(xt[:, pl + 1:pl + 2], 0.0), [], [xt])
            P.dma("sp", xt[:, lo - p0 + 1:hi - p0 + 1], k.PFM.ap[row0:row0 + 128, t0 + lo:t0 + hi], xt, k.PFM)
        for ci in range(24):
            for (t0, L, p0, pl) in pieces:
                xt = xin[n % 3]; a = acc[n % 2]; o = ob[n % 2]; n += 1
                load_halo(xt, ci * 128, t0, L, p0, pl)
                conv3(xt, xt, hs, ci, pl, a, a)
                P.op("act", lambda e: e.copy(o[:, 0:pl], a[:, 0:pl]), [a], [o])
                P.dma("pool", k.PFC.ap[ci * 128:(ci + 1) * 128, t0 + p0:t0 + p0 + pl], o[:, 0:pl], k.PFC, o)
        for ci in range(8):
            for (t0, L, p0, pl) in pieces:
                xc = xin[n % 3]; xx = xin[(n + 1) % 3]; a = acc[n % 2]; o = ob[n % 2]; bgt = bg[n % 2]; cxt = cx[n % 2]; n += 2
                load_halo(xc, 3072 + 1024 + ci * 128, t0, L, p0, pl)
                load_halo(xx, 3072 + 2048 + ci * 128, t0, L, p0, pl)
                P.dma("sp", bgt[:, 0:pl], k.PFM.ap[3072 + ci * 128:3072 + (ci + 1) * 128, t0 + p0:t0 + p0 + pl], bgt, k.PFM)
                P.op("pool", lambda e: e.tensor_tensor(cxt[:, 0:pl + 2], xc[:, 0:pl + 2], xx[:, 0:pl + 2], ALU.mult), [xc, xx], [cxt])
                conv3(cxt, cxt, scw, ci, pl, a, a)
                P.op("dve", lambda e: e.tensor_tensor(o[:, 0:pl], a[:, 0:pl], bgt[:, 0:pl], ALU.mult), [a, bgt], [o])
                P.dma("pool", k.YFM.ap[1024 + ci * 128:1024 + (ci + 1) * 128, t0 + p0:t0 + p0 + pl], o[:, 0:pl], k.YFM, o)


def hyena_conv(k, io, seg_ctx, L, HGbuf):
    P, cfg, I = k.P, k.cfg, k.I
    NB = cfg.NB
    n2 = L // 128
    NC = n2 * NB
    dmax = n2 - 1
    TW = 128 * (2 * n2 - 1)
    QC = 32
    tokbase = [b * cfg.TB + (cfg.S if seg_ctx else 0) for b in range(NB)]
    with P.scope():
        anti = P.sbuf("hc_antif", [128, 128], F32); antib = P.sbuf("hc_antib", [128, 128], BF16)
        P.dma("sp", anti[:], I["antiid"].ap[:], anti, I["antiid"])
        P.op("dve", lambda e: e.tensor_copy(antib[:], anti[:]), [anti], [antib])
        Vt = P.sbuf("hc_Vt", [128, QC, NC], BF16)
        G1t = P.sbuf("hc_G1t", [128, QC, NC], BF16)
        G2t = P.sbuf("hc_G2t", [128, QC, NC], BF16)
        Y2t = P.sbuf("hc_Y2t", [128, NC, QC], BF16)
        TT = [P.sbuf("hc_T%d" % i, [128, TW], BF16) for i in range(2)]
        z1 = [P.sbuf("hc_z1%d" % i, [128, NC], BF16) for i in range(2)]
        fm = [P.sbuf("hc_fm%d" % i, [QC, 1024], BF16) for i in range(3)]
        ptr = [P.psum("hc_ptr%d" % i, [128, 4, QC], BF16) for i in range(2)]
        py = [P.psum("hc_py%d" % i, [128, 512], F32) for i in range(2)]
        pob = [P.psum("hc_pob%d" % i, [QC, 512], BF16) for i in range(2)]
        ost = [P.sbuf("hc_ost%d" % i, [QC, 512], BF16) for i in range(2)]
        nf = nt = ntt = nz = no = 0
        PL = min(1024, L)
        for q in range(1024 // QC):
            c0 = q * QC
            for (dst, rbase, idm) in ((Vt, 0, k.identb), (G1t, 1024, antib), (G2t, 2048, k.identb)):
                for b in range(NB):
                    for p0 in range(0, L, PL):
                        f = fm[nf % 3]; nf += 1
                        P.dma("sp", f[:, 0:PL], k.PFC.ap[rbase + c0:rbase + c0 + QC, tokbase[b] + p0:tokbase[b] + p0 + PL], f, k.PFC)
                        for j0 in range(0, PL // 128, 4):
                            nj = min(4, PL // 128 - j0)
                            pt = ptr[nt % 2]; nt += 1
                            for j in range(nj):
                                P.op("pe", lambda e: e.transpose(pt[:, j, :], f[:, (j0 + j) * 128:(j0 + j + 1) * 128], k.identb[0:QC, 0:QC]), [f, k.identb], [pt], pe_acc=True)
                            s2 = p0 // 128 + j0
                            oap = dst[:].rearrange("p c (s b) -> p c s b", b=NB)[:, :, s2:s2 + nj, b]
                            iap = pt[:, 0:nj, :].rearrange("p j c -> p c j")
                            if nt % 2 == 0:
                                P.op("dve", lambda e: e.tensor_copy(oap, iap), [pt], [dst])
                            else:
                                P.op("act", lambda e: e.copy(oap, iap), [pt], [dst])
            g1flat = G1t[:].rearrange("p c n -> p (c n)")
            tot = QC * NC
            for o0 in range(0, tot, 512):
                w_ = min(512, tot - o0)
                pp = py[nz % 2]; nz += 1
                P.op("pe", lambda e: e.matmul(pp[:, 0:w_], antib[:], g1flat[:, o0:o0 + w_], start=True, stop=True), [antib, G1t], [pp], pe_acc=True)
                P.op("dve", lambda e: e.tensor_copy(g1flat[:, o0:o0 + w_], pp[:, 0:w_]), [pp], [G1t])
            for ci in range(QC):
                ch = c0 + ci
                for order in range(2):
                    T = TT[ntt % 2]; ntt += 1
                    srcap = bass.AP(HGbuf.handle, (order * 1024 + ch) * 2 * L, [[1, 128], [1, TW]])
                    P.dma("sp", T[:], srcap, T, HGbuf)
                    pp = py[nz % 2]; nz += 1
                    mov = Vt[:, ci, :] if order == 0 else z1[ci % 2][:]
                    movb = Vt if order == 0 else z1[ci % 2]
                    ds = [0] + [d for d in range(-dmax, dmax + 1) if d != 0]
                    for di, d in enumerate(ds):
                        blk = (dmax - d) if order == 0 else (dmax + d)
                        lo, hi = max(0, d), min(n2, n2 + d)
                        P.op("pe", lambda e: e.matmul(pp[:, lo * NB:hi * NB], T[:, blk * 128:(blk + 1) * 128], mov[:, (lo - d) * NB:(hi - d) * NB],
                                                      start=(di == 0), stop=(di == len(ds) - 1)), [T, movb], [pp], pe_acc=True)
                    if order == 0:
                        zz = z1[ci % 2]
                        P.op("dve", lambda e: e.tensor_tensor(zz[:], pp[:, 0:NC], G1t[:, ci, :], ALU.mult), [pp, G1t], [zz])
                    else:
                        P.op("dve", lambda e: e.tensor_tensor(Y2t[:, :, ci], pp[:, 0:NC], G2t[:, ci, :], ALU.mult), [pp, G2t], [Y2t])
            for b in range(NB):
                for s0 in range(0, n2, 4):
                    ns = min(4, n2 - s0)
                    po = pob[no % 2]; os_ = ost[no % 2]; no += 1
                    for j in range(ns):
                        P.op("pe", lambda e: e.transpose(po[:, j * 128:(j + 1) * 128], Y2t[:, (s0 + j) * NB + b, :], k.identb[:]), [Y2t, k.identb], [po], pe_acc=True)
                    P.op("act", lambda e: e.copy(os_[:, 0:ns * 128], po[:, 0:ns * 128]), [po], [os_])
                    P.dma("pool", k.YFM.ap[c0:c0 + QC, tokbase[b] + s0 * 128:tokbase[b] + (s0 + ns) * 128], os_[:, 0:ns * 128], k.YFM, os_)


def mixer_odd(k, l, io, with_ctx):
    cfg = k.cfg
    short_conv_fm(k, io, with_ctx)
    hyena_filters(k, io, 0, cfg.S, k.HG)
    hyena_conv(k, io, False, cfg.S, k.HG)
    if with_ctx:
        hyena_filters(k, io, 1, cfg.LC, k.HGC)
        hyena_conv(k, io, True, cfg.LC, k.HGC)


def host_odd(cfg, inp, m):
    S, LC = cfg.S, cfg.LC
    f = lambda a: np.ascontiguousarray(a, dtype=np.float32)
    NO = inp["w_in_odd"].shape[0]
    m["w_in_odd"] = f(inp["w_in_odd"]); m["w_out_odd"] = f(inp["w_out_odd"])
    m["hy_short"] = f(np.transpose(inp["hy_short"], (0, 2, 1)))
    m["sc_conv"] = f(np.transpose(inp["sc_conv"], (0, 2, 1)))
    for nm in ("hy_w1", "hy_w2", "hy_w3", "hy_w4"):
        m[nm] = f(inp[nm])
    for nm in ("hy_b1", "hy_b2", "hy_b3"):
        m[nm] = f(inp[nm][:, :, None])
    m["hy_bias"] = f(inp["hy_bias"][:, :, :, None])
    embT = np.zeros((2, 2, 33, S), np.float32)
    tt = np.zeros((2, 2, S), np.float32)
    for seg, L in enumerate((S, LC)):
        t = np.linspace(0.0, 1.0, L, dtype=np.float32)[:, None]
        bands = 16
        freqs = np.linspace(1e-4, bands - 1, bands, dtype=np.float32)[None, :]
        w = (np.float32(2.0 * math.pi / L) * np.arange(L, dtype=np.float32))[:, None]
        emb = np.concatenate([t, np.cos(freqs * w), -np.sin(freqs * w)], axis=-1).astype(np.float32)
        embT[seg, 0, :, :L] = emb.T
        embT[seg, 1, :, :L] = emb[::-1].T
        tt[seg, 0, :L] = t[:, 0]
        tt[seg, 1, :L] = t[::-1, 0]
    m["hy_embT"] = embT
    m["hy_t"] = tt
    deltas = np.abs(np.linspace(math.log(1e-2) / 1.5, math.log(1e-2) / 0.3, 1024, dtype=np.float32))
    m["hy_delta"] = f(deltas[:, None])
    m["antiid"] = f(np.eye(128)[::-1])

def mixer_na(k, ie, ctx_out):
    P, cfg, I = k.P, k.cfg, k.I
    S, LC, TB, rows = cfg.S, cfg.LC, cfg.TB, cfg.rows
    kr = min(8, rows)
    RB = 16
    with P.scope():
        qT = P.sbuf("na_qT", [128, TB], BF16)
        kT = P.sbuf("na_kT", [128, TB], BF16)
        V = P.sbuf("na_V", [64, rows, 128], BF16)
        Vc = P.sbuf("na_Vc", [128, 2, 128], BF16)
        bias = P.sbuf("na_bias", [64, 8, 512], F32)
        ps_s = [P.psum("na_ps%d" % i, [128, 2, 512], F32) for i in range(1)]
        pTl = P.psum("na_pTl", [64, 8, 64], BF16)
        pTc = P.psum("na_pTc", [128, 2, 128], BF16)
        po = [P.psum("na_po%d" % i, [128, 128], F32) for i in range(2)]
        sl = [P.sbuf("na_sl%d" % i, [128, 768], F32) for i in range(2)]
        pe_ = [P.sbuf("na_pe%d" % i, [128, 768], BF16) for i in range(2)]
        st = [P.sbuf("na_st%d" % i, [128, 4], F32) for i in range(2)]
        pl = [P.sbuf("na_pl%d" % i, [64, 8, 64], BF16) for i in range(2)]
        pc = [P.sbuf("na_pc%d" % i, [128, 2, 128], BF16) for i in range(2)]
        ost = [P.sbuf("na_ost%d" % i, [64, RB, 128], BF16) for i in range(2)]
        oc = [P.sbuf("na_oc%d" % i, [128, 128], BF16) for i in range(2)]
        n = 0
        nos = 0
        for h in range(8):
            P.dma("sp", bias[:], I["na_bias"].ap[ie, h].rearrange("v q n -> q v n"), bias, I["na_bias"])
            for b in range(cfg.NB):
                tb0 = b * TB
                P.dma("sp", qT[:], k.PFM.ap[h * 128:(h + 1) * 128, tb0:tb0 + TB], qT, k.PFM)
                P.dma("sp", kT[:], k.PFM.ap[1024 + h * 128:1024 + (h + 1) * 128, tb0:tb0 + TB], kT, k.PFM)
                P.dma("sp", V[:], k.PTM.ap[tb0:tb0 + S, h * 128:(h + 1) * 128].rearrange("(r w) d -> w r d", w=64), V, k.PTM)
                P.dma("sp", Vc[:], k.PTM.ap[tb0 + S:tb0 + TB, h * 128:(h + 1) * 128].rearrange("(i p) d -> p i d", p=128), Vc, k.PTM)
                for r in range(rows):
                    i = n % 2; n += 1
                    rs = min(max(r - kr // 2, 0), rows - kr)
                    delta = r - rs
                    pp = ps_s[0]
                    q_ = qT[:, r * 64:(r + 1) * 64]
                    P.op("pe", lambda e: e.matmul(pp[0:64, 0, 0:kr * 64], q_, kT[:, rs * 64:(rs + kr) * 64], start=True, stop=True), [qT, kT], [pp], pe_acc=True)
                    P.op("pe", lambda e: e.matmul(pp[0:64, 1, 0:LC], q_, kT[:, S:S + LC], start=True, stop=True), [qT, kT], [pp], pe_acc=True)
                    s_ = sl[i]; e_ = pe_[i]; t_ = st[i]
                    nl = kr * 64
                    P.op("dve", lambda e: e.tensor_tensor(s_[0:64, 0:nl], pp[0:64, 0, 0:nl], bias[:, delta, 0:nl], ALU.add), [pp, bias], [s_])
                    P.op("act", lambda e: e.copy(s_[0:64, nl:nl + LC], pp[0:64, 1, 0:LC]), [pp], [s_])
                    P.op("dve", lambda e: e.tensor_reduce(t_[0:64, 0:1], s_[0:64, 0:nl + LC], AX.X, ALU.max, negate=True), [s_], [t_])
                    P.op("act", lambda e: e.activation(e_[0:64, 0:nl + LC], s_[0:64, 0:nl + LC], AF.Exp, bias=t_[0:64, 0:1], scale=1.0, accum_out=t_[0:64, 1:2]), [s_, t_], [e_, t_])
                    for j in range(kr):
                        P.op("pe", lambda e: e.transpose(pTl[:, j, :], e_[0:64, j * 64:(j + 1) * 64], k.identb[0:64, 0:64]), [e_, k.identb], [pTl], pe_acc=True)
                    for j in range(LC // 128):
                        P.op("pe", lambda e: e.transpose(pTc[:, j, 0:64], e_[0:64, nl + j * 128:nl + (j + 1) * 128], k.identb[0:64, 0:64]), [e_, k.identb], [pTc], pe_acc=True)
                    P.op("dve", lambda e: e.tensor_copy(pl[i][:, 0:kr, :], pTl[:, 0:kr, :]), [pTl], [pl[i]])
                    P.op("act", lambda e: e.copy(pc[i][:, :, 0:64], pTc[:, :, 0:64]), [pTc], [pc[i]])
                    o_ = po[i]
                    for j in range(kr):
                        P.op("pe", lambda e: e.matmul(o_[0:64, :], pl[i][:, j, :], V[:, rs + j, :], start=(j == 0), stop=False), [pl[i], V], [o_], pe_acc=True)
                    for j in range(LC // 128):
                        P.op("pe", lambda e: e.matmul(o_[0:64, :], pc[i][:, j, 0:64], Vc[:, j, :], start=False, stop=(j == LC // 128 - 1)), [pc[i], Vc], [o_], pe_acc=True)
                    P.op("dve", lambda e: e.reciprocal(t_[0:64, 2:3], t_[0:64, 1:2]), [t_], [t_])
                    os_ = ost[nos % 2]
                    P.op("dve", lambda e: e.tensor_scalar(os_[:, r % RB, :], o_[0:64, :], t_[0:64, 2:3], None, ALU.mult), [o_, t_], [os_])
                    if r % RB == RB - 1 or r == rows - 1:
                        r0 = (r // RB) * RB
                        nr = r - r0 + 1
                        P.dma("pool", k.YTM.ap[tb0 + r0 * 64:tb0 + (r0 + nr) * 64, h * 128:(h + 1) * 128].rearrange("(r w) d -> w r d", w=64), os_[:, 0:nr, :], k.YTM, os_)
                        nos += 1
                if ctx_out:
                    for qi in range(LC // 128):
                        i = n % 2; n += 1
                        pp = ps_s[0]
                        s_ = sl[i]; e_ = pe_[i]; t_ = st[i]
                        P.op("pe", lambda e: e.matmul(pp[:, 0, 0:LC], qT[:, S + qi * 128:S + (qi + 1) * 128], kT[:, S:S + LC], start=True, stop=True), [qT, kT], [pp], pe_acc=True)
                        P.op("dve", lambda e: e.tensor_reduce(t_[:, 0:1], pp[:, 0, 0:LC], AX.X, ALU.max, negate=True), [pp], [t_])
                        P.op("act", lambda e: e.activation(e_[:, 0:LC], pp[:, 0, 0:LC], AF.Exp, bias=t_[:, 0:1], scale=1.0, accum_out=t_[:, 1:2]), [pp, t_], [e_, t_])
                        for j in range(LC // 128):
                            P.op("pe", lambda e: e.transpose(pTc[:, j, :], e_[:, j * 128:(j + 1) * 128], k.identb[:]), [e_, k.identb], [pTc], pe_acc=True)
                        P.op("act", lambda e: e.copy(pc[i][:], pTc[:]), [pTc], [pc[i]])
                        o_ = po[i]
                        for j in range(LC // 128):
                            P.op("pe", lambda e: e.matmul(o_[:], pc[i][:, j, :], Vc[:, j, :], start=(j == 0), stop=(j == LC // 128 - 1)), [pc[i], Vc], [o_], pe_acc=True)
                        P.op("dve", lambda e: e.reciprocal(t_[:, 2:3], t_[:, 1:2]), [t_], [t_])
                        P.op("dve", lambda e: e.tensor_scalar(oc[i][:], o_[:], t_[:, 2:3], None, ALU.mult), [o_, t_], [oc[i]])
                        P.dma("pool", k.YTM.ap[tb0 + S + qi * 128:tb0 + S + (qi + 1) * 128, h * 128:(h + 1) * 128], oc[i][:], k.YTM, oc[i])


def mixer_gla(k, ie):
    P, cfg, I = k.P, k.cfg, k.I
    S, LC, TB = cfg.S, cfg.LC, cfg.TB
    nlt = S // 128
    nct = LC // 128
    with P.scope():
        cm = P.sbuf("gl_cm", [128, 2, 4, 128], F32)
        P.dma("sp", cm[:], I["gla_cm"].ap.rearrange("d m s t -> s d m t"), cm, I["gla_cm"])
        mk = P.sbuf("gl_mk", [128, 2, 128], F32)
        P.dma("sp", mk[:], I["gla_mask"].ap.rearrange("d s t -> s d t"), mk, I["gla_mask"])
        gup = P.sbuf("gl_gup", [16, 2, 256], F32)
        P.dma("sp", gup[:], I["gk_up"].ap[ie, :, 0:16, :].rearrange("d r u -> r d u"), gup, I["gk_up"])
        gkb = P.sbuf("gl_gkb", [1, 2, 256], F32)
        P.dma("sp", gkb[:], I["gk_up"].ap[ie, :, 16:17, :].rearrange("d r u -> r d u"), gkb, I["gk_up"])
        ones1 = P.sbuf("gl_ones", [1, 128], F32)
        P.op("dve", lambda e: e.memset(ones1[:], 1.0), [], [ones1])
        gn = P.sbuf("gl_gn", [128, 256], F32)
        P.dma("sp", gn[:], bc_rows(I["gla_norm"].ap[ie:ie + 1, :]), gn, I["gla_norm"])
        qk = [P.sbuf("gl_qk%d" % i, [128, 1024], BF16) for i in range(2)]
        vv = [P.sbuf("gl_v%d" % i, [128, 1024], BF16) for i in range(2)]
        gg = [P.sbuf("gl_g%d" % i, [128, 1024], BF16) for i in range(2)]
        lrb = [P.sbuf("gl_lrb%d" % i, [16, 128], BF16) for i in range(2)]
        lrf = [P.sbuf("gl_lrf%d" % i, [16, 128], F32) for i in range(2)]
        rq = [P.sbuf("gl_rq%d" % i, [128, 2, 64], F32) for i in range(2)]
        rk = [P.sbuf("gl_rk%d" % i, [128, 2, 64], F32) for i in range(2)]
        qr = P.sbuf("gl_qr", [128, 512], BF16)
        kr_ = P.sbuf("gl_kr", [128, 512], BF16)
        tmp = [P.sbuf("gl_tmp%d" % i, [128, 256], F32) for i in range(4)]
        la = P.sbuf("gl_la", [128, 256], F32)
        lax = P.sbuf("gl_lax", [128, 512], F32)
        e3 = P.sbuf("gl_e3", [128, 256], F32)
        khat = P.sbuf("gl_khat", [128, 512], BF16)
        E1 = P.sbuf("gl_E1", [128, 512], F32); E2 = P.sbuf("gl_E2", [128, 512], F32); E4 = P.sbuf("gl_E4", [128, 512], F32)
        dec = P.sbuf("gl_dec", [128, 8], F32)
        qtT = P.sbuf("gl_qtT", [128, 512], BF16); ktT = P.sbuf("gl_ktT", [128, 512], BF16); qeT = P.sbuf("gl_qeT", [128, 512], BF16)
        att = [P.sbuf("gl_att%d" % i, [128, 128], BF16) for i in range(2)]
        St = [P.sbuf("gl_S%d" % i, [128, 256], F32) for i in range(4)]
        Sb = [P.sbuf("gl_Sb%d" % i, [128, 256], BF16) for i in range(4)]
        of = [P.sbuf("gl_of%d" % i, [128, 1024], F32) for i in range(2)]
        ot = P.sbuf("gl_ot", [128, 1024], F32)
        sq = P.sbuf("gl_sq", [128, 256], BF16)
        fs = P.sbuf("gl_fs", [128, 16], F32)
        sg = P.sbuf("gl_sg", [128, 1024], F32)
        yo = [P.sbuf("gl_yo%d" % i, [128, 1024], BF16) for i in range(2)]
        psZ = P.psum("gl_psZ", [128, 256], F32)
        ps1 = P.psum("gl_ps1", [128, 512], F32)
        ps2 = P.psum("gl_ps2", [128, 512], F32)
        ps4 = P.psum("gl_ps4", [128, 4, 128], F32)
        psT = P.psum("gl_psT", [128, 8, 128], BF16)
        psA = P.psum("gl_psA", [128, 128], F32)
        psO = P.psum("gl_psO", [128, 256], F32)
        psD = P.psum("gl_psD", [128, 256], F32)
        n = 0
        SCALE = 128.0 ** -0.5

        def v4(ap):
            return ap.rearrange("p (h a b i) -> p h a b i", h=4, a=2, b=2)

        def u3(ap):
            return ap.rearrange("p (h a i) -> p h a i", h=4, a=2)

        for b in range(cfg.NB):
            tb0 = b * TB
            for d in range(2):
                for h in range(4):
                    P.op("dve", lambda e: e.memset(St[h][:], 0.0), [], [St[h]])
                    P.op("pool", lambda e: e.memset(Sb[h][:], 0.0), [], [Sb[h]])
                if d == 0:
                    order = [(True, i) for i in range(nct)] + [(False, t) for t in range(nlt)]
                else:
                    order = [(True, i) for i in reversed(range(nct))] + [(False, t) for t in reversed(range(nlt))]
                for (is_ctx, ti) in order:
                    i = n % 2; n += 1
                    t0 = tb0 + (S + ti * 128 if is_ctx else ti * 128)
                    q_, v_, g_ = qk[i], vv[i], gg[i]
                    P.dma("sp", q_[:], k.PTM.ap[t0:t0 + 128, 1024:2048], q_, k.PTM)
                    P.dma("sp", v_[:], k.PTM.ap[t0:t0 + 128, 2048:3072], v_, k.PTM)
                    P.dma("sp", lrb[i][:], k.PFM.ap[2048 + d * 16:2048 + (d + 1) * 16, t0:t0 + 128], lrb[i], k.PFM)
                    P.op("act", lambda e: e.copy(lrf[i][:], lrb[i][:]), [lrb[i]], [lrf[i]])
                    if not is_ctx:
                        P.dma("sp", rq[i][:], I["rope_q"].ap[ti * 128:(ti + 1) * 128], rq[i], I["rope_q"])
                        P.dma("sp", rk[i][:], I["rope_k"].ap[ti * 128:(ti + 1) * 128], rk[i], I["rope_k"])
                        for (src, dst, tab, eng) in ((q_[:, 0:512], qr, rq[i], "dve"), (q_[:, 512:1024], kr_, rk[i], "pool")):
                            x1 = v4(src)[:, :, :, 0, :]; x2 = v4(src)[:, :, :, 1, :]
                            o1 = v4(dst[:])[:, :, :, 0, :]; o2 = v4(dst[:])[:, :, :, 1, :]
                            cs = tab[:, 0, :].rearrange("p (a i) -> p a i", a=2).unsqueeze(1).to_broadcast([128, 4, 2, 32])
                            sn = tab[:, 1, :].rearrange("p (a i) -> p a i", a=2).unsqueeze(1).to_broadcast([128, 4, 2, 32])
                            ta, tb_ = u3(tmp[0][:]), u3(tmp[1][:])
                            if eng == "pool":
                                ta, tb_ = u3(tmp[2][:]), u3(tmp[3][:])
                            tA = tmp[0] if eng == "dve" else tmp[2]
                            tB = tmp[1] if eng == "dve" else tmp[3]
                            P.op(eng, lambda e: e.tensor_tensor(ta, x1, cs, ALU.mult), [q_, tab], [tA])
                            P.op(eng, lambda e: e.tensor_tensor(tb_, x2, sn, ALU.mult), [q_, tab], [tB])
                            P.op(eng, lambda e: e.tensor_tensor(o1, ta, tb_, ALU.subtract), [tA, tB], [dst])
                            P.op(eng, lambda e: e.tensor_tensor(ta, x2, cs, ALU.mult), [q_, tab, dst], [tA])
                            P.op(eng, lambda e: e.tensor_tensor(tb_, x1, sn, ALU.mult), [q_, tab, dst], [tB])
                            P.op(eng, lambda e: e.tensor_tensor(o2, ta, tb_, ALU.add), [tA, tB], [dst])
                    else:
                        P.op("dve", lambda e: e.tensor_scalar(qr[:], q_[:, 0:512], SCALE, None, ALU.mult), [q_], [qr])
                        P.op("pool", lambda e: e.tensor_copy(kr_[:], q_[:, 512:1024]), [q_], [kr_])
                    P.op("pe", lambda e: e.matmul(psZ[:], lrf[i][:], gup[:, d, :], start=True, stop=False), [lrf[i], gup], [psZ], pe_acc=True)
                    P.op("pe", lambda e: e.matmul(psZ[:], ones1[:], gkb[:, d, :], start=False, stop=True), [ones1, gkb], [psZ], pe_acc=True)
                    P.op("act", lambda e: e.activation(la[:], psZ[:], AF.Exp, scale=-1.0), [psZ], [la])
                    P.op("act", lambda e: e.activation(la[:], la[:], AF.Ln, bias=1.0, scale=1.0), [la], [la])
                    for bb in range(2):
                        P.op("pool", lambda e: e.tensor_copy(v4(lax[:])[:, :, :, bb, :], u3(la[:])), [la], [lax])
                    P.op("pe", lambda e: e.matmul(psZ[:], cm[:, d, 2, :], la[:], start=True, stop=True), [cm, la], [psZ], pe_acc=True)
                    P.op("act", lambda e: e.activation(e3[:], psZ[:], AF.Exp), [psZ], [e3])
                    for bb in range(2):
                        P.op("dve", lambda e: e.tensor_tensor(v4(khat[:])[:, :, :, bb, :], v4(kr_[:])[:, :, :, bb, :], u3(e3[:]), ALU.mult), [kr_, e3], [khat])
                    for h in range(4):
                        P.op("pe", lambda e: e.matmul(ps1[:, h * 128:(h + 1) * 128], lax[:, h * 128:(h + 1) * 128], cm[:, d, 0, :], start=True, stop=True), [lax, cm], [ps1], pe_acc=True)
                        P.op("pe", lambda e: e.matmul(ps2[:, h * 128:(h + 1) * 128], lax[:, h * 128:(h + 1) * 128], cm[:, d, 1, :], start=True, stop=True), [lax, cm], [ps2], pe_acc=True)
                        P.op("pe", lambda e: e.matmul(ps4[:, h, :], lax[:, h * 128:(h + 1) * 128], cm[:, d, 3, :], start=True, stop=True), [lax, cm], [ps4], pe_acc=True)
                    P.op("act", lambda e: e.activation(E1[:], ps1[:], AF.Exp), [ps1], [E1])
                    P.op("act", lambda e: e.activation(E2[:], ps1[:], AF.Exp, scale=-1.0), [ps1], [E2])
                    P.op("act", lambda e: e.activation(E4[:], ps2[:], AF.Exp), [ps2], [E4])
                    P.op("act", lambda e: e.activation(dec[:].rearrange("p (h c) -> p h c", h=4), ps4[:].rearrange("p h (c x) -> p h c x", x=64)[:, :, :, 0], AF.Exp), [ps4], [dec])
                    for h in range(4):
                        P.op("pe", lambda e: e.transpose(psT[:, h, :], qr[:, h * 128:(h + 1) * 128], k.identb[:]), [qr, k.identb], [psT], pe_acc=True)
                        P.op("pe", lambda e: e.transpose(psT[:, 4 + h, :], kr_[:, h * 128:(h + 1) * 128], k.identb[:]), [kr_, k.identb], [psT], pe_acc=True)
                    pq = psT[:, 0:4, :].rearrange("p h t -> p (h t)")
                    pk = psT[:, 4:8, :].rearrange("p h t -> p (h t)")
                    P.op("dve", lambda e: e.tensor_tensor(qtT[:], pq, E1[:], ALU.mult), [psT, E1], [qtT])
                    P.op("dve", lambda e: e.tensor_tensor(ktT[:], pk, E2[:], ALU.mult), [psT, E2], [ktT])
                    P.op("dve", lambda e: e.tensor_tensor(qeT[:], pq, E4[:], ALU.mult), [psT, E4], [qeT])
                    if d == 1:
                        P.dma("sp", of[i][:], k.OF.ap[t0:t0 + 128, :], of[i], k.OF)
                        P.dma("sp", g_[:], k.PTM.ap[t0:t0 + 128, 3072:4096], g_, k.PTM)
                    chunks = (0, 1) if d == 0 else (1, 0)
                    for h in range(4):
                        a_ = att[h % 2]
                        P.op("pe", lambda e: e.matmul(psA[:], ktT[:, h * 128:(h + 1) * 128], qtT[:, h * 128:(h + 1) * 128], start=True, stop=True), [ktT, qtT], [psA], pe_acc=True)
                        P.op("dve", lambda e: e.tensor_tensor(a_[:], psA[:], mk[:, d, :], ALU.mult), [psA, mk], [a_])
                        P.op("pe", lambda e: e.matmul(psO[:], a_[:], v_[:, h * 256:(h + 1) * 256], start=True, stop=False), [a_, v_], [psO], pe_acc=True)
                        for ci, c in enumerate(chunks):
                            rsl = slice(c * 64, (c + 1) * 64)
                            P.op("pe", lambda e: e.matmul(psO[rsl, :], qeT[:, h * 128 + c * 64:h * 128 + (c + 1) * 64], Sb[h][:], start=False, stop=(ci == 1)), [qeT, Sb[h]], [psO], pe_acc=True)
                            P.op("pe", lambda e: e.matmul(psD[:], khat[rsl, h * 128:(h + 1) * 128], v_[rsl, h * 256:(h + 1) * 256], start=True, stop=True), [khat, v_], [psD], pe_acc=True)
                            P.op("dve", lambda e: e.scalar_tensor_tensor(St[h][:], St[h][:], dec[:, h * 2 + c:h * 2 + c + 1], psD[:], ALU.mult, ALU.add), [St[h], dec, psD], [St[h]])
                            P.op("act", lambda e: e.copy(Sb[h][:], St[h][:]), [St[h]], [Sb[h]])
                        if d == 0:
                            P.op("act", lambda e: e.copy(of[i][:, h * 256:(h + 1) * 256], psO[:]), [psO], [of[i]])
                        else:
                            P.op("dve", lambda e: e.tensor_tensor(ot[:, h * 256:(h + 1) * 256], psO[:], of[i][:, h * 256:(h + 1) * 256], ALU.add), [psO, of[i]], [ot])
                    if d == 0:
                        P.dma("pool", k.OF.ap[t0:t0 + 128, :], of[i][:], k.OF, of[i])
                    else:
                        for h in range(4):
                            P.op("act", lambda e: e.activation(sq[:], ot[:, h * 256:(h + 1) * 256], AF.Square, accum_out=fs[:, h:h + 1]), [ot], [sq, fs])
                        P.op("dve", lambda e: e.tensor_scalar(fs[:, 4:8], fs[:, 0:4], 1.0 / 256.0, EPS, ALU.mult, ALU.add), [fs], [fs])
                        P.op("act", lambda e: e.activation(fs[:, 8:12], fs[:, 4:8], AF.Sqrt), [fs], [fs])
                        P.op("dve", lambda e: e.reciprocal(fs[:, 12:16], fs[:, 8:12]), [fs], [fs])
                        P.op("act", lambda e: e.activation(sg[:], g_[:], AF.Silu), [g_], [sg])
                        for h in range(4):
                            P.op("dve", lambda e: e.scalar_tensor_tensor(ot[:, h * 256:(h + 1) * 256], ot[:, h * 256:(h + 1) * 256], fs[:, 12 + h:13 + h], gn[:], ALU.mult, ALU.mult), [ot, fs, gn], [ot])
                        y_ = yo[i]
                        P.op("pool", lambda e: e.tensor_tensor(y_[:], ot[:], sg[:], ALU.mult), [ot, sg], [y_])
                        P.dma("pool", k.YTM.ap[t0:t0 + 128, 1024:2048], y_[:], k.YTM, y_)


def mixer_even(k, l, ie, ctx_out):
    mixer_na(k, ie, ctx_out)
    mixer_gla(k, ie)


def host_even(cfg, inp, m):
    S = cfg.S
    rows = cfg.rows
    f = lambda a: np.ascontiguousarray(a, dtype=np.float32)
    NE = inp["w_in_even"].shape[0]
    m["w_in_even"] = f(inp["w_in_even"]); m["w_out_even"] = f(inp["w_out_even"])
    rpb = np.asarray(inp["na_rpb"], np.float32)
    kr = min(8, rows)
    q = np.arange(64)[:, None]; w = np.arange(64)[None, :]
    cstart = np.clip(q - 8, 0, 64 - 16)
    col_ok = (w >= cstart) & (w < cstart + 16)
    coff = np.clip(w - q, -15, 15) + 15
    nab = np.full((NE, 8, 8, 64, 512), -1e30, np.float32)
    for delta in range(8):
        for j in range(kr):
            roff = j - delta + 7
            if roff < 0 or roff > 14:
                continue
            g = rpb[:, :, roff, :][:, :, coff]
            nab[:, :, delta, :, j * 64:(j + 1) * 64] = np.where(col_ok[None, None], g, np.float32(-1e30))
    m["na_bias"] = nab
    gk = np.zeros((NE, 2, 17, 256), np.float32)
    gk[:, :, 0:16, :] = inp["gla_gk_up"]
    gk[:, :, 16, :] = inp["gla_gk_bias"]
    m["gk_up"] = gk
    m["gla_norm"] = f(inp["gla_norm"])
    nf = 32
    inv = (10000.0 ** (-np.arange(nf, dtype=np.float32) / nf)).astype(np.float32)
    pos = np.arange(S)
    ang = np.stack([(pos // 64).astype(np.float32)[:, None] * inv[None, :], (pos % 64).astype(np.float32)[:, None] * inv[None, :]], axis=1)
    cs = np.cos(ang).astype(np.float32).reshape(S, 64); sn = np.sin(ang).astype(np.float32).reshape(S, 64)
    m["rope_k"] = f(np.stack([cs, sn], axis=1))
    m["rope_q"] = f(np.stack([cs, sn], axis=1) * np.float32(128.0 ** -0.5))
    cmat = np.zeros((2, 5, 128, 128), np.float32)
    mask = np.zeros((2, 128, 128), np.float32)
    s = np.arange(128)[:, None]; t = np.arange(128)[None, :]
    same = (s // 64) == (t // 64)
    g = -1.0 / 16.0
    for d in range(2):
        if d == 0:
            Mb = same & (s <= t)
            Mmid = same & ((s % 64) <= 31)
        else:
            Mb = same & (s >= t)
            Mmid = same & ((s % 64) >= 32)
        Mlast = same
        cmat[d, 0] = g * (Mb.astype(np.float32) - Mmid.astype(np.float32))
        cmat[d, 1] = g * Mb
        cmat[d, 2] = g * (Mlast.astype(np.float32) - Mb.astype(np.float32))
        cmat[d, 3] = g * Mlast
        mask[d] = Mb
    m["gla_cm"] = f(cmat[:, 0:4])
    m["gla_mask"] = mask

def host_inputs(cfg, inp, batches):
    NB, S, LC, E, NG = cfg.NB, cfg.S, cfg.LC, cfg.E, cfg.NG
    NL = len(cfg.layers)
    f = lambda a: np.ascontiguousarray(a, dtype=np.float32)
    m = {}
    m["x"] = f(inp["x"][batches].reshape(NB * S, D))
    m["ctx"] = f(inp["ctx"][batches].reshape(NB * LC, D))
    m["cvec"] = f(np.concatenate([inp["c"][batches], inp["c_ctx"][None, :]], axis=0))
    m["w_mod"] = f(inp["w_mod"][:NL]); m["b_mod"] = f(inp["b_mod"][:NL])
    m["norm_mix"] = f(inp["norm_mix"][:NL]); m["norm_ffn"] = f(inp["norm_ffn"][:NL])
    m["norm_final"] = f(inp["norm_final"][None, :])
    m["w_router"] = f(np.concatenate([inp["moe_w_group"][:NL], inp["moe_w_expert"][:NL]], axis=2))
    m["b_router"] = f(np.concatenate([inp["moe_b_group"][:NL], inp["moe_b_expert"][:NL]], axis=1))
    w1 = inp["moe_w1"][:NL]; w3 = inp["moe_w3"][:NL]; w2 = inp["moe_w2"][:NL]
    m["w1h"] = f(w1.reshape(NL, E, KC, 128, 512).transpose(0, 1, 3, 2, 4).reshape(NL * E * 128, KC * 512))
    m["w3h"] = f(w3.reshape(NL, E, KC, 128, 512).transpose(0, 1, 3, 2, 4).reshape(NL * E * 128, KC * 512))
    m["w2h"] = f(w2.reshape(NL, E, 4, 128, D).transpose(0, 1, 3, 2, 4).reshape(NL * E * 128, 4 * D))
    m["ident"] = np.eye(128, dtype=np.float32)
    m["lstrict"] = np.triu(np.ones((128, 128), np.float32), 1)
    m["iota_p"] = np.arange(128, dtype=np.float32).reshape(128, 1)
    m["blkstart"] = (128.0 * np.arange(cfg.nblk, dtype=np.float32)).reshape(1, -1)
    m["iota_e"] = np.arange(E, dtype=np.float32).reshape(1, -1)
    kinds = [kd for kd, _ in cfg.layers]
    if "even" in kinds:
        host_even(cfg, inp, m)
    if "odd" in kinds:
        host_odd(cfg, inp, m)
    return m


FULL_LAYERS = [("even", True), ("odd", True), ("even", False), ("odd", False)]


def kernel(**inputs):
    inp = {k_: np.asarray(v) for k_, v in inputs.items()}
    NCORE, NB = 4, 1
    cfg = Cfg(NB=NB, S=8192, layers=FULL_LAYERS)
    nc, k = build_program(cfg)
    maps = [host_inputs(cfg, inp, list(range(c * NB, (c + 1) * NB))) for c in range(NCORE)]
    for c in range(1, NCORE):
        for name, arr in maps[0].items():
            if name not in ("x", "ctx", "cvec"):
                maps[c][name] = arr
    res = run_bass_kernel_spmd(nc, maps, core_ids=list(range(NCORE)))
    out = np.concatenate([res.results[c]["out"].reshape(NB, 8192, D) for c in range(NCORE)], axis=0)
    return np.ascontiguousarray(out.astype(np.float32))
```
